# Optimizing a Trainium2 kernel written in Bass

```python
import math
import jax, jax.numpy as jnp
from jax import lax
import numpy as np

D_MODEL = 1024
BATCH = 8
SEQ = 4096
DEPTH = 4

HEAD_DIM = 64
N_HEADS = D_MODEL // HEAD_DIM
SWA_HEADS = N_HEADS // 4
SWA_KV_HEADS = SWA_HEADS // 2
SWA_WINDOW = 128
FOX_HEADS = N_HEADS // 4
NSA_HEADS = N_HEADS // 2
NSA_KV_HEADS = NSA_HEADS // 4
NSA_CMP_LEN = 32
NSA_CMP_STRIDE = 16
NSA_CMP_HIDDEN = 2 * HEAD_DIM
NSA_SLC_BLOCK = 64
NSA_TOPK = 16
NSA_WINDOW = 512
NSA_QUERY_BLOCK = 64
QUERY_BLOCK = 128
REL_BUCKETS = 32
REL_MAX_DISTANCE = 1024
BIAS_HEADS = SWA_HEADS + NSA_HEADS
FFN_HIDDEN = ((8 * D_MODEL + 3 * 256 - 1) // (3 * 256)) * 256
ADA_CHUNKS = 6
RMS_EPS = 1e-6
NEG_INF = -1e30
FORCE_SELECT = 1e30
SWA_WIDTH = SWA_HEADS * HEAD_DIM
FOX_WIDTH = FOX_HEADS * HEAD_DIM
NSA_WIDTH = NSA_HEADS * HEAD_DIM
NSA_KV_WIDTH = NSA_KV_HEADS * HEAD_DIM
IN_SPLITS = (
    SWA_WIDTH, SWA_KV_HEADS * HEAD_DIM, SWA_KV_HEADS * HEAD_DIM,
    FOX_WIDTH, FOX_WIDTH, FOX_WIDTH, FOX_HEADS,
    NSA_WIDTH,
    NSA_KV_WIDTH, NSA_KV_WIDTH,
    NSA_KV_WIDTH, NSA_KV_WIDTH,
    NSA_KV_WIDTH, NSA_KV_WIDTH,
    NSA_HEADS * 3,
)
N_IN = sum(IN_SPLITS)

kernel_name = "hybrid_swa_fox_nsa_block"


def rms_norm(x, gain):
    x32 = x.astype(jnp.float32)
    y = x32 * lax.rsqrt(jnp.mean(x32 * x32, axis=-1, keepdims=True) + RMS_EPS)
    return (y * gain.astype(jnp.float32)).astype(x.dtype)


def t5_bucket(dist):
    n = jnp.maximum(dist, 0)
    max_exact = REL_BUCKETS // 2
    nf = jnp.maximum(n, 1).astype(jnp.float32)
    large = max_exact + (jnp.log(nf / max_exact) / math.log(REL_MAX_DISTANCE / max_exact)
                         * (REL_BUCKETS - max_exact)).astype(jnp.int32)
    large = jnp.minimum(large, REL_BUCKETS - 1)
    return jnp.where(n < max_exact, n, large)


def split_heads(t, n_heads):
    return t.reshape(t.shape[0], t.shape[1], n_heads, HEAD_DIM)


def banded_attention(q, k, v, window, bias_table, sinks):
    b, s_len, h, d = q.shape
    hkv = k.shape[2]
    g = h // hkv
    n_blk = s_len // QUERY_BLOCK
    n_back = -(-(window - 1) // QUERY_BLOCK)
    pad = n_back * QUERY_BLOCK
    lk = pad + QUERY_BLOCK
    kp = jnp.pad(k, ((0, 0), (pad, 0), (0, 0), (0, 0)))
    vp = jnp.pad(v, ((0, 0), (pad, 0), (0, 0), (0, 0)))
    dist = jnp.arange(QUERY_BLOCK)[:, None] + pad - jnp.arange(lk)[None, :]
    in_window = (dist >= 0) & (dist < window)
    bias = bias_table[t5_bucket(dist)].astype(jnp.float32).transpose(2, 0, 1).reshape(hkv, g, QUERY_BLOCK, lk)
    q_blocks = jnp.moveaxis(q.reshape(b, n_blk, QUERY_BLOCK, hkv, g, d), 1, 0)
    scale = 1.0 / math.sqrt(d)

    def one_block(args):
        i, q_blk = args
        start = i * QUERY_BLOCK
        k_blk = lax.dynamic_slice_in_dim(kp, start, lk, axis=1)
        v_blk = lax.dynamic_slice_in_dim(vp, start, lk, axis=1)
        key_pos = start - pad + jnp.arange(lk)
        valid = in_window & (key_pos >= 0)[None, :]
        logits = jnp.einsum('bqhgd,bkhd->bhgqk', q_blk, k_blk).astype(jnp.float32) * scale + bias
        logits = jnp.where(valid, logits, NEG_INF)
        if sinks is None:
            p = jax.nn.softmax(logits, axis=-1)
        else:
            sink = sinks.astype(jnp.float32).reshape(1, hkv, g, 1, 1)
            m = jnp.maximum(jnp.max(logits, axis=-1, keepdims=True), sink)
            e = jnp.exp(logits - m)
            p = e / (jnp.sum(e, axis=-1, keepdims=True) + jnp.exp(sink - m))
        return jnp.einsum('bhgqk,bkhd->bqhgd', p.astype(v.dtype), v_blk)

    out = lax.map(one_block, (jnp.arange(n_blk), q_blocks))
    return jnp.moveaxis(out, 0, 1).reshape(b, s_len, h * d)


def forgetting_attention(q, k, v, log_f):
    b, s_len, h, d = q.shape
    n_blk = s_len // QUERY_BLOCK
    cum = lax.cumsum(log_f.astype(jnp.float32), axis=1)
    cum_k = cum.transpose(0, 2, 1)
    q_blocks = jnp.moveaxis(q.reshape(b, n_blk, QUERY_BLOCK, h, d), 1, 0)
    cum_q_blocks = jnp.moveaxis(cum_k.reshape(b, h, n_blk, QUERY_BLOCK), 2, 0)
    key_pos = jnp.arange(s_len)
    scale = 1.0 / math.sqrt(d)

    def one_block(args):
        i, q_blk, cq = args
        q_pos = i * QUERY_BLOCK + jnp.arange(QUERY_BLOCK)
        logits = (jnp.einsum('bqhd,bkhd->bhqk', q_blk, k).astype(jnp.float32) * scale
                  + (cq[..., None] - cum_k[:, :, None, :]))
        logits = jnp.where(key_pos[None, :] <= q_pos[:, None], logits, NEG_INF)
        p = jax.nn.softmax(logits, axis=-1)
        return jnp.einsum('bhqk,bkhd->bqhd', p.astype(v.dtype), v)

    out = lax.map(one_block, (jnp.arange(n_blk), q_blocks, cum_q_blocks))
    return jnp.moveaxis(out, 0, 1).reshape(b, s_len, h * d)


def compress_blocks(x, pe, w1, w2):
    b, s_len, hkv, d = x.shape
    n_c = (s_len - NSA_CMP_LEN) // NSA_CMP_STRIDE + 1
    idx = jnp.arange(n_c)[:, None] * NSA_CMP_STRIDE + jnp.arange(NSA_CMP_LEN)[None, :]
    blocks = x[:, idx] + pe[None, None, :, None, :]
    flat = blocks.transpose(0, 1, 3, 2, 4).reshape(b, n_c, hkv, NSA_CMP_LEN * d)
    return jax.nn.gelu(flat @ w1) @ w2


def nsa_attention(q, k_c, v_c, k_s, v_s, k_w, v_w, gates, cmp_pe, cmp_w1, cmp_w2, bias_table):
    b, s_len, h, d = q.shape
    hkv = k_s.shape[2]
    g = h // hkv
    scale = 1.0 / math.sqrt(d)
    qg = q.reshape(b, s_len, hkv, g, d)
    pos = jnp.arange(s_len)

    k_cmp = compress_blocks(k_c, cmp_pe[0], cmp_w1[0], cmp_w2[0])
    v_cmp = compress_blocks(v_c, cmp_pe[1], cmp_w1[1], cmp_w2[1])
    n_c = k_cmp.shape[1]
    blk_end = jnp.arange(n_c) * NSA_CMP_STRIDE + NSA_CMP_LEN - 1
    dist_c = pos[:, None] - blk_end[None, :]
    valid_c = dist_c >= 0
    bias_c = bias_table[t5_bucket(dist_c)].astype(jnp.float32).transpose(2, 0, 1).reshape(hkv, g, s_len, n_c)
    logits = jnp.einsum('bthgd,bnhd->bhgtn', qg, k_cmp).astype(jnp.float32) * scale + bias_c
    logits = jnp.where(valid_c, logits, NEG_INF)
    p_cmp = jnp.where(valid_c, jax.nn.softmax(logits, axis=-1), 0.0)
    o_cmp = jnp.einsum('bhgtn,bnhd->bthgd', p_cmp.astype(v_cmp.dtype), v_cmp)

    n_s = s_len // NSA_SLC_BLOCK
    top_k = min(NSA_TOPK, n_s)
    c_start = jnp.arange(n_c) * NSA_CMP_STRIDE
    s_start = jnp.arange(n_s) * NSA_SLC_BLOCK
    overlap = jnp.clip(jnp.minimum(c_start[:, None] + NSA_CMP_LEN, s_start[None, :] + NSA_SLC_BLOCK)
                       - jnp.maximum(c_start[:, None], s_start[None, :]), 0, None).astype(jnp.float32) / NSA_CMP_LEN
    importance = jnp.einsum('bhgtn,ns->bhts', p_cmp, overlap)
    q_blk_id = pos // NSA_SLC_BLOCK
    blk = jnp.arange(n_s)
    forced = (blk[None, :] == 0) | (blk[None, :] == q_blk_id[:, None]) | (blk[None, :] == q_blk_id[:, None] - 1)
    future = blk[None, :] > q_blk_id[:, None]
    importance = jnp.where(forced, FORCE_SELECT, jnp.where(future, NEG_INF, importance))
    _, sel = lax.top_k(importance, top_k)

    k_blocks = k_s.reshape(b, n_s, NSA_SLC_BLOCK, hkv, d).transpose(0, 3, 1, 2, 4)
    v_blocks = v_s.reshape(b, n_s, NSA_SLC_BLOCK, hkv, d).transpose(0, 3, 1, 2, 4)
    bias_sel_tab = bias_table.reshape(REL_BUCKETS, hkv, g).transpose(1, 0, 2)
    nq = s_len // NSA_QUERY_BLOCK
    q_blocks = jnp.moveaxis(qg.reshape(b, nq, NSA_QUERY_BLOCK, hkv, g, d), 1, 0)
    sel_blocks = jnp.moveaxis(sel.reshape(b, hkv, nq, NSA_QUERY_BLOCK, top_k), 2, 0)
    bi = jnp.arange(b)[:, None, None, None]
    hi = jnp.arange(hkv)[None, :, None, None]
    n_sel = top_k * NSA_SLC_BLOCK

    def one_block(args):
        i, q_blk, sel_blk = args
        k_g = k_blocks[bi, hi, sel_blk].reshape(b, hkv, NSA_QUERY_BLOCK, n_sel, d)
        v_g = v_blocks[bi, hi, sel_blk].reshape(b, hkv, NSA_QUERY_BLOCK, n_sel, d)
        tok_pos = (sel_blk[..., None] * NSA_SLC_BLOCK + jnp.arange(NSA_SLC_BLOCK)).reshape(b, hkv, NSA_QUERY_BLOCK, n_sel)
        q_pos = i * NSA_QUERY_BLOCK + jnp.arange(NSA_QUERY_BLOCK)
        dist = q_pos[None, None, :, None] - tok_pos
        bias = bias_sel_tab[hi, t5_bucket(dist)].astype(jnp.float32).transpose(0, 1, 4, 2, 3)
        logits = jnp.einsum('bqhgd,bhqnd->bhgqn', q_blk, k_g).astype(jnp.float32) * scale + bias
        logits = jnp.where((dist >= 0)[:, :, None], logits, NEG_INF)
        p = jax.nn.softmax(logits, axis=-1)
        return jnp.einsum('bhgqn,bhqnd->bqhgd', p.astype(v_g.dtype), v_g)

    o_slc = jnp.moveaxis(lax.map(one_block, (jnp.arange(nq), q_blocks, sel_blocks)), 0, 1)

    o_win = banded_attention(q, k_w, v_w, NSA_WINDOW, bias_table, None).reshape(b, s_len, h, d)

    o = (gates[..., 0:1] * o_cmp.reshape(b, s_len, h, d)
         + gates[..., 1:2] * o_slc.reshape(b, s_len, h, d)
         + gates[..., 2:3] * o_win)
    return o.reshape(b, s_len, h * d)


def setup_inputs(seed: int = 0) -> dict:
    key = jax.random.key(seed)
    ks = jax.random.split(key, 20)

    def normal(k, shape, scale):
        return jax.random.normal(k, shape, jnp.float32) * scale

    def gain(k):
        return 1.0 + normal(k, (DEPTH, D_MODEL), 0.05)

    return {
        "x": normal(ks[0], (BATCH, SEQ, D_MODEL), 1.0),
        "c": normal(ks[1], (BATCH, D_MODEL), 1.0),
        "rel_bias": normal(ks[2], (REL_BUCKETS, BIAS_HEADS), 0.5),
        "ada_w": normal(ks[3], (DEPTH, D_MODEL, ADA_CHUNKS * D_MODEL), D_MODEL ** -0.5),
        "ada_b": normal(ks[4], (DEPTH, ADA_CHUNKS * D_MODEL), 0.02),
        "attn_pre_norm": gain(ks[5]),
        "attn_post_norm": gain(ks[6]),
        "ffn_pre_norm": gain(ks[7]),
        "ffn_post_norm": gain(ks[8]),
        "w_in": normal(ks[9], (DEPTH, D_MODEL, N_IN), D_MODEL ** -0.5),
        "forget_bias": 3.0 + normal(ks[10], (DEPTH, FOX_HEADS), 0.5),
        "swa_sinks": normal(ks[11], (DEPTH, SWA_HEADS), 0.5),
        "cmp_pos": normal(ks[12], (DEPTH, 2, NSA_CMP_LEN, HEAD_DIM), 0.1),
        "cmp_w1": normal(ks[13], (DEPTH, 2, NSA_CMP_LEN * HEAD_DIM, NSA_CMP_HIDDEN), (NSA_CMP_LEN * HEAD_DIM) ** -0.5),
        "cmp_w2": normal(ks[14], (DEPTH, 2, NSA_CMP_HIDDEN, HEAD_DIM), NSA_CMP_HIDDEN ** -0.5),
        "group_norm": gain(ks[15]),
        "w_out": normal(ks[16], (DEPTH, D_MODEL, D_MODEL), D_MODEL ** -0.5),
        "ffn_w_gate": normal(ks[17], (DEPTH, D_MODEL, FFN_HIDDEN), D_MODEL ** -0.5),
        "ffn_w_up": normal(ks[18], (DEPTH, D_MODEL, FFN_HIDDEN), D_MODEL ** -0.5),
        "ffn_w_down": normal(ks[19], (DEPTH, FFN_HIDDEN, D_MODEL), FFN_HIDDEN ** -0.5),
    }


def reference(x, c, rel_bias, ada_w, ada_b, attn_pre_norm, attn_post_norm, ffn_pre_norm, ffn_post_norm,
              w_in, forget_bias, swa_sinks, cmp_pos, cmp_w1, cmp_w2, group_norm, w_out,
              ffn_w_gate, ffn_w_up, ffn_w_down):
    b, s_len, _ = x.shape
    offsets = np.cumsum(IN_SPLITS)[:-1].tolist()
    c_act = jax.nn.silu(c)
    bias_swa = rel_bias[:, :SWA_HEADS]
    bias_nsa = rel_bias[:, SWA_HEADS:]
    for layer in range(DEPTH):
        mod = c_act @ ada_w[layer] + ada_b[layer]
        shift_a, scale_a, gate_a, shift_f, scale_f, gate_f = [m[:, None, :] for m in jnp.split(mod, ADA_CHUNKS, axis=-1)]

        h = rms_norm(x, attn_pre_norm[layer]) * (1 + scale_a) + shift_a
        proj = h @ w_in[layer]
        qa, ka, va, qb, kb, vb, fb, qc, kc, vc, ksl, vsl, kw, vw, gc = jnp.split(proj, offsets, axis=-1)

        o_swa = banded_attention(split_heads(qa, SWA_HEADS), split_heads(ka, SWA_KV_HEADS),
                                 split_heads(va, SWA_KV_HEADS), SWA_WINDOW, bias_swa, swa_sinks[layer])
        log_f = jax.nn.log_sigmoid(fb.astype(jnp.float32) + forget_bias[layer].astype(jnp.float32))
        o_fox = forgetting_attention(split_heads(qb, FOX_HEADS), split_heads(kb, FOX_HEADS),
                                     split_heads(vb, FOX_HEADS), log_f)
        gates = jax.nn.sigmoid(gc.reshape(b, s_len, NSA_HEADS, 3))
        o_nsa = nsa_attention(split_heads(qc, NSA_HEADS),
                              split_heads(kc, NSA_KV_HEADS), split_heads(vc, NSA_KV_HEADS),
                              split_heads(ksl, NSA_KV_HEADS), split_heads(vsl, NSA_KV_HEADS),
                              split_heads(kw, NSA_KV_HEADS), split_heads(vw, NSA_KV_HEADS),
                              gates, cmp_pos[layer], cmp_w1[layer], cmp_w2[layer], bias_nsa)
        gn = group_norm[layer]
        mixed = jnp.concatenate([
            rms_norm(o_swa, gn[:SWA_WIDTH]),
            rms_norm(o_fox, gn[SWA_WIDTH:SWA_WIDTH + FOX_WIDTH]),
            rms_norm(o_nsa, gn[SWA_WIDTH + FOX_WIDTH:]),
        ], axis=-1)
        y = mixed @ w_out[layer]
        x = x + gate_a * rms_norm(y, attn_post_norm[layer])

        h = rms_norm(x, ffn_pre_norm[layer]) * (1 + scale_f) + shift_f
        y = (jax.nn.silu(h @ ffn_w_gate[layer]) * (h @ ffn_w_up[layer])) @ ffn_w_down[layer]
        x = x + gate_f * rms_norm(y, ffn_post_norm[layer])
    return x
```

```python
import math
import numpy as np
import ml_dtypes
import concourse.bass as bass
import concourse.mybir as mybir
from concourse.bass_utils import run_bass_kernel_spmd

F32 = mybir.dt.float32
BF16 = mybir.dt.bfloat16
U8 = mybir.dt.uint8
AF = mybir.ActivationFunctionType
ALU = mybir.AluOpType

D = 1024
HD = 64
FFN = 2816
NIN = 2588
NEG = -30000.0
DSZ = {F32: 4, BF16: 2, U8: 1}

COMPUTE = ("act", "dve", "pool", "pe")
DMAQ = ("sp", "act")


class Res:
    __slots__ = ("name", "writers", "readers", "wdeps")

    def __init__(self, name=""):
        self.name = name
        self.writers = []
        self.readers = []
        self.wdeps = []


class Tile(Res):
    __slots__ = ("ap",)

    def __init__(self, name, ap):
        Res.__init__(self, name)
        self.ap = ap

    def __getitem__(self, k):
        return self.ap[k]


class Op:
    __slots__ = ("eng", "fn", "deps", "dma", "signal", "sem", "val", "epoch", "prev")

    def __init__(self, eng, fn, dma, epoch):
        self.eng = eng
        self.fn = fn
        self.dma = dma
        self.deps = {}
        self.signal = False
        self.sem = None
        self.val = 0
        self.epoch = epoch
        self.prev = None


class Prog:
    def __init__(self, nc):
        self.nc = nc
        self.ops = []
        self.epoch = 0

    @staticmethod
    def _push(lst, op):
        if not op.dma:
            for i, o in enumerate(lst):
                if (not o.dma) and o.eng == op.eng:
                    lst[i] = op
                    return
        lst.append(op)

    def add(self, eng, fn, r=(), w=(), dma=False):
        op = Op(eng, fn, dma, self.epoch)
        deps = op.deps
        for res in r:
            for wop in res.writers:
                deps[wop] = True
        for res in w:
            if res.readers:
                for rop in res.readers:
                    deps.setdefault(rop, False)
                for wop in res.writers:
                    deps.setdefault(wop, False)
            else:
                for pop in res.wdeps:
                    deps.setdefault(pop, False)
        for res in w:
            if res.readers or (res in r):
                if res.readers:
                    res.wdeps = list(res.readers) + list(res.writers)
                res.writers = [op]
                res.readers = []
            else:
                self._push(res.writers, op)
        for res in r:
            if res not in w:
                self._push(res.readers, op)
        self.ops.append(op)
        return op

    def dma(self, q, out_ap, in_ap, r=(), w=(), slow=False):
        if slow:
            return self.add(q, lambda e: e.dma_start(out=out_ap, in_=in_ap, allow_slow_non_contiguous=True),
                            r=r, w=w, dma=True)
        return self.add(q, lambda e: e.dma_start(out=out_ap, in_=in_ap), r=r, w=w, dma=True)

    def emit(self, stack, n_epochs):
        nc = self.nc
        NPOOL = 6
        engsem = {}
        for e in COMPUTE:
            engsem[e] = [stack.enter_context(nc.semaphore("c_%s_%d" % (e, k))) for k in range(n_epochs)]
        dmasem = {}
        for q in DMAQ:
            dmasem[q] = [stack.enter_context(nc.semaphore("d_%s_%d" % (q, k))) for k in range(NPOOL)]
        for op in self.ops:
            for d in op.deps:
                d.signal = True
        cnt = {}
        dcount = {}
        dlast = {}
        dk = {q: 0 for q in DMAQ}
        for op in self.ops:
            if op.dma:
                s = dmasem[op.eng][dk[op.eng] % NPOOL]
                dk[op.eng] += 1
                dcount[s] = dcount.get(s, 0) + 1
                op.sem = s
                op.val = 16 * dcount[s]
                op.prev = dlast.get(s)
                dlast[s] = op
            elif op.signal:
                key = (op.eng, op.epoch)
                cnt[key] = cnt.get(key, 0) + 1
                op.sem = engsem[op.eng][op.epoch]
                op.val = cnt[key]
        print("sem counts", {k: v for k, v in cnt.items()}, "dma max", max(dcount.values()) * 16 if dcount else 0)
        streams = {"sp": [], "act": [], "dve": [], "pool": [], "pe": []}
        for op in self.ops:
            streams[op.eng].append(op)
        nwaits = [0]

        def run(engname, e):
            known = {}
            kep = {}

            def need(d):
                if d.dma:
                    return known.get(id(d.sem), 0) < d.val
                if kep.get(d.eng, -1) > d.epoch:
                    return False
                return known.get(id(d.sem), 0) < d.val

            def wait(d):
                e.wait_ge(d.sem, d.val)
                nwaits[0] += 1
                known[id(d.sem)] = d.val
                if not d.dma:
                    if kep.get(d.eng, -1) < d.epoch:
                        kep[d.eng] = d.epoch

            for op in streams[engname]:
                for d, raw in op.deps.items():
                    if (not d.dma) and (not op.dma) and d.eng == op.eng:
                        if engname == "pe":
                            continue
                    if need(d):
                        wait(d)
                if op.dma and op.prev is not None and need(op.prev):
                    wait(op.prev)
                ins = op.fn(e)
                if op.dma:
                    ins.then_inc(op.sem, 16)
                elif op.signal:
                    ins.then_inc(op.sem, 1)
            if engname in DMAQ:
                for s in dmasem[engname]:
                    if s in dlast and known.get(id(s), 0) < dlast[s].val:
                        e.wait_ge(s, dlast[s].val)

        with nc.Block() as block:
            @block.sync
            def _(e):
                run("sp", e)

            @block.scalar
            def _(e):
                run("act", e)

            @block.vector
            def _(e):
                run("dve", e)

            @block.gpsimd
            def _(e):
                run("pool", e)

            @block.tensor
            def _(e):
                run("pe", e)
        return nwaits[0]


class Arena:
    def __init__(self, base_ap, size):
        self.base = base_ap
        self.size = size
        self.top = 0
        self.live = []
        self.grave = []

    def alloc(self, name, free_shape, dt, parts=128):
        n = 1
        for s in free_shape:
            n *= s
        nb = n * DSZ[dt]
        nb_al = (nb + 63) // 64 * 64
        off = self.top
        assert off + nb_al <= self.size, "SBUF arena overflow at %s: %d + %d > %d" % (name, off, nb_al, self.size)
        self.top += nb_al
        ap = self.base[0:parts, off:off + nb]
        if dt != U8:
            ap = ap.bitcast(dt)
        if len(free_shape) == 2:
            ap = ap.rearrange("p (a b) -> p a b", a=free_shape[0])
        elif len(free_shape) == 3:
            ap = ap.rearrange("p (a b c) -> p a b c", a=free_shape[0], b=free_shape[1])
        elif len(free_shape) == 4:
            ap = ap.rearrange("p (a b c d) -> p a b c d", a=free_shape[0], b=free_shape[1], c=free_shape[2])
        t = Tile(name, ap)
        keep = []
        for (g0, g1, gt) in self.grave:
            if g0 < off + nb_al and off < g1:
                t.readers.extend(gt.readers)
                t.readers.extend(gt.writers)
                if g0 >= off and g1 <= off + nb_al:
                    continue
            keep.append((g0, g1, gt))
        self.grave = keep
        self.live.append((off, off + nb_al, t))
        return t

    def mark(self):
        return (self.top, len(self.live))

    def release(self, m):
        top, nl = m
        for ent in self.live[nl:]:
            self.grave.append(ent)
        del self.live[nl:]
        self.top = top


class PsumArena:
    def __init__(self, banks):
        self.banks = banks
        self.top = 0
        self.live = []
        self.grave = []

    def alloc(self, name, ncols, dt=F32, parts=128):
        nb = ncols * DSZ[dt]
        nb = (nb + 3) // 4 * 4
        if self.top % 2048:
            self.top = (self.top // 2048 + 1) * 2048
        off = self.top
        assert off + nb <= 8 * 2048, "PSUM overflow at %s" % name
        self.top += nb
        bank = off // 2048
        c0 = (off % 2048) // 4
        ap = self.banks[bank][0:parts, c0:c0 + nb // 4]
        if dt != F32:
            ap = ap.bitcast(dt)
        t = Tile(name, ap)
        keep = []
        for (g0, g1, gt) in self.grave:
            if g0 < off + nb and off < g1:
                t.readers.extend(gt.readers)
                t.readers.extend(gt.writers)
                if g0 >= off and g1 <= off + nb:
                    continue
            keep.append((g0, g1, gt))
        self.grave = keep
        self.live.append((off, off + nb, t))
        return t

    def mark(self):
        return (self.top, len(self.live))

    def release(self, m):
        top, nl = m
        for ent in self.live[nl:]:
            self.grave.append(ent)
        del self.live[nl:]
        self.top = top


def _t5_bucket(dist):
    n = np.maximum(dist, 0)
    nf = np.maximum(n, 1).astype(np.float32)
    large = 16 + (np.log(nf / np.float32(16)) / np.float32(math.log(1024 / 16)) * np.float32(16)).astype(np.int32)
    large = np.minimum(large, 31)
    return np.where(n < 16, n, large)


def _onehot(dvals, valid):
    oh = np.zeros((33, len(dvals)), np.float32)
    b = _t5_bucket(dvals)
    for x in range(len(dvals)):
        if valid[x]:
            oh[b[x], x] = 1.0
        else:
            oh[32, x] = 1.0
    return oh


DL_SWA = 383
DL_NSA = 1791
DL_CMP = 6128


def host_consts(S):
    bf = ml_dtypes.bfloat16
    c = {}
    c["k_identb"] = np.eye(128, dtype=np.float32).astype(bf)
    c["k_identf"] = np.eye(128, dtype=np.float32)
    tp = np.arange(128)
    c["k_tri"] = (tp[:, None] <= tp[None, :]).astype(np.float32)
    d = np.arange(DL_SWA) - 127
    c["k_ohswa"] = _onehot(d, (d >= 0) & (d < 128))
    d = np.arange(DL_NSA) - 127
    c["k_ohnsa"] = _onehot(d, d >= 0)
    d = np.arange(DL_CMP) - 2063
    c["k_ohcmp"] = _onehot(d, d >= 0)
    s = np.arange(S)
    c["k_sel"] = (s[None, :] // 64 == np.arange(64)[:, None]).astype(np.float32).astype(bf)
    a = np.arange(128)[:, None]
    u = np.arange(1024)[None, :]
    c["k_wm"] = np.where(u - a < 512, 0.0, NEG).astype(np.float32).astype(bf)
    u = np.arange(512)[None, :]
    c["k_cm"] = np.where(u - a >= 0, 0.0, NEG).astype(np.float32).astype(bf)
    z = np.arange(127)[None, :] - 63
    rel = z - (np.arange(128)[:, None] // 64)
    forced = (rel == 0) | (rel == -1)
    future = rel > 0
    c["k_km"] = np.where(forced | future, 0.0, 1.0).astype(np.float32)
    c["k_ov"] = np.where(forced, 1e30, np.where(future, -1e30, 0.0)).astype(np.float32)
    n_c = S // 16 - 1
    cs = np.arange(n_c) * 16
    ss = np.arange(64) * 64
    ov = np.clip(np.minimum(cs[:, None] + 32, ss[None, :] + 64) - np.maximum(cs[:, None], ss[None, :]), 0, None)
    ovl = np.zeros((256, 64), np.float32)
    ovl[:n_c] = ov.astype(np.float32) / 32.0
    c["k_ovl"] = ovl
    c["k_ones"] = np.ones((8, S), np.float32).astype(bf)
    return c


CONST_SPECS = [
    ("k_identb", [128, 128], BF16), ("k_identf", [128, 128], F32), ("k_tri", [128, 128], F32),
    ("k_ohswa", [33, DL_SWA], F32), ("k_ohnsa", [33, DL_NSA], F32), ("k_ohcmp", [33, DL_CMP], F32),
    ("k_sel", None, BF16), ("k_wm", [128, 1024], BF16), ("k_cm", [128, 512], BF16),
    ("k_km", [128, 127], F32), ("k_ov", [128, 127], F32), ("k_ovl", [256, 64], F32),
    ("k_ones", 8, BF16),
]


WSPLITS = [
    ("swa_q", 0, 256), ("swa_k", 256, 128), ("swa_v", 384, 128), ("fox_q", 512, 256), ("fox_k", 768, 256),
    ("fox_v", 1024, 256), ("fb", 1280, 4), ("nsa_q", 1284, 512), ("kc", 1796, 128), ("vc", 1924, 128),
    ("ks", 2052, 128), ("vs", 2180, 128), ("kw", 2308, 128), ("vw", 2436, 128), ("gc", 2564, 24),
]
TORDER = ["swa_q", "fox_q", "nsa_q", "swa_k", "fox_k", "kc", "vc", "ks", "kw"]
VORDER = ["swa_v", "fox_v", "vs", "vw", "fb", "gc"]
QT_SWAQ, QT_FOXQ, QT_NSAQ, QT_SWAK, QT_FOXK, QT_KC, QT_VC, QT_KS, QT_KW = 0, 256, 512, 1024, 1152, 1408, 1536, 1664, 1792
NFT = 1920
V_SWA, V_FOX, V_S, V_W = 0, 128, 384, 512
NV = 640


def dap(t, offset, dims):
    return bass.AP(tensor=t.tensor, offset=offset, ap=[list(x) for x in dims])


def build(S, DEPTH, debug=False, stop_after=None):
    from contextlib import ExitStack
    nc = bass.Bass("TRN2", target_bir_lowering=False)
    NT = S // 128
    NQ = S // 512
    NCMP = S // 16 - 1
    NNT = (NCMP + 127) // 128
    nsz = [min(128, NCMP - 128 * k) for k in range(NNT)]

    def din(name, shape, dt=F32):
        return nc.dram_tensor(name, list(shape), dt, kind="ExternalInput").ap()

    def dscr(name, shape, dt):
        return nc.dram_tensor(name, list(shape), dt, kind="Internal").ap()

    x_in = din("x", [S, D])
    c_in = din("c", [D])
    rel_bias = din("rel_bias", [32, 12])
    ada_w = din("ada_w", [DEPTH, D, 6 * D])
    ada_b = din("ada_b", [DEPTH, 6 * D])
    g_apre = din("attn_pre_norm", [DEPTH, D])
    g_apost = din("attn_post_norm", [DEPTH, D])
    g_fpre = din("ffn_pre_norm", [DEPTH, D])
    g_fpost = din("ffn_post_norm", [DEPTH, D])
    w_in = din("w_in", [DEPTH, D, NIN])
    forget_bias = din("forget_bias", [DEPTH, 4])
    swa_sinks = din("swa_sinks", [DEPTH, 4])
    cmp_pos = din("cmp_pos", [DEPTH, 2, 32, 64])
    cmp_w1 = din("cmp_w1", [DEPTH, 2, 2048, 128])
    cmp_w2 = din("cmp_w2", [DEPTH, 2, 128, 64])
    group_norm = din("group_norm", [DEPTH, D])
    w_out = din("w_out", [DEPTH, D, D])
    w_gate = din("ffn_w_gate", [DEPTH, D, FFN])
    w_up = din("ffn_w_up", [DEPTH, D, FFN])
    w_down = din("ffn_w_down", [DEPTH, FFN, D])
    K = {}
    for name, shape, dt in CONST_SPECS:
        if shape is None:
            shape = [64, S]
        elif shape == 8:
            shape = [8, S]
        K[name] = din(name, shape, dt)
    out = nc.dram_tensor("out", [S, D], F32, kind="ExternalOutput").ap()

    qT_d = dscr("qT_d", [NFT, S], BF16)
    v_d = dscr("v_d", [S, NV], BF16)
    o_d = dscr("o_d", [S, D], F32)
    g_swa_d = dscr("g_swa_d", [4, DL_SWA], BF16)
    g_nsa_d = dscr("g_nsa_d", [8, DL_NSA], BF16)
    g_cmp_d = dscr("g_cmp_d", [8, DL_CMP], BF16)
    t_swa_d = dscr("t_swa_d", [4, 128, DL_SWA], BF16)
    t_nsa_d = dscr("t_nsa_d", [8, 128, DL_NSA], BF16)
    t_cmp_d = dscr("t_cmp_d", [8, 128, DL_CMP], BF16)
    wgu_d = dscr("wgu_d", [DEPTH, 22, 128, 2, 8, 128], BF16)
    wd_d = dscr("wd_d", [DEPTH, 128, 22, D], BF16)
    wo_d = dscr("wo_d", [DEPTH, 128, 8, D], BF16)
    dbg = {}
    if debug:
        dbg["qT"] = nc.dram_tensor("dbg_qT", [NFT, S], BF16, kind="ExternalOutput").ap()
        dbg["v"] = nc.dram_tensor("dbg_v", [S, NV], BF16, kind="ExternalOutput").ap()
        dbg["o"] = nc.dram_tensor("dbg_o", [S, D], F32, kind="ExternalOutput").ap()
        dbg["mod"] = nc.dram_tensor("dbg_mod", [128, 6 * D], F32, kind="ExternalOutput").ap()
        dbg["fg"] = nc.dram_tensor("dbg_fg", [128, NT * 28], F32, kind="ExternalOutput").ap()
        dbg["x1"] = nc.dram_tensor("dbg_x1", [S, D], F32, kind="ExternalOutput").ap()

    stack = ExitStack()
    with stack:
        ARENA_BYTES = 206 * 1024
        arena_t = stack.enter_context(nc.sbuf_tensor("arena", [128, ARENA_BYTES], U8))
        banks = [stack.enter_context(nc.psum_tensor("pb%d" % k, [128, 512], F32)) for k in range(8)]
        A = Arena(arena_t[:, :], ARENA_BYTES)
        PS = PsumArena([b[:, :] for b in banks])
        P = Prog(nc)
        RQT = Res("qT_d")
        RV = Res("v_d")
        RO = Res("o_d")
        RX = [Res("xblk%d" % k) for k in range(NQ)]
        RW = Res("wconv")
        rr = [0]

        def evac(out_t, out_ap, in_t, in_ap, scale=None, eng=None):
            rr[0] += 1
            if eng == "act" or (eng is None and rr[0] % 2 == 0):
                if scale is None:
                    P.add("act", lambda e: e.copy(out=out_ap, in_=in_ap), r=[in_t], w=[out_t])
                else:
                    P.add("act", lambda e: e.mul(out=out_ap, in_=in_ap, mul=scale), r=[in_t], w=[out_t])
            else:
                if scale is None:
                    P.add("dve", lambda e: e.tensor_copy(out=out_ap, in_=in_ap), r=[in_t], w=[out_t])
                else:
                    P.add("dve", lambda e: e.tensor_scalar(out=out_ap, in0=in_ap, scalar1=scale, scalar2=None,
                                                           op0=ALU.mult), r=[in_t], w=[out_t])

        def mm(out_t, out_ap, lt, lap, rt, rap, start, stop):
            P.add("pe", lambda e: e.matmul(out_ap, lap, rap, start=start, stop=stop), r=[lt, rt], w=[out_t])

        def tr(out_t, out_ap, in_t, in_ap, ident_t, ident_ap):
            P.add("pe", lambda e: e.transpose(out_ap, in_ap, ident_ap), r=[in_t, ident_t], w=[out_t])

        identb = A.alloc("identb", [128], BF16)
        identf = A.alloc("identf", [128], F32)
        tri = A.alloc("tri", [128], F32)
        onesf = A.alloc("onesf", [128], F32)
        onesb = A.alloc("onesb", [512], BF16)
        cmt = A.alloc("cm", [512], BF16)
        wmt = A.alloc("wm", [1024], BF16)
        kmt = A.alloc("km", [127], F32)
        ovt = A.alloc("ov", [127], F32)
        P.dma("sp", identb[:, :], K["k_identb"], w=[identb])
        P.dma("sp", identf[:, :], K["k_identf"], w=[identf])
        P.dma("sp", tri[:, :], K["k_tri"], w=[tri])
        P.dma("sp", cmt[:, :], K["k_cm"], w=[cmt])
        P.dma("sp", wmt[:, :], K["k_wm"], w=[wmt])
        P.dma("sp", kmt[:, :], K["k_km"], w=[kmt])
        P.dma("sp", ovt[:, :], K["k_ov"], w=[ovt])
        P.add("dve", lambda e: e.memset(onesf[:, :], 1.0), w=[onesf])
        P.add("dve", lambda e: e.memset(onesb[:, :], 1.0), w=[onesb])

        cvf = [A.alloc("cvf%d" % k, [1024], F32) for k in range(2)]
        cvb = [A.alloc("cvb%d" % k, [1024], BF16) for k in range(2)]
        cvk = [0]
        pend = []

        def conv_chunks(l):
            jobs = []

            def job(load_src_ap, a, dst_ap):
                def run(q="sp"):
                    f, b = cvf[cvk[0] % 2], cvb[cvk[0] % 2]
                    cvk[0] += 1
                    fv = f[:, :].rearrange("p (a b) -> p a b", a=a) if a > 1 else f[:, :]
                    bv = b[:, :].rearrange("p (a b) -> p a b", a=a) if a > 1 else b[:, :]
                    P.dma(q, fv, load_src_ap, w=[f])
                    if pend:
                        pend.pop(0)()
                    P.add("pool", lambda e: e.tensor_copy(out=b[:, :], in_=f[:, :]), r=[f], w=[b])
                    pend.append(lambda: P.dma(q, dst_ap, bv, r=[b], w=[RW]))
                jobs.append(run)

            for hc in range(22):
                for gi, src in enumerate((w_gate, w_up)):
                    job(src[l, :, hc * 128:(hc + 1) * 128].rearrange("(c p) n -> p c n", p=128), 8, wgu_d[l, hc, :, gi])
            for hc in range(22):
                job(w_down[l, hc * 128:(hc + 1) * 128, :], 1, wd_d[l, :, hc, :])
            for c in range(8):
                job(w_out[l, c * 128:(c + 1) * 128, :], 1, wo_d[l, :, c, :])
            return jobs

        m0 = A.mark()
        pm0 = PS.mark()
        tabl = A.alloc("tabl", [12], F32, parts=33)
        P.add("dve", lambda e: e.memset(tabl[32:33, :], NEG), w=[tabl])
        P.dma("sp", tabl[0:32, :], rel_bias, w=[tabl])
        oht = [A.alloc("oht%d" % k, [512], F32, parts=33) for k in range(2)]
        gst = [A.alloc("gst%d" % k, [512], BF16, parts=8) for k in range(2)]
        pst = [PS.alloc("pst%d" % k, 512, F32, parts=8) for k in range(2)]
        RG = Res("gtab")
        kk = 0
        for (ohn, h0, nh, DL, gd, td) in (("k_ohswa", 0, 4, DL_SWA, g_swa_d, t_swa_d),
                                           ("k_ohnsa", 4, 8, DL_NSA, g_nsa_d, t_nsa_d),
                                           ("k_ohcmp", 4, 8, DL_CMP, g_cmp_d, t_cmp_d)):
            for c0 in range(0, DL, 512):
                n = min(512, DL - c0)
                ot, gs, ps = oht[kk % 2], gst[kk % 2], pst[kk % 2]
                kk += 1
                P.dma("sp", ot[:, 0:n], K[ohn][:, c0:c0 + n], w=[ot])
                mm(ps, ps[0:nh, 0:n], tabl, tabl[:, h0:h0 + nh], ot, ot[:, 0:n], True, True)
                evac(gs, gs[0:nh, 0:n], ps, ps[0:nh, 0:n])
                P.dma("sp", gd[:, c0:c0 + n], gs[0:nh, 0:n], r=[gs], w=[RG])
            import os
            if not os.environ.get("K_SKIP_REP"):
                for h in range(nh):
                    P.dma("sp", td[h], dap(gd, h * DL, [[0, 128], [1, DL]]), r=[RG], w=[RG])
        A.release(m0)
        PS.release(pm0)
        cb = A.alloc("cb", [8, 128], F32)
        m0 = A.mark()
        cl = A.alloc("cl", [8], F32)
        P.dma("sp", cl[:, :], c_in.rearrange("(c p) -> p c", p=128), w=[cl], slow=True)
        P.add("act", lambda e: e.activation(out=cl[:, :], in_=cl[:, :], func=AF.Silu), r=[cl], w=[cl])
        P.add("dve", lambda e: e.tensor_copy(out=cb[:, :, :], in_=cl[:, :].unsqueeze(2).to_broadcast([128, 8, 128])),
              r=[cl], w=[cb])
        A.release(m0)

        import os
        conv_jobs = {}
        if not os.environ.get("K_SKIP_CONV"):
            for l in range(DEPTH):
                conv_jobs[l] = conv_chunks(l)

        persist_mark = A.mark()
        pspersist = PS.mark()

        for l in range(DEPTH):
            P.epoch = l
            xsrc = x_in if l == 0 else out
            mod = A.alloc("mod", [6, D], F32)
            gn = A.alloc("gn", [D], F32)
            mA = A.mark()
            pA = PS.mark()
            ones1 = A.alloc("ones1", [128], F32, parts=1)
            P.add("dve", lambda e: e.memset(ones1[:, :], 1.0), w=[ones1])
            wts = [A.alloc("adaw%d" % k, [8, 512], F32) for k in range(2)]
            bts = [A.alloc("adab%d" % k, [512], F32, parts=1) for k in range(2)]
            pss = [PS.alloc("psA%d" % k, 512) for k in range(2)]
            for ch in range(12):
                wt, bt, ps = wts[ch % 2], bts[ch % 2], pss[ch % 2]
                n0 = ch * 512
                P.dma("sp", wt[:, :, :], ada_w[l, :, n0:n0 + 512].rearrange("(c p) n -> p c n", p=128), w=[wt])
                P.dma("sp", bt[:, :], ada_b[l:l + 1, n0:n0 + 512], w=[bt])
                for c in range(8):
                    mm(ps, ps[:, :], cb, cb[:, c, :], wt, wt[:, c, :], c == 0, False)
                mm(ps, ps[:, :], ones1, ones1[:, :], bt, bt[:, :], False, True)
                evac(mod, mod[:, ch // 2, (ch % 2) * 512:(ch % 2) * 512 + 512], ps, ps[:, :])
            g4 = A.alloc("g4", [4, D], F32)
            for k, gsrc in enumerate((g_apre, g_apost, g_fpre, g_fpost)):
                P.dma("sp", g4[:, k, :], dap(gsrc, l * D, [[0, 128], [1, D]]), w=[g4])
            P.dma("sp", gn[:, :], dap(group_norm, l * D, [[0, 128], [1, D]]), w=[gn])
            for (mi, gi) in ((1, 0), (4, 2)):
                P.add("dve", lambda e, mi=mi, gi=gi: e.scalar_tensor_tensor(
                    out=mod[:, mi, :], in0=mod[:, mi, :], scalar=1.0, in1=g4[:, gi, :], op0=ALU.add, op1=ALU.mult),
                    r=[mod, g4], w=[mod])
            for (mi, gi) in ((2, 1), (5, 3)):
                P.add("dve", lambda e, mi=mi, gi=gi: e.tensor_tensor(
                    out=mod[:, mi, :], in0=mod[:, mi, :], in1=g4[:, gi, :], op=ALU.mult), r=[mod, g4], w=[mod])
            if debug and l == 0:
                P.dma("sp", dbg["mod"], mod[:, :, :].rearrange("p a b -> p (a b)"), r=[mod])
            A.release(mA)
            PS.release(pA)
            if stop_after == "A":
                break

            fg_mark = A.mark()
            fg = A.alloc("fg", [NT, 28], F32)
            mB = A.mark()
            pB = PS.mark()
            wsb = A.alloc("wsb", [8, NIN], BF16)
            dcol = {}
            c0 = 0
            for nm in TORDER + VORDER:
                dcol[nm] = c0
                c0 += [w for (n_, s_, w) in WSPLITS if n_ == nm][0]
            for (nm, src, wd) in WSPLITS:
                for cc in range(0, wd, 128):
                    n = min(128, wd - cc)
                    sft = cvf[cvk[0] % 2]
                    cvk[0] += 1
                    sf = sft[:, :].rearrange("p (a b) -> p a b", a=8)
                    P.dma("sp", sf[:, :, 0:n], w_in[l, :, src + cc:src + cc + n].rearrange("(c p) n -> p c n", p=128), w=[sft])
                    P.add(os.environ.get("K_CAST", "pool"), lambda e, sf=sf, n=n, d0=dcol[nm] + cc: e.tensor_copy(out=wsb[:, :, d0:d0 + n], in_=sf[:, :, 0:n]),
                          r=[sft], w=[wsb])
            xts = [A.alloc("xt%d" % k, [4, D], F32) for k in range(1)]
            hf = A.alloc("hf", [D], F32)
            hb = A.alloc("hb", [4, D], BF16)
            hT = A.alloc("hT", [8, 512], BF16)
            junk = A.alloc("junk", [D], BF16)
            ss = A.alloc("ss", [4], F32)
            rs = A.alloc("rs", [4], F32)
            stg = [A.alloc("stg%d" % k, [512], BF16) for k in range(3)]
            vst = [A.alloc("vst%d" % k, [4, NV], BF16) for k in range(2)]
            ptr = [PS.alloc("ptr%d" % k, 512, BF16) for k in range(2)]
            pmm = [PS.alloc("pmm%d" % k, 512) for k in range(4)]
            pv2 = [PS.alloc("pv2%d" % k, 156) for k in range(2)]
            kq = 0
            for tb in range(NQ):
                t0 = tb * 512
                xt = xts[0]
                P.dma("sp", xt[:, :, :], xsrc[t0:t0 + 512, :].rearrange("(s p) d -> p s d", p=128), r=[RX[tb]], w=[xt])
                if int(os.environ.get("K_B", "9")) < 1:
                    continue
                P.add("dve", lambda e: e.memset(ss[:, :], 0.0), w=[ss])
                for s in range(4):
                    P.add("act", lambda e, s=s, xt=xt: e.activation(out=junk[:, :], in_=xt[:, s, :], func=AF.Square,
                                                                  accum_out=ss[:, s:s + 1]), r=[xt, ss], w=[junk, ss])
                P.add("dve", lambda e: e.tensor_scalar(out=rs[:, :], in0=ss[:, :], scalar1=1.0 / D, scalar2=1e-6,
                                                       op0=ALU.mult, op1=ALU.add), r=[ss], w=[rs])
                P.add("act", lambda e: e.activation(out=rs[:, :], in_=rs[:, :], func=AF.Sqrt), r=[rs], w=[rs])
                P.add("dve", lambda e: e.reciprocal(out=rs[:, :], in_=rs[:, :]), r=[rs], w=[rs])
                for s in range(4):
                    P.add("dve", lambda e, s=s, xt=xt: e.scalar_tensor_tensor(
                        out=hf[:, :], in0=xt[:, s, :], scalar=rs[:, s:s + 1], in1=mod[:, 1, :],
                        op0=ALU.mult, op1=ALU.mult), r=[xt, rs, mod], w=[hf])
                    P.add("dve", lambda e, s=s: e.tensor_tensor(out=hb[:, s, :], in0=hf[:, :], in1=mod[:, 0, :],
                                                               op=ALU.add), r=[hf, mod], w=[hb])
                KB = int(os.environ.get("K_B", "9"))
                if KB < 2:
                    continue
                for c in range(8):
                    pt = ptr[c % 2]
                    for s in range(4):
                        tr(pt, pt[:, s * 128:(s + 1) * 128], hb, hb[:, s, c * 128:(c + 1) * 128], identb, identb[:, :])
                    evac(hT, hT[:, c, :], pt, pt[:, :])
                if KB < 3:
                    continue
                for m in range(NFT // 128):
                    ps = pmm[kq % 4]
                    sg = stg[kq % 3]
                    kq += 1
                    for c in range(8):
                        mm(ps, ps[:, :], wsb, wsb[:, c, m * 128:(m + 1) * 128], hT, hT[:, c, :], c == 0, c == 7)
                    evac(sg, sg[:, :], ps, ps[:, :], scale=(0.125 if m < 8 else None))
                    P.dma("act", qT_d[m * 128:(m + 1) * 128, t0:t0 + 512], sg[:, :], r=[sg], w=[RQT])
                if KB < 4:
                    continue
                vs_ = vst[tb % 2]
                for s in range(4):
                    ps = pmm[kq % 4]
                    kq += 1
                    p2 = pv2[s % 2]
                    for c in range(8):
                        mm(ps, ps[:, :], hT, hT[:, c, s * 128:(s + 1) * 128], wsb, wsb[:, c, NFT:NFT + 512], c == 0, c == 7)
                    for c in range(8):
                        mm(p2, p2[:, :], hT, hT[:, c, s * 128:(s + 1) * 128], wsb, wsb[:, c, NFT + 512:NFT + 668],
                           c == 0, c == 7)
                    if KB < 5:
                        continue
                    evac(vs_, vs_[:, s, 0:512], ps, ps[:, :])
                    evac(vs_, vs_[:, s, 512:640], p2, p2[:, 0:128], eng="dve")
                    if KB < 6:
                        continue
                    evac(fg, fg[:, tb * 4 + s, :], p2, p2[:, 128:156], eng="dve")
                if KB < 7:
                    continue
                P.dma("act", v_d[t0:t0 + 512, :].rearrange("(s p) f -> p s f", p=128), vs_[:, :, :], r=[vs_], w=[RV])
            if debug and l == 0 and KB >= 8:
                P.dma("sp", dbg["fg"], fg[:, :, :].rearrange("p a b -> p (a b)"), r=[fg])
                P.dma("sp", dbg["qT"], qT_d, r=[RQT])
                P.dma("sp", dbg["v"], v_d, r=[RV])
            A.release(mB)
            PS.release(pB)
            if stop_after == "B":
                break
            build_attention(locals())
            if debug and l == 0:
                if stop_after == "FS":
                    P.dma("sp", dbg["o"][:, 0:512], o_d[:, 0:512], r=[RO])
                else:
                    P.dma("sp", dbg["o"], o_d, r=[RO])
            if stop_after in ("C", "FS"):
                break
            A.release(fg_mark)
            build_ffn(locals())
            A.release(persist_mark)
            PS.release(pspersist)

        nw = P.emit(stack, max(DEPTH, 1))
        print("ops", len(P.ops), "waits", nw)
    return nc


def make_in_map(inputs, b, S, consts=None):
    m = {}
    for k, v in inputs.items():
        v = np.asarray(v)
        if k == "x" or k == "c":
            m[k] = np.ascontiguousarray(v[b], dtype=np.float32)
        else:
            m[k] = np.ascontiguousarray(v, dtype=np.float32)
    m.update(consts if consts is not None else host_consts(S))
    return m


def kernel(**inputs):
    x = np.asarray(inputs["x"])
    B, S, _ = x.shape
    DEPTH = int(np.asarray(inputs["ada_w"]).shape[0])
    nc = build(S, DEPTH)
    consts = host_consts(S)
    shared = {k: np.ascontiguousarray(np.asarray(v), dtype=np.float32) for k, v in inputs.items() if k not in ("x", "c")}
    in_maps = []
    for b in range(B):
        m = dict(shared)
        m["x"] = np.ascontiguousarray(x[b], dtype=np.float32)
        m["c"] = np.ascontiguousarray(np.asarray(inputs["c"])[b], dtype=np.float32)
        m.update(consts)
        in_maps.append(m)
    res = run_bass_kernel_spmd(nc, in_maps, core_ids=list(range(B)))
    return np.stack([np.asarray(res.results[b]["out"], dtype=np.float32) for b in range(B)], 0)


def pipeline(n, fa, fb, fc, la=2):
    for k in range(n + la):
        if k < n:
            fa(k)
        if k - la >= 0:
            fb(k - la)
            fc(k - la)


def build_attention(Ld):
    from types import SimpleNamespace
    L = SimpleNamespace(**Ld)
    P, A, PS, K = L.P, L.A, L.PS, L.K
    S, NT, NQ, NCMP, NNT, nsz, l = L.S, L.NT, L.NQ, L.NCMP, L.NNT, L.nsz, L.l
    fg, identb, identf, tri, onesf = L.fg, L.identb, L.identf, L.tri, L.onesf
    cmt, wmt, kmt, ovt = L.cmt, L.wmt, L.kmt, L.ovt
    qT_d, v_d, o_d, RQT, RV, RO, RG = L.qT_d, L.v_d, L.o_d, L.RQT, L.RV, L.RO, L.RG
    mm, tr, evac = L.mm, L.tr, L.evac
    t_swa_d, t_nsa_d = L.t_swa_d, L.t_nsa_d
    m_att = A.mark()
    mswa = A.alloc("mswa", [4, 256], BF16)
    mnsa = A.alloc("mnsa", [8, 1664], BF16)
    P.dma("sp", mswa[:, :, :], dap(t_swa_d, 127, [[DL_SWA - 1, 128], [128 * DL_SWA, 4], [1, 256]]), r=[RG], w=[mswa])
    for h in range(8):
        P.dma("sp", mnsa[:, h, :], dap(t_nsa_d, h * 128 * DL_NSA + 127, [[DL_NSA - 1, 128], [1, 1664]]),
              r=[RG], w=[mnsa])
    b31 = A.alloc("b31", [8], F32)
    P.add("dve", lambda e: e.tensor_copy(out=b31[:, :], in_=mnsa[:, :, 1663]), r=[mnsa], w=[b31])
    L.mswa, L.mnsa, L.b31 = mswa, mnsa, b31

    sg = A.alloc("sg", [NT, 24], F32)
    P.add("act", lambda e: e.activation(out=sg[:, :, :], in_=fg[:, :, 4:28], func=AF.Sigmoid), r=[fg], w=[sg])

    mF = A.mark()
    pF = PS.mark()
    m1 = A.mark()
    fbt = A.alloc("fbt", [4], F32)
    P.dma("sp", fbt[:, :], dap(L.forget_bias, l * 4, [[0, 128], [1, 4]]), w=[fbt])
    z = A.alloc("z", [NT, 4], F32)
    P.add("dve", lambda e: e.tensor_tensor(out=z[:, :, :], in0=fg[:, :, 0:4],
                                           in1=fbt[:, :].unsqueeze(1).to_broadcast([128, NT, 4]), op=ALU.add),
          r=[fg, fbt], w=[z])
    P.add("act", lambda e: e.activation(out=z[:, :, :], in_=z[:, :, :], func=AF.Exp, scale=-1.0), r=[z], w=[z])
    P.add("act", lambda e: e.activation(out=z[:, :, :], in_=z[:, :, :], func=AF.Ln, bias=1.0), r=[z], w=[z])
    psc = PS.alloc("psc", NT * 4)
    pst = PS.alloc("pst", NT * 4)
    zf = z[:, :, :].rearrange("p a b -> p (a b)")
    mm(psc, psc[:, :], tri, tri[:, :], z, zf, True, True)
    mm(pst, pst[:, :], onesf, onesf[:, :], z, zf, True, True)
    tot = A.alloc("tot", [NT, 4], F32)
    evac(tot, tot[:, :, :].rearrange("p a b -> p (a b)"), pst, pst[:, :])
    pre = A.alloc("pre", [NT, 4], F32)
    P.add("dve", lambda e: e.memset(pre[:, 0, :], 0.0), w=[pre])
    for k in range(1, NT):
        P.add("dve", lambda e, k=k: e.tensor_tensor(out=pre[:, k, :], in0=pre[:, k - 1, :], in1=tot[:, k - 1, :],
                                                    op=ALU.add), r=[pre, tot], w=[pre])
    Nn = A.alloc("Nn", [NT, 4], F32)
    P.add("dve", lambda e: e.tensor_tensor(out=Nn[:, :, :], in0=psc[:, :].rearrange("p (a b) -> p a b", b=4),
                                           in1=pre[:, :, :], op=ALU.add), r=[psc, pre], w=[Nn])
    R = A.alloc("R", [NT, 4, 6], BF16)
    r1 = A.alloc("r1", [NT, 4], F32)
    P.add("dve", lambda e: e.tensor_copy(out=R[:, :, :, 3], in_=Nn[:, :, :]), r=[Nn], w=[R])
    P.add("dve", lambda e: e.tensor_tensor(out=r1[:, :, :], in0=Nn[:, :, :], in1=R[:, :, :, 3], op=ALU.subtract),
          r=[Nn, R], w=[r1])
    P.add("dve", lambda e: e.tensor_copy(out=R[:, :, :, 4], in_=r1[:, :, :]), r=[r1], w=[R])
    P.add("dve", lambda e: e.tensor_tensor(out=r1[:, :, :], in0=r1[:, :, :], in1=R[:, :, :, 4], op=ALU.subtract),
          r=[r1, R], w=[r1])
    P.add("dve", lambda e: e.tensor_copy(out=R[:, :, :, 5], in_=r1[:, :, :]), r=[r1], w=[R])
    for r_ in range(3):
        P.add("dve", lambda e, r_=r_: e.tensor_scalar(out=R[:, :, :, r_], in0=R[:, :, :, 3 + r_], scalar1=-1.0,
                                                      scalar2=None, op0=ALU.mult), r=[R], w=[R])
    rowsT = A.alloc("rowsT", [S], BF16, parts=24)
    psR = [PS.alloc("psR%d" % k, 512, BF16, parts=24) for k in range(2)]
    for k0 in range(0, NT, 4):
        pr = psR[(k0 // 4) % 2]
        for k in range(k0, k0 + 4):
            tr(pr, pr[:, (k - k0) * 128:(k - k0 + 1) * 128], R, R[:, k, :, :].rearrange("p a b -> p (a b)"),
               identb, identb[:, :])
        evac(rowsT, rowsT[:, k0 * 128:(k0 + 4) * 128], pr, pr[:, :])
    PS.release(pF)
    qa = [A.alloc("qa%d" % k, [S], BF16) for k in range(2)]
    ka = [A.alloc("ka%d" % k, [S], BF16) for k in range(2)]
    vf = A.alloc("vf", [NT, 4, 65], BF16)
    for h in range(4):
        P.dma("sp", vf[:, :, h, 0:64], v_d[:, V_FOX + 64 * h:V_FOX + 64 * h + 64].rearrange("(k p) d -> p k d", p=128),
              r=[RV], w=[vf])
    P.add("dve", lambda e: e.memset(vf[:, :, :, 64:65], 1.0), w=[vf])
    rz = A.alloc("rz", [4], F32)
    ofs = [A.alloc("of%d" % k, [4, 64], F32) for k in range(2)]
    pts = [A.alloc("pt%d" % k, [512], BF16) for k in range(4)]
    pss = [PS.alloc("ps%d" % k, 512) for k in range(4)]
    accs = [PS.alloc("acc%d" % k, 512, parts=65) for k in range(2)]
    accN = PS.alloc("accN", 512)
    acnv = accN[:, :].rearrange("p (a b) -> p a b", a=4)
    oTs = A.alloc("oTs", [512], F32, parts=65)
    kcnt = [0]
    bg = list(L.conv_jobs.get(l, []))
    bgper = (len(bg) + 4 * NQ - 1) // (4 * NQ)

    def load_head(h):
        qaug, kaug = qa[h % 2], ka[h % 2]
        P.dma("sp", qaug[0:64, :], qT_d[QT_FOXQ + 64 * h:QT_FOXQ + 64 * h + 64, :], r=[RQT], w=[qaug])
        P.dma("sp", qaug[64:67, :], rowsT[6 * h:6 * h + 3, :], r=[rowsT], w=[qaug])
        P.dma("sp", qaug[67:70, :], K["k_ones"][0:3, :], w=[qaug])
        P.dma("sp", kaug[0:64, :], qT_d[QT_FOXK + 64 * h:QT_FOXK + 64 * h + 64, :], r=[RQT], w=[kaug])
        P.dma("sp", kaug[64:67, :], K["k_ones"][0:3, :], w=[kaug])
        P.dma("sp", kaug[67:70, :], rowsT[6 * h + 3:6 * h + 6, :], r=[rowsT], w=[kaug])

    load_head(0)
    for h in range(4):
        qaug, kaug = qa[h % 2], ka[h % 2]
        if h + 1 < 4:
            load_head(h + 1)
        units = [(i, j) for i in range(NQ) for j in range(4 * i + 4)]
        base = kcnt[0]
        kcnt[0] += len(units)

        def fa(k, units=units, base=base, qaug=qaug, kaug=kaug):
            i, j = units[k]
            bb0 = max(0, j - 4 * i)
            ps = pss[(base + k) % 4]
            diag = j >= 4 * i
            mm(ps, ps[:, bb0 * 128:512], kaug, kaug[0:70, j * 128:(j + 1) * 128],
               qaug, qaug[0:70, i * 512 + bb0 * 128:(i + 1) * 512], True, not diag)
            if diag:
                mm(ps, ps[:, bb0 * 128:512], identb, identb[:, :], cmt, cmt[:, 0:(4 - bb0) * 128], False, True)

        def fb(k, units=units, base=base):
            i, j = units[k]
            bb0 = max(0, j - 4 * i)
            ps = pss[(base + k) % 4]
            pt = pts[(base + k) % 4]
            P.add("act", lambda e: e.activation(out=pt[:, bb0 * 128:512], in_=ps[:, bb0 * 128:512], func=AF.Exp),
                  r=[ps], w=[pt])

        def fc(k, units=units, base=base, h=h):
            i, j = units[k]
            bb0 = max(0, j - 4 * i)
            pt = pts[(base + k) % 4]
            acc = accs[i % 2]
            mm(acc, acc[0:65, bb0 * 128:512], vf, vf[:, j, h, :], pt, pt[:, bb0 * 128:512], j == 0, j == 4 * i + 3)
            if j == 4 * i + 3:
                of = ofs[i % 2]
                for _ in range(bgper):
                    if bg:
                        bg.pop(0)()
                evac(oTs, oTs[0:65, :], acc, acc[0:65, :], eng="dve")
                for bb in range(4):
                    tr(accN, acnv[:, bb, 0:65], oTs, oTs[0:65, bb * 128:(bb + 1) * 128], identf, identf[0:65, 0:65])
                P.add("dve", lambda e: e.reciprocal(out=rz[:, :], in_=acnv[:, :, 64]), r=[accN], w=[rz])
                for bb in range(4):
                    P.add("dve", lambda e, bb=bb: e.tensor_scalar(out=of[:, bb, :], in0=acnv[:, bb, 0:64],
                                                                  scalar1=rz[:, bb:bb + 1], scalar2=None, op0=ALU.mult),
                          r=[accN, rz], w=[of])
                P.dma("sp", o_d[i * 512:(i + 1) * 512, 256 + 64 * h:256 + 64 * h + 64].rearrange("(s p) d -> p s d", p=128),
                      of[:, :, :], r=[of], w=[RO])

        pipeline(len(units), fa, fb, fc, la=3)
    while bg:
        bg.pop(0)()
    while L.pend:
        L.pend.pop(0)()
    A.release(mF)
    PS.release(pF)

    mS = A.mark()
    pS = PS.mark()
    qs = A.alloc("qs", [2, S], BF16)
    P.dma("sp", qs[0:64, :, :], qT_d[QT_SWAQ:QT_SWAQ + 128, :].rearrange("(g d) t -> d g t", g=2), r=[RQT], w=[qs])
    P.dma("sp", qs[64:128, :, :], qT_d[QT_SWAQ + 128:QT_SWAQ + 256, :].rearrange("(g d) t -> d g t", g=2), r=[RQT], w=[qs])
    ksw = A.alloc("ksw", [S], BF16)
    P.dma("sp", ksw[:, :], qT_d[QT_SWAK:QT_SWAK + 128, :], r=[RQT], w=[ksw])
    vsw = A.alloc("vsw", [NT, 2, 65], BF16)
    for kv in range(2):
        P.dma("sp", vsw[:, :, kv, 0:64], v_d[:, V_SWA + 64 * kv:V_SWA + 64 * kv + 64].rearrange("(k p) d -> p k d", p=128),
              r=[RV], w=[vsw])
    P.add("dve", lambda e: e.memset(vsw[:, :, :, 64:65], 1.0), w=[vsw])
    es = A.alloc("es", [4], F32)
    P.dma("sp", es[:, :], dap(L.swa_sinks, l * 4, [[0, 128], [1, 4]]), w=[es])
    P.add("act", lambda e: e.activation(out=es[:, :], in_=es[:, :], func=AF.Exp), r=[es], w=[es])
    pts = [A.alloc("spt%d" % k, [256], BF16) for k in range(4)]
    pss = [PS.alloc("sps%d" % k, 256) for k in range(4)]
    accs = [PS.alloc("sacc%d" % k, 65) for k in range(4)]
    osw = [A.alloc("osw%d" % k, [4, 64], F32) for k in range(2)]
    zz = A.alloc("zz", [4], F32)
    units = [(ti, h) for ti in range(NT) for h in range(4)]

    def fa(k):
        ti, h = units[k]
        kv, g = h // 2, h % 2
        ps = pss[k % 4]
        rows = slice(kv * 64, kv * 64 + 64)
        if ti > 0:
            mm(ps, ps[:, 0:128], ksw, ksw[rows, (ti - 1) * 128:ti * 128], qs, qs[rows, g, ti * 128:(ti + 1) * 128], True, False)
            mm(ps, ps[:, 0:128], identb, identb[:, :], mswa, mswa[:, h, 128:256], False, True)
        mm(ps, ps[:, 128:256], ksw, ksw[rows, ti * 128:(ti + 1) * 128], qs, qs[rows, g, ti * 128:(ti + 1) * 128], True, False)
        mm(ps, ps[:, 128:256], identb, identb[:, :], mswa, mswa[:, h, 0:128], False, True)

    def fb(k):
        ti, h = units[k]
        ps, pt = pss[k % 4], pts[k % 4]
        c0 = 0 if ti > 0 else 128
        P.add("act", lambda e: e.activation(out=pt[:, c0:256], in_=ps[:, c0:256], func=AF.Exp), r=[ps], w=[pt])

    def fc(k):
        ti, h = units[k]
        kv = h // 2
        pt, acc = pts[k % 4], accs[k % 4]
        if ti > 0:
            mm(acc, acc[:, 0:65], pt, pt[:, 0:128], vsw, vsw[:, ti - 1, kv, :], True, False)
        mm(acc, acc[:, 0:65], pt, pt[:, 128:256], vsw, vsw[:, ti, kv, :], ti == 0, True)
        ow = osw[ti % 2]
        P.add("dve", lambda e: e.tensor_tensor(out=zz[:, h:h + 1], in0=acc[:, 64:65], in1=es[:, h:h + 1], op=ALU.add),
              r=[acc, es], w=[zz])
        P.add("dve", lambda e: e.reciprocal(out=zz[:, h:h + 1], in_=zz[:, h:h + 1]), r=[zz], w=[zz])
        P.add("dve", lambda e: e.tensor_scalar(out=ow[:, h, :], in0=acc[:, 0:64], scalar1=zz[:, h:h + 1], scalar2=None,
                                               op0=ALU.mult), r=[acc, zz], w=[ow])
        if h == 3:
            P.dma("sp", o_d[ti * 128:(ti + 1) * 128, 0:256], ow[:, :, :].rearrange("p a b -> p (a b)"), r=[ow], w=[RO])

    pipeline(len(units), fa, fb, fc, la=3)
    A.release(mS)
    PS.release(pS)
    build_nsa(L, sg)
    A.release(m_att)


def build_nsa(L, sg):
    P, A, PS, K = L.P, L.A, L.PS, L.K
    S, NT, NQ, NCMP, NNT, nsz, l = L.S, L.NT, L.NQ, L.NCMP, L.NNT, L.nsz, L.l
    identb, identf = L.identb, L.identf
    wmt, kmt, ovt, mnsa, b31 = L.wmt, L.kmt, L.ovt, L.mnsa, L.b31
    qT_d, v_d, o_d, RQT, RV, RO, RG = L.qT_d, L.v_d, L.o_d, L.RQT, L.RV, L.RO, L.RG
    mm, tr, evac = L.mm, L.tr, L.evac
    mN = A.mark()
    pN = PS.mark()
    kcmpT = A.alloc("kcmpT", [2, 256], BF16, parts=64)
    Rg = A.alloc("Rg", [NNT, 2, 128], F32)
    for nt in range(NNT):
        for g in range(2):
            P.dma("sp", Rg[0:nsz[nt], nt, g, 64:128], K["k_ovl"][nt * 128:nt * 128 + nsz[nt], :], w=[Rg])
    m0 = A.mark()
    p0 = PS.mark()
    xTs = [A.alloc("cxT%d" % k, [S], BF16, parts=64) for k in range(2)]
    w1s = [A.alloc("cw1%d" % k, [32, 128], BF16, parts=64) for k in range(2)]
    pes = [A.alloc("cpe%d" % k, [32], BF16, parts=64) for k in range(2)]
    w2s = [A.alloc("cw2%d" % k, [64], BF16) for k in range(2)]
    w1f = A.alloc("cw1f", [32, 128], F32, parts=64)
    pef = A.alloc("cpef", [32], F32, parts=64)
    w2f = A.alloc("cw2f", [64], F32)
    bsb = [A.alloc("cbias%d" % k, [1], F32) for k in range(2)]
    Gts = [A.alloc("cG%d" % k, [256], BF16) for k in range(2)]
    psG = [PS.alloc("psG%d" % k, 256) for k in range(2)]
    psb = [PS.alloc("psb%d" % k, 1) for k in range(2)]
    psK = PS.alloc("psK", 256, parts=64)
    psV = [PS.alloc("psV%d" % k, 64) for k in range(2)]
    kk = 0
    for which in range(2):
        w1, pe, w2 = w1s[which], pes[which], w2s[which]
        P.dma("sp", w1f[:, :, :], L.cmp_w1[l, which].rearrange("(l d) j -> d l j", d=64), w=[w1f])
        P.add("pool", lambda e, w1=w1: e.tensor_copy(out=w1[:, :, :], in_=w1f[:, :, :]), r=[w1f], w=[w1])
        P.dma("sp", pef[:, :], L.cmp_pos[l, which].rearrange("l d -> d l"), w=[pef], slow=True)
        P.add("pool", lambda e, pe=pe: e.tensor_copy(out=pe[:, :], in_=pef[:, :]), r=[pef], w=[pe])
        P.dma("sp", w2f[:, :], L.cmp_w2[l, which], w=[w2f])
        P.add("pool", lambda e, w2=w2: e.tensor_copy(out=w2[:, :], in_=w2f[:, :]), r=[w2f], w=[w2])
        pb_, bs = psb[which], bsb[which]
        for ll in range(32):
            mm(pb_, pb_[:, 0:1], w1, w1[:, ll, :], pe, pe[:, ll:ll + 1], ll == 0, ll == 31)
        evac(bs, bs[:, :], pb_, pb_[:, 0:1])
        for g in range(2):
            xT, pG, Gt = xTs[kk % 2], psG[kk % 2], Gts[kk % 2]
            kk += 1
            row = (QT_KC if which == 0 else QT_VC) + 64 * g
            P.dma("sp", xT[:, :], qT_d[row:row + 64, :], r=[RQT], w=[xT])
            for ll in range(32):
                mm(pG, pG[:, 0:NCMP], w1, w1[:, ll, :], xT, xT[:, ll:ll + 16 * (NCMP - 1) + 1:16], ll == 0, ll == 31)
            P.add("act", lambda e, Gt=Gt, pG=pG, bs=bs: e.activation(out=Gt[:, 0:NCMP], in_=pG[:, 0:NCMP],
                                                                     func=AF.Gelu_apprx_tanh, bias=bs[:, 0:1]),
                  r=[pG, bs], w=[Gt])
            if which == 0:
                mm(psK, psK[:, 0:NCMP], w2, w2[:, :], Gt, Gt[:, 0:NCMP], True, True)
                evac(kcmpT, kcmpT[:, g, 0:NCMP], psK, psK[:, 0:NCMP])
            else:
                for nt in range(NNT):
                    pV = psV[nt % 2]
                    mm(pV, pV[0:nsz[nt], 0:64], Gt, Gt[:, nt * 128:nt * 128 + nsz[nt]], w2, w2[:, :], True, True)
                    evac(Rg, Rg[0:nsz[nt], nt, g, 0:64], pV, pV[0:nsz[nt], 0:64])
    A.release(m0)
    PS.release(p0)

    kaug = [A.alloc("kaug%d" % g, [S], BF16) for g in range(2)]
    kwT = A.alloc("kwT", [2, S], BF16, parts=64)
    vsl = A.alloc("vsl", [NT, 2, 65], BF16)
    vwn = A.alloc("vwn", [NT, 2, 65], BF16)
    for g in range(2):
        P.dma("sp", kaug[g][0:64, :], qT_d[QT_KS + 64 * g:QT_KS + 64 * g + 64, :], r=[RQT], w=[kaug[g]])
        P.dma("sp", kaug[g][64:128, :], K["k_sel"], w=[kaug[g]])
        P.dma("sp", kwT[:, g, :], qT_d[QT_KW + 64 * g:QT_KW + 64 * g + 64, :], r=[RQT], w=[kwT])
        P.dma("sp", vsl[:, :, g, 0:64], v_d[:, V_S + 64 * g:V_S + 64 * g + 64].rearrange("(k p) d -> p k d", p=128),
              r=[RV], w=[vsl])
        P.dma("sp", vwn[:, :, g, 0:64], v_d[:, V_W + 64 * g:V_W + 64 * g + 64].rearrange("(k p) d -> p k d", p=128),
              r=[RV], w=[vwn])
    P.add("dve", lambda e: e.memset(vsl[:, :, :, 64:65], 1.0), w=[vsl])
    P.add("dve", lambda e: e.memset(vwn[:, :, :, 64:65], 1.0), w=[vwn])
    qaugs = [[A.alloc("qaug%d_%d" % (k, h), [512], BF16) for h in range(8)] for k in range(2)]
    Tcs = [A.alloc("Tc%d" % nt, [8, 512], BF16) for nt in range(NNT)]
    pcs = [A.alloc("pc%d" % k, [512], F32) for k in range(3)]
    pts = [A.alloc("npt%d" % k, [512], BF16) for k in range(3)]
    imp = A.alloc("imp", [4, 64], F32)
    wk = A.alloc("wk", [64], F32)
    m8 = A.alloc("m8", [16], F32)
    nm = A.alloc("nm", [4, 128], BF16)
    zc = A.alloc("zc", [4], F32)
    coef = A.alloc("coef", [4], F32)
    onsa = [A.alloc("onsa%d" % k, [4, 512], F32) for k in range(2)]
    pss = [PS.alloc("nps%d" % k, 512) for k in range(3)]
    acccs = [PS.alloc("accc%d" % k, 512) for k in range(2)]
    accs = [PS.alloc("nacc%d" % k, 512, parts=65) for k in range(2)]
    pxx = PS.alloc("pxx", 512)
    psM = Tile("psMv", pxx[:, :].bitcast(BF16)[:, 0:512])
    acnv = pxx[:, :].rearrange("p (a b) -> p a b", a=4)
    oTs = A.alloc("noTs", [512], F32, parts=65)
    kc = [0]
    ac = [0]
    for i in range(NQ):
        qg = qaugs[i % 2]
        on = onsa[i % 2]
        for h in range(8):
            P.dma("sp", qg[h][0:64, :], qT_d[QT_NSAQ + 64 * h:QT_NSAQ + 64 * h + 64, i * 512:(i + 1) * 512], r=[RQT], w=[qg[h]])
        nts = [nt for nt in range(NNT) if 512 * (i + 1) - 1 >= 16 * 128 * nt + 31]
        for nt in nts:
            u0 = 512 * i - 2048 * nt
            assert u0 >= 0
            P.dma("sp", Tcs[nt][:, :, :], dap(L.t_cmp_d, u0 + 2032, [[DL_CMP - 16, 128], [128 * DL_CMP, 8], [1, 512]]),
                  r=[RG], w=[Tcs[nt]])
        for g in range(2):
            units = [(hh, nt) for hh in range(4) for nt in nts]
            base = kc[0]
            kc[0] += len(units)

            def fa(k, units=units, base=base, g=g, qg=qg):
                hh, nt = units[k]
                h = 4 * g + hh
                ps = pss[(base + k) % 3]
                n = nsz[nt]
                mm(ps, ps[0:n, :], kcmpT, kcmpT[0:64, g, nt * 128:nt * 128 + n], qg[h], qg[h][0:64, :], True, False)
                mm(ps, ps[0:n, :], identb, identb[0:n, 0:n], Tcs[nt], Tcs[nt][0:n, h, :], False, True)

            def fb(k, units=units, base=base):
                hh, nt = units[k]
                ps, pc = pss[(base + k) % 3], pcs[(base + k) % 3]
                n = nsz[nt]
                P.add("act", lambda e: e.activation(out=pc[0:n, :], in_=ps[0:n, :], func=AF.Exp), r=[ps], w=[pc])

            def fc(k, units=units, base=base, g=g, i=i, on=on):
                hh, nt = units[k]
                h = 4 * g + hh
                pc = pcs[(base + k) % 3]
                n = nsz[nt]
                accc = acccs[hh % 2]
                avc = accc[:, :].rearrange("p (a b) -> p a b", a=4)
                for bb in range(4):
                    mm(accc, avc[:, bb, :], pc, pc[0:n, bb * 128:(bb + 1) * 128], Rg, Rg[0:n, nt, g, :],
                       nt == nts[0] and bb == 0, nt == nts[-1] and bb == 3)
                if nt == nts[-1]:
                    P.add("dve", lambda e: e.reduce_sum(out=zc[:, :], in_=avc[:, :, 64:128], axis=mybir.AxisListType.X),
                          r=[accc], w=[zc])
                    P.add("dve", lambda e: e.tensor_scalar(out=zc[:, :], in0=zc[:, :], scalar1=1e-30, scalar2=None,
                                                           op0=ALU.max), r=[zc], w=[zc])
                    P.add("dve", lambda e: e.reciprocal(out=zc[:, :], in_=zc[:, :]), r=[zc], w=[zc])
                    P.add("dve", lambda e: e.tensor_tensor(out=coef[:, :], in0=zc[:, :], in1=sg[:, 4 * i:4 * i + 4, 3 * h],
                                                           op=ALU.mult), r=[zc, sg], w=[coef])
                    for bb in range(4):
                        P.add("dve", lambda e, bb=bb: e.tensor_scalar(
                            out=on[:, bb, 64 * h:64 * h + 64], in0=avc[:, bb, 0:64], scalar1=coef[:, bb:bb + 1],
                            scalar2=None, op0=ALU.mult), r=[accc, coef], w=[on])
                        if hh == 0:
                            P.add("dve", lambda e, bb=bb: e.tensor_scalar(
                                out=imp[:, bb, :], in0=avc[:, bb, 64:128], scalar1=zc[:, bb:bb + 1], scalar2=None,
                                op0=ALU.mult), r=[accc, zc], w=[imp])
                        else:
                            P.add("dve", lambda e, bb=bb: e.scalar_tensor_tensor(
                                out=imp[:, bb, :], in0=avc[:, bb, 64:128], scalar=zc[:, bb:bb + 1], in1=imp[:, bb, :],
                                op0=ALU.mult, op1=ALU.add), r=[accc, zc, imp], w=[imp])

            pipeline(len(units), fa, fb, fc, la=2)
            for bb in range(4):
                ti = 4 * i + bb
                w0 = 63 - 2 * ti
                P.add("dve", lambda e, bb=bb, w0=w0: e.tensor_tensor(out=imp[:, bb, :], in0=imp[:, bb, :],
                                                                   in1=kmt[:, w0:w0 + 64], op=ALU.mult), r=[imp, kmt], w=[imp])
                P.add("dve", lambda e, bb=bb, w0=w0: e.tensor_tensor(out=imp[:, bb, :], in0=imp[:, bb, :],
                                                                   in1=ovt[:, w0:w0 + 64], op=ALU.add), r=[imp, ovt], w=[imp])
                P.add("dve", lambda e, bb=bb: e.memset(imp[:, bb, 0:1], 1e30), r=[imp], w=[imp])
                P.add("dve", lambda e, bb=bb: e.max(out=m8[:, 0:8], in_=imp[:, bb, :]), r=[imp], w=[m8])
                P.add("dve", lambda e, bb=bb: e.match_replace(out=wk[:, :], in_to_replace=m8[:, 0:8], in_values=imp[:, bb, :],
                                                              imm_value=-3e38), r=[imp, m8], w=[wk])
                P.add("dve", lambda e: e.max(out=m8[:, 8:16], in_=wk[:, :]), r=[wk, m8], w=[m8])
                P.add("dve", lambda e, bb=bb: e.tensor_scalar(out=nm[:, bb, 0:64], in0=imp[:, bb, :], scalar1=m8[:, 15:16],
                                                              scalar2=NEG, op0=ALU.is_lt, op1=ALU.mult), r=[imp, m8], w=[nm])
                P.add("dve", lambda e, bb=bb: e.tensor_copy(out=nm[:, bb, 64:128], in_=nm[:, bb, 0:64]), r=[nm], w=[nm])

            def mask_rows(g=g, qg=qg):
                for bb in range(4):
                    tr(pxx, psM[:, bb * 128:(bb + 1) * 128], nm, nm[:, bb, :], identb, identb[:, :])
                for hh in range(4):
                    qh = qg[4 * g + hh]
                    evac(qh, qh[64:128, :], pxx, psM[64:128, :], eng="dve")

            for br in (1, 0):
                if br == 0:
                    mask_rows()
                if br == 0:
                    js = list(range(4 * i + 4))
                else:
                    js = list(range(max(0, 4 * i - 4), 4 * i + 4))
                units = [(hh, j) for hh in range(4) for j in js]
                base = kc[0]
                kc[0] += len(units)
                abase = ac[0]
                ac[0] += 4
                jfirst = js[0]

                def rng(j, br=br, i=i):
                    if br == 0:
                        return max(0, j - 4 * i), 4
                    if j == max(0, 4 * i - 4):
                        return 0, 4
                    bbs = [bb for bb in range(4) if 0 <= 4 * i + bb - j <= 4]
                    return bbs[0], bbs[-1] + 1

                def fa(k, units=units, base=base, g=g, qg=qg, br=br, i=i, rng=rng):
                    hh, j = units[k]
                    h = 4 * g + hh
                    qh = qg[h]
                    bb0, bb1 = rng(j)
                    ps = pss[(base + k) % 3]
                    ucol = 128 * (4 * i + bb0 - j)
                    wdt = (bb1 - bb0) * 128
                    if br == 0:
                        near = (4 * i - j) <= 9
                        mm(ps, ps[:, bb0 * 128:bb1 * 128], kaug[g], kaug[g][:, j * 128:(j + 1) * 128],
                           qh, qh[:, bb0 * 128:bb1 * 128], True, not near)
                        if near:
                            mm(ps, ps[:, bb0 * 128:bb1 * 128], identb, identb[:, :], mnsa, mnsa[:, h, ucol:ucol + wdt],
                               False, True)
                    else:
                        needw = ucol + wdt > 512
                        mm(ps, ps[:, bb0 * 128:bb1 * 128], kwT, kwT[0:64, g, j * 128:(j + 1) * 128],
                           qh, qh[0:64, bb0 * 128:bb1 * 128], True, False)
                        mm(ps, ps[:, bb0 * 128:bb1 * 128], identb, identb[:, :], mnsa, mnsa[:, h, ucol:ucol + wdt],
                           False, not needw)
                        if needw:
                            if j == max(0, 4 * i - 4):
                                mm(ps, ps[:, bb0 * 128:bb1 * 128], identb, identb[:, :], wmt, wmt[:, ucol:ucol + wdt],
                                   False, True)
                            else:
                                bw = j - 4 * i + 4
                                mm(ps, ps[:, bw * 128:(bw + 1) * 128], identb, identb[:, :], wmt, wmt[:, 512:640], False, True)

                def fb(k, units=units, base=base, g=g, br=br, i=i, rng=rng):
                    hh, j = units[k]
                    h = 4 * g + hh
                    bb0, bb1 = rng(j)
                    ps, pt = pss[(base + k) % 3], pts[(base + k) % 3]
                    if br == 0 and (4 * i - j) > 9:
                        P.add("act", lambda e: e.activation(out=pt[:, bb0 * 128:bb1 * 128], in_=ps[:, bb0 * 128:bb1 * 128],
                                                            func=AF.Exp, bias=b31[:, h:h + 1]), r=[ps, b31], w=[pt])
                    else:
                        P.add("act", lambda e: e.activation(out=pt[:, bb0 * 128:bb1 * 128], in_=ps[:, bb0 * 128:bb1 * 128],
                                                            func=AF.Exp), r=[ps], w=[pt])

                def fc(k, units=units, base=base, abase=abase, g=g, br=br, i=i, rng=rng, jfirst=jfirst, on=on):
                    hh, j = units[k]
                    h = 4 * g + hh
                    bb0, bb1 = rng(j)
                    pt = pts[(base + k) % 3]
                    acc = accs[(abase + hh) % 2]
                    acv = acnv
                    vt = vsl if br == 0 else vwn
                    mm(acc, acc[0:65, bb0 * 128:bb1 * 128], vt, vt[:, j, g, :], pt, pt[:, bb0 * 128:bb1 * 128],
                       j == jfirst, j == 4 * i + 3)
                    for dk in [d for d in list(deferred) if d[0] <= k]:
                        deferred.remove(dk)
                        dk[1]()
                    if j == 4 * i + 3:
                        evac(oTs, oTs[0:65, :], acc, acc[0:65, :], eng="dve")

                        def fin(h=h, br=br, i=i, on=on):
                            for bb in range(4):
                                tr(pxx, acv[:, bb, 0:65], oTs, oTs[0:65, bb * 128:(bb + 1) * 128], identf, identf[0:65, 0:65])
                            P.add("dve", lambda e: e.reciprocal(out=zc[:, :], in_=acv[:, :, 64]), r=[pxx], w=[zc])
                            P.add("dve", lambda e: e.tensor_tensor(out=coef[:, :], in0=zc[:, :],
                                                                   in1=sg[:, 4 * i:4 * i + 4, 3 * h + 1 + br], op=ALU.mult),
                                  r=[zc, sg], w=[coef])
                            for bb in range(4):
                                P.add("dve", lambda e, bb=bb: e.scalar_tensor_tensor(
                                    out=on[:, bb, 64 * h:64 * h + 64], in0=acv[:, bb, 0:64], scalar=coef[:, bb:bb + 1],
                                    in1=on[:, bb, 64 * h:64 * h + 64], op0=ALU.mult, op1=ALU.add), r=[pxx, coef, on], w=[on])
                        deferred.append((k + 3, fin))

                deferred = []
                pipeline(len(units), fa, fb, fc, la=2)
                for dk in deferred:
                    dk[1]()
        P.dma("sp", o_d[i * 512:(i + 1) * 512, 512:1024].rearrange("(s p) d -> p s d", p=128), on[:, :, :], r=[on], w=[RO])
    A.release(mN)
    PS.release(pN)


def build_ffn(Ld):
    from types import SimpleNamespace
    L = SimpleNamespace(**Ld)
    P, A, PS = L.P, L.A, L.PS
    S, NT, NQ, l = L.S, L.NT, L.NQ, L.l
    mod, gn, identb = L.mod, L.gn, L.identb
    o_d, RO, RX, RW = L.o_d, L.RO, L.RX, L.RW
    mm, tr, evac = L.mm, L.tr, L.evac
    xsrc, out = L.xsrc, L.out
    mE = A.mark()
    pE = PS.mark()
    wo = A.alloc("wo", [8, D], BF16)
    P.dma("sp", wo[:, :, :], L.wo_d[l], r=[RW], w=[wo])
    ots = [A.alloc("ot%d" % k, [D], F32) for k in range(2)]
    xts = [A.alloc("fx%d" % k, [D], F32) for k in range(2)]
    x1s = [A.alloc("x1_%d" % k, [4, D], F32) for k in range(2)]
    mxb = A.alloc("mxb", [D], BF16)
    mxT = A.alloc("mxT", [8, 128], BF16)
    hb = A.alloc("fhb", [4, D], BF16)
    hT = A.alloc("fhT", [8, 512], BF16)
    tmp = A.alloc("ftmp", [D], F32)
    junk = A.alloc("fjunk", [D], BF16)
    st = A.alloc("fst", [8], F32)
    actT = A.alloc("actT", [22, 512], BF16)
    sil = [A.alloc("sil%d" % k, [512], F32) for k in range(2)]
    wgus = [A.alloc("wgu%d" % k, [2, 8, 128], BF16) for k in range(3)]
    wds = [A.alloc("wd%d" % k, [2, D], BF16) for k in range(3)]
    xo = [A.alloc("xo%d" % k, [D], F32) for k in range(2)]
    ptr = [PS.alloc("fptr%d" % k, 512) for k in range(2)]
    pg = [PS.alloc("pg%d" % k, 512) for k in range(2)]

    def bfv(p_):
        return p_[:, :].bitcast(BF16)[:, 0:512]

    pu = [PS.alloc("pu%d" % k, 512) for k in range(2)]
    py = [PS.alloc("py%d" % k, 512) for k in range(2)]
    kt = [0]
    kw = [0]

    def rstd_from(ss_ap, ss_t, n):
        P.add("dve", lambda e: e.tensor_scalar(out=ss_ap, in0=ss_ap, scalar1=1.0 / n, scalar2=1e-6, op0=ALU.mult, op1=ALU.add),
              r=[ss_t], w=[ss_t])
        P.add("act", lambda e: e.activation(out=ss_ap, in_=ss_ap, func=AF.Sqrt), r=[ss_t], w=[ss_t])
        P.add("dve", lambda e: e.reciprocal(out=ss_ap, in_=ss_ap), r=[ss_t], w=[ss_t])

    def dprime(tb, s_):
        t0 = tb * 512
        x1 = x1s[tb % 2]
        r0 = t0 + s_ * 128
        ot, xt = ots[s_ % 2], xts[s_ % 2]
        P.dma("sp", ot[:, :], o_d[r0:r0 + 128, :], r=[RO], w=[ot])
        P.dma("sp", xt[:, :], xsrc[r0:r0 + 128, :], r=[RX[tb]], w=[xt])
        P.add("dve", lambda e: e.memset(st[:, :], 0.0), w=[st])
        for gi, (c0, c1) in enumerate(((0, 256), (256, 512), (512, 1024))):
            P.add("act", lambda e, ot=ot, gi=gi, c0=c0, c1=c1: e.activation(
                out=junk[:, c0:c1], in_=ot[:, c0:c1], func=AF.Square, accum_out=st[:, gi:gi + 1]), r=[ot, st], w=[junk, st])
        P.add("dve", lambda e: e.tensor_scalar(out=st[:, 0:2], in0=st[:, 0:2], scalar1=1.0 / 256, scalar2=1e-6,
                                               op0=ALU.mult, op1=ALU.add), r=[st], w=[st])
        P.add("dve", lambda e: e.tensor_scalar(out=st[:, 2:3], in0=st[:, 2:3], scalar1=1.0 / 512, scalar2=1e-6,
                                               op0=ALU.mult, op1=ALU.add), r=[st], w=[st])
        P.add("act", lambda e: e.activation(out=st[:, 0:3], in_=st[:, 0:3], func=AF.Sqrt), r=[st], w=[st])
        P.add("dve", lambda e: e.reciprocal(out=st[:, 0:3], in_=st[:, 0:3]), r=[st], w=[st])
        for gi, (c0, c1) in enumerate(((0, 256), (256, 512), (512, 1024))):
            P.add("dve", lambda e, ot=ot, gi=gi, c0=c0, c1=c1: e.scalar_tensor_tensor(
                out=mxb[:, c0:c1], in0=ot[:, c0:c1], scalar=st[:, gi:gi + 1], in1=gn[:, c0:c1],
                op0=ALU.mult, op1=ALU.mult), r=[ot, st, gn], w=[mxb])
        for c in range(8):
            pt = ptr[kt[0] % 2]
            tr(pt, bfv(pt)[:, (c % 4) * 128:(c % 4 + 1) * 128], mxb, mxb[:, c * 128:(c + 1) * 128], identb, identb[:, :])
            if c % 4 == 3:
                evac(mxT, mxT[:, c - 3:c + 1, :].rearrange("p a b -> p (a b)"), pt, bfv(pt))
                kt[0] += 1
        for nh in range(2):
            p_ = py[nh]
            for c in range(8):
                mm(p_, p_[:, :], mxT, mxT[:, c, :], wo, wo[:, c, nh * 512:(nh + 1) * 512], c == 0, c == 7)
        P.add("dve", lambda e: e.memset(st[:, 4:6], 0.0), w=[st])
        for nh in range(2):
            P.add("act", lambda e, nh=nh: e.activation(out=junk[:, nh * 512:(nh + 1) * 512], in_=py[nh][:, :], func=AF.Square,
                                                       accum_out=st[:, 4 + nh:5 + nh]), r=[py[nh], st], w=[junk, st])
        P.add("dve", lambda e: e.tensor_tensor(out=st[:, 6:7], in0=st[:, 4:5], in1=st[:, 5:6], op=ALU.add), r=[st], w=[st])
        rstd_from(st[:, 6:7], st, D)
        for nh in range(2):
            P.add("dve", lambda e, nh=nh: e.scalar_tensor_tensor(
                out=tmp[:, nh * 512:(nh + 1) * 512], in0=py[nh][:, :], scalar=st[:, 6:7], in1=mod[:, 2, nh * 512:(nh + 1) * 512],
                op0=ALU.mult, op1=ALU.mult), r=[py[nh], st, mod], w=[tmp])
        P.add("dve", lambda e, xt=xt, s_=s_: e.tensor_tensor(out=x1[:, s_, :], in0=tmp[:, :], in1=xt[:, :], op=ALU.add),
              r=[tmp, xt], w=[x1])
        P.add("dve", lambda e: e.memset(st[:, 7:8], 0.0), w=[st])
        P.add("act", lambda e, s_=s_: e.activation(out=junk[:, :], in_=x1[:, s_, :], func=AF.Square, accum_out=st[:, 7:8]),
              r=[x1, st], w=[junk, st])
        rstd_from(st[:, 7:8], st, D)
        P.add("dve", lambda e, s_=s_: e.scalar_tensor_tensor(out=tmp[:, :], in0=x1[:, s_, :], scalar=st[:, 7:8],
                                                            in1=mod[:, 4, :], op0=ALU.mult, op1=ALU.mult),
              r=[x1, st, mod], w=[tmp])
        P.add("dve", lambda e, s_=s_: e.tensor_tensor(out=hb[:, s_, :], in0=tmp[:, :], in1=mod[:, 3, :], op=ALU.add),
              r=[tmp, mod], w=[hb])

    nxt = []
    nper = (len(nxt) + NQ * 11 - 1) // (NQ * 11) if nxt else 0
    def tpose():
        for c in range(8):
            pt = ptr[kt[0] % 2]
            kt[0] += 1
            for s_ in range(4):
                tr(pt, bfv(pt)[:, s_ * 128:(s_ + 1) * 128], hb, hb[:, s_, c * 128:(c + 1) * 128], identb, identb[:, :])
            evac(hT, hT[:, c, :], pt, bfv(pt))

    for s_ in range(4):
        dprime(0, s_)
    for tb in range(NQ):
        t0 = tb * 512
        x1 = x1s[tb % 2]
        if L.debug and l == 0:
            P.dma("sp", L.dbg["x1"][t0:t0 + 512, :].rearrange("(s p) d -> p s d", p=128), x1[:, :, :], r=[x1])
        if tb == 0:
            tpose()
        for hc in range(22):
            wgu = wgus[kw[0] % 3]
            kw[0] += 1
            P.dma("sp", wgu[:, :, :, :], L.wgu_d[l, hc], r=[RW], w=[wgu])
            pg_, pu_, sl = pg[hc % 2], pu[hc % 2], sil[hc % 2]
            for c in range(8):
                mm(pg_, pg_[:, :], wgu, wgu[:, 0, c, :], hT, hT[:, c, :], c == 0, c == 7)
            for c in range(8):
                mm(pu_, pu_[:, :], wgu, wgu[:, 1, c, :], hT, hT[:, c, :], c == 0, c == 7)
            P.add("act", lambda e, pg_=pg_, sl=sl: e.activation(out=sl[:, :], in_=pg_[:, :], func=AF.Silu), r=[pg_], w=[sl])
            P.add("dve", lambda e, pu_=pu_, sl=sl, hc=hc: e.tensor_tensor(out=actT[:, hc, :], in0=sl[:, :], in1=pu_[:, :],
                                                                        op=ALU.mult), r=[sl, pu_], w=[actT])
            if tb + 1 < NQ and hc in (2, 6, 10, 14):
                dprime(tb + 1, (hc - 2) // 4)
            if hc % 2 == 1:
                for _ in range(nper):
                    if nxt:
                        nxt.pop(0)("act")
        if tb + 1 < NQ:
            tpose()
        yps = [py[0], py[1], pg[0], pg[1], pu[0], pu[1], ptr[0], ptr[1]]

        def ybank(p_):
            return p_[:, :]

        for hc in range(22):
            wd_ = wds[(hc // 2) % 3]
            if hc % 2 == 0:
                P.dma("sp", wd_[:, :, :], L.wd_d[l, :, hc:hc + 2, :], r=[RW], w=[wd_])
            for s_ in range(4):
                for nh in range(2):
                    p_ = yps[s_ * 2 + nh]
                    mm(p_, ybank(p_), actT, actT[:, hc, s_ * 128:(s_ + 1) * 128], wd_, wd_[:, hc % 2, nh * 512:(nh + 1) * 512],
                       hc == 0, hc == 21)
        for es_, s_ in enumerate((1, 2, 0, 3)):
            r0 = t0 + s_ * 128
            xo_ = xo[es_ % 2]
            P.add("dve", lambda e: e.memset(st[:, 4:6], 0.0), w=[st])
            for nh in range(2):
                p_ = yps[s_ * 2 + nh]
                P.add("act", lambda e, nh=nh, p_=p_: e.activation(out=junk[:, nh * 512:(nh + 1) * 512], in_=ybank(p_), func=AF.Square,
                                                                 accum_out=st[:, 4 + nh:5 + nh]), r=[p_, st], w=[junk, st])
            P.add("dve", lambda e: e.tensor_tensor(out=st[:, 6:7], in0=st[:, 4:5], in1=st[:, 5:6], op=ALU.add), r=[st], w=[st])
            rstd_from(st[:, 6:7], st, D)
            for nh in range(2):
                p_ = yps[s_ * 2 + nh]
                P.add("dve", lambda e, nh=nh, p_=p_: e.scalar_tensor_tensor(
                    out=tmp[:, nh * 512:(nh + 1) * 512], in0=ybank(p_), scalar=st[:, 6:7], in1=mod[:, 5, nh * 512:(nh + 1) * 512],
                    op0=ALU.mult, op1=ALU.mult), r=[p_, st, mod], w=[tmp])
            P.add("dve", lambda e, s_=s_, xo_=xo_, x1=x1: e.tensor_tensor(out=xo_[:, :], in0=tmp[:, :], in1=x1[:, s_, :], op=ALU.add),
                  r=[tmp, x1], w=[xo_])
            P.dma("act", out[r0:r0 + 128, :], xo_[:, :], r=[xo_], w=[RX[tb]])
    while nxt:
        nxt.pop(0)("act")
    A.release(mE)
    PS.release(pE)
```

```python
import math
import numpy as np
import ml_dtypes
import concourse.bass as bass
import concourse.mybir as mybir
from concourse.bass_utils import run_bass_kernel_spmd

F32 = mybir.dt.float32
BF16 = mybir.dt.bfloat16
U8 = mybir.dt.uint8
AF = mybir.ActivationFunctionType
ALU = mybir.AluOpType

D = 1024
HD = 64
FFN = 2816
NIN = 2588
NEG = -30000.0
DSZ = {F32: 4, BF16: 2, U8: 1}

COMPUTE = ("act", "dve", "pool", "pe")
DMAQ = ("sp", "act")


class Res:
    __slots__ = ("name", "writers", "readers", "wdeps")

    def __init__(self, name=""):
        self.name = name
        self.writers = []
        self.readers = []
        self.wdeps = []


class Tile(Res):
    __slots__ = ("ap",)

    def __init__(self, name, ap):
        Res.__init__(self, name)
        self.ap = ap

    def __getitem__(self, k):
        return self.ap[k]


class Op:
    __slots__ = ("eng", "fn", "deps", "dma", "signal", "sem", "val", "epoch", "prev")

    def __init__(self, eng, fn, dma, epoch):
        self.eng = eng
        self.fn = fn
        self.dma = dma
        self.deps = {}
        self.signal = False
        self.sem = None
        self.val = 0
        self.epoch = epoch
        self.prev = None


class Prog:
    def __init__(self, nc):
        self.nc = nc
        self.ops = []
        self.epoch = 0

    @staticmethod
    def _push(lst, op):
        if not op.dma:
            for i, o in enumerate(lst):
                if (not o.dma) and o.eng == op.eng:
                    lst[i] = op
                    return
        lst.append(op)

    def add(self, eng, fn, r=(), w=(), dma=False):
        op = Op(eng, fn, dma, self.epoch)
        deps = op.deps
        for res in r:
            for wop in res.writers:
                deps[wop] = True
        for res in w:
            if res.readers:
                for rop in res.readers:
                    deps.setdefault(rop, False)
                for wop in res.writers:
                    deps.setdefault(wop, False)
            else:
                for pop in res.wdeps:
                    deps.setdefault(pop, False)
        for res in w:
            if res.readers or (res in r):
                if res.readers:
                    res.wdeps = list(res.readers) + list(res.writers)
                res.writers = [op]
                res.readers = []
            else:
                self._push(res.writers, op)
        for res in r:
            if res not in w:
                self._push(res.readers, op)
        self.ops.append(op)
        return op

    def dma(self, q, out_ap, in_ap, r=(), w=(), slow=False):
        if slow:
            return self.add(q, lambda e: e.dma_start(out=out_ap, in_=in_ap, allow_slow_non_contiguous=True),
                            r=r, w=w, dma=True)
        return self.add(q, lambda e: e.dma_start(out=out_ap, in_=in_ap), r=r, w=w, dma=True)

    def emit(self, stack, n_epochs):
        nc = self.nc
        NPOOL = 6
        engsem = {}
        for e in COMPUTE:
            engsem[e] = [stack.enter_context(nc.semaphore("c_%s_%d" % (e, k))) for k in range(n_epochs)]
        dmasem = {}
        for q in DMAQ:
            dmasem[q] = [stack.enter_context(nc.semaphore("d_%s_%d" % (q, k))) for k in range(NPOOL)]
        for op in self.ops:
            for d in op.deps:
                d.signal = True
        cnt = {}
        dcount = {}
        dlast = {}
        dk = {q: 0 for q in DMAQ}
        for op in self.ops:
            if op.dma:
                s = dmasem[op.eng][dk[op.eng] % NPOOL]
                dk[op.eng] += 1
                dcount[s] = dcount.get(s, 0) + 1
                op.sem = s
                op.val = 16 * dcount[s]
                op.prev = dlast.get(s)
                dlast[s] = op
            elif op.signal:
                key = (op.eng, op.epoch)
                cnt[key] = cnt.get(key, 0) + 1
                op.sem = engsem[op.eng][op.epoch]
                op.val = cnt[key]
        print("sem counts", {k: v for k, v in cnt.items()}, "dma max", max(dcount.values()) * 16 if dcount else 0)
        streams = {"sp": [], "act": [], "dve": [], "pool": [], "pe": []}
        for op in self.ops:
            streams[op.eng].append(op)
        nwaits = [0]

        def run(engname, e):
            known = {}
            kep = {}

            def need(d):
                if d.dma:
                    return known.get(id(d.sem), 0) < d.val
                if kep.get(d.eng, -1) > d.epoch:
                    return False
                return known.get(id(d.sem), 0) < d.val

            def wait(d):
                e.wait_ge(d.sem, d.val)
                nwaits[0] += 1
                known[id(d.sem)] = d.val
                if not d.dma:
                    if kep.get(d.eng, -1) < d.epoch:
                        kep[d.eng] = d.epoch

            for op in streams[engname]:
                for d, raw in op.deps.items():
                    if (not d.dma) and (not op.dma) and d.eng == op.eng:
                        if engname == "pe":
                            continue
                    if need(d):
                        wait(d)
                if op.dma and op.prev is not None and need(op.prev):
                    wait(op.prev)
                ins = op.fn(e)
                if op.dma:
                    ins.then_inc(op.sem, 16)
                elif op.signal:
                    ins.then_inc(op.sem, 1)
            if engname in DMAQ:
                for s in dmasem[engname]:
                    if s in dlast and known.get(id(s), 0) < dlast[s].val:
                        e.wait_ge(s, dlast[s].val)

        with nc.Block() as block:
            @block.sync
            def _(e):
                run("sp", e)

            @block.scalar
            def _(e):
                run("act", e)

            @block.vector
            def _(e):
                run("dve", e)

            @block.gpsimd
            def _(e):
                run("pool", e)

            @block.tensor
            def _(e):
                run("pe", e)
        return nwaits[0]


class Arena:
    def __init__(self, base_ap, size):
        self.base = base_ap
        self.size = size
        self.top = 0
        self.live = []
        self.grave = []

    def alloc(self, name, free_shape, dt, parts=128):
        n = 1
        for s in free_shape:
            n *= s
        nb = n * DSZ[dt]
        nb_al = (nb + 63) // 64 * 64
        off = self.top
        assert off + nb_al <= self.size, "SBUF arena overflow at %s: %d + %d > %d" % (name, off, nb_al, self.size)
        self.top += nb_al
        ap = self.base[0:parts, off:off + nb]
        if dt != U8:
            ap = ap.bitcast(dt)
        if len(free_shape) == 2:
            ap = ap.rearrange("p (a b) -> p a b", a=free_shape[0])
        elif len(free_shape) == 3:
            ap = ap.rearrange("p (a b c) -> p a b c", a=free_shape[0], b=free_shape[1])
        elif len(free_shape) == 4:
            ap = ap.rearrange("p (a b c d) -> p a b c d", a=free_shape[0], b=free_shape[1], c=free_shape[2])
        t = Tile(name, ap)
        keep = []
        for (g0, g1, gt) in self.grave:
            if g0 < off + nb_al and off < g1:
                t.readers.extend(gt.readers)
                t.readers.extend(gt.writers)
                if g0 >= off and g1 <= off + nb_al:
                    continue
            keep.append((g0, g1, gt))
        self.grave = keep
        self.live.append((off, off + nb_al, t))
        return t

    def mark(self):
        return (self.top, len(self.live))

    def release(self, m):
        top, nl = m
        for ent in self.live[nl:]:
            self.grave.append(ent)
        del self.live[nl:]
        self.top = top


class PsumArena:
    def __init__(self, banks):
        self.banks = banks
        self.top = 0
        self.live = []
        self.grave = []

    def alloc(self, name, ncols, dt=F32, parts=128):
        nb = ncols * DSZ[dt]
        nb = (nb + 3) // 4 * 4
        if self.top % 2048:
            self.top = (self.top // 2048 + 1) * 2048
        off = self.top
        assert off + nb <= 8 * 2048, "PSUM overflow at %s" % name
        self.top += nb
        bank = off // 2048
        c0 = (off % 2048) // 4
        ap = self.banks[bank][0:parts, c0:c0 + nb // 4]
        if dt != F32:
            ap = ap.bitcast(dt)
        t = Tile(name, ap)
        keep = []
        for (g0, g1, gt) in self.grave:
            if g0 < off + nb and off < g1:
                t.readers.extend(gt.readers)
                t.readers.extend(gt.writers)
                if g0 >= off and g1 <= off + nb:
                    continue
            keep.append((g0, g1, gt))
        self.grave = keep
        self.live.append((off, off + nb, t))
        return t

    def mark(self):
        return (self.top, len(self.live))

    def release(self, m):
        top, nl = m
        for ent in self.live[nl:]:
            self.grave.append(ent)
        del self.live[nl:]
        self.top = top


def _t5_bucket(dist):
    n = np.maximum(dist, 0)
    nf = np.maximum(n, 1).astype(np.float32)
    large = 16 + (np.log(nf / np.float32(16)) / np.float32(math.log(1024 / 16)) * np.float32(16)).astype(np.int32)
    large = np.minimum(large, 31)
    return np.where(n < 16, n, large)


def _onehot(dvals, valid):
    oh = np.zeros((33, len(dvals)), np.float32)
    b = _t5_bucket(dvals)
    for x in range(len(dvals)):
        if valid[x]:
            oh[b[x], x] = 1.0
        else:
            oh[32, x] = 1.0
    return oh


DL_SWA = 383
DL_NSA = 1791
DL_CMP = 6128


def host_consts(S):
    bf = ml_dtypes.bfloat16
    c = {}
    c["k_identb"] = np.eye(128, dtype=np.float32).astype(bf)
    c["k_identf"] = np.eye(128, dtype=np.float32)
    tp = np.arange(128)
    c["k_tri"] = (tp[:, None] <= tp[None, :]).astype(np.float32)
    d = np.arange(DL_SWA) - 127
    c["k_ohswa"] = _onehot(d, (d >= 0) & (d < 128))
    d = np.arange(DL_NSA) - 127
    c["k_ohnsa"] = _onehot(d, d >= 0)
    d = np.arange(DL_CMP) - 2063
    c["k_ohcmp"] = _onehot(d, d >= 0)
    s = np.arange(S)
    c["k_sel"] = (s[None, :] // 64 == np.arange(64)[:, None]).astype(np.float32).astype(bf)
    a = np.arange(128)[:, None]
    u = np.arange(1024)[None, :]
    c["k_wm"] = np.where(u - a < 512, 0.0, NEG).astype(np.float32).astype(bf)
    u = np.arange(512)[None, :]
    c["k_cm"] = np.where(u - a >= 0, 0.0, NEG).astype(np.float32).astype(bf)
    z = np.arange(127)[None, :] - 63
    rel = z - (np.arange(128)[:, None] // 64)
    forced = (rel == 0) | (rel == -1)
    future = rel > 0
    c["k_km"] = np.where(forced | future, 0.0, 1.0).astype(np.float32)
    c["k_ov"] = np.where(forced, 1e30, np.where(future, -1e30, 0.0)).astype(np.float32)
    n_c = S // 16 - 1
    cs = np.arange(n_c) * 16
    ss = np.arange(64) * 64
    ov = np.clip(np.minimum(cs[:, None] + 32, ss[None, :] + 64) - np.maximum(cs[:, None], ss[None, :]), 0, None)
    ovl = np.zeros((256, 64), np.float32)
    ovl[:n_c] = ov.astype(np.float32) / 32.0
    c["k_ovl"] = ovl
    c["k_ones"] = np.ones((8, S), np.float32).astype(bf)
    return c


CONST_SPECS = [
    ("k_identb", [128, 128], BF16), ("k_identf", [128, 128], F32), ("k_tri", [128, 128], F32),
    ("k_ohswa", [33, DL_SWA], F32), ("k_ohnsa", [33, DL_NSA], F32), ("k_ohcmp", [33, DL_CMP], F32),
    ("k_sel", None, BF16), ("k_wm", [128, 1024], BF16), ("k_cm", [128, 512], BF16),
    ("k_km", [128, 127], F32), ("k_ov", [128, 127], F32), ("k_ovl", [256, 64], F32),
    ("k_ones", 8, BF16),
]


WSPLITS = [
    ("swa_q", 0, 256), ("swa_k", 256, 128), ("swa_v", 384, 128), ("fox_q", 512, 256), ("fox_k", 768, 256),
    ("fox_v", 1024, 256), ("fb", 1280, 4), ("nsa_q", 1284, 512), ("kc", 1796, 128), ("vc", 1924, 128),
    ("ks", 2052, 128), ("vs", 2180, 128), ("kw", 2308, 128), ("vw", 2436, 128), ("gc", 2564, 24),
]
TORDER = ["swa_q", "fox_q", "nsa_q", "swa_k", "fox_k", "kc", "vc", "ks", "kw"]
VORDER = ["swa_v", "fox_v", "vs", "vw", "fb", "gc"]
QT_SWAQ, QT_FOXQ, QT_NSAQ, QT_SWAK, QT_FOXK, QT_KC, QT_VC, QT_KS, QT_KW = 0, 256, 512, 1024, 1152, 1408, 1536, 1664, 1792
NFT = 1920
V_SWA, V_FOX, V_S, V_W = 0, 128, 384, 512
NV = 640


def dap(t, offset, dims):
    return bass.AP(tensor=t.tensor, offset=offset, ap=[list(x) for x in dims])


def build(S, DEPTH, debug=False, stop_after=None):
    from contextlib import ExitStack
    nc = bass.Bass("TRN2", target_bir_lowering=False)
    NT = S // 128
    NQ = S // 512
    NCMP = S // 16 - 1
    NNT = (NCMP + 127) // 128
    nsz = [min(128, NCMP - 128 * k) for k in range(NNT)]

    def din(name, shape, dt=F32):
        return nc.dram_tensor(name, list(shape), dt, kind="ExternalInput").ap()

    def dscr(name, shape, dt):
        return nc.dram_tensor(name, list(shape), dt, kind="Internal").ap()

    x_in = din("x", [S, D])
    c_in = din("c", [D])
    rel_bias = din("rel_bias", [32, 12])
    ada_w = din("ada_w", [DEPTH, D, 6 * D])
    ada_b = din("ada_b", [DEPTH, 6 * D])
    g_apre = din("attn_pre_norm", [DEPTH, D])
    g_apost = din("attn_post_norm", [DEPTH, D])
    g_fpre = din("ffn_pre_norm", [DEPTH, D])
    g_fpost = din("ffn_post_norm", [DEPTH, D])
    w_in = din("w_in", [DEPTH, D, NIN])
    forget_bias = din("forget_bias", [DEPTH, 4])
    swa_sinks = din("swa_sinks", [DEPTH, 4])
    cmp_pos = din("cmp_pos", [DEPTH, 2, 32, 64])
    cmp_w1 = din("cmp_w1", [DEPTH, 2, 2048, 128])
    cmp_w2 = din("cmp_w2", [DEPTH, 2, 128, 64])
    group_norm = din("group_norm", [DEPTH, D])
    w_out = din("w_out", [DEPTH, D, D])
    w_gate = din("ffn_w_gate", [DEPTH, D, FFN])
    w_up = din("ffn_w_up", [DEPTH, D, FFN])
    w_down = din("ffn_w_down", [DEPTH, FFN, D])
    K = {}
    for name, shape, dt in CONST_SPECS:
        if shape is None:
            shape = [64, S]
        elif shape == 8:
            shape = [8, S]
        K[name] = din(name, shape, dt)
    out = nc.dram_tensor("out", [S, D], F32, kind="ExternalOutput").ap()

    qT_d = dscr("qT_d", [NFT, S], BF16)
    v_d = dscr("v_d", [S, NV], BF16)
    o_d = dscr("o_d", [S, D], F32)
    g_swa_d = dscr("g_swa_d", [4, DL_SWA], BF16)
    g_nsa_d = dscr("g_nsa_d", [8, DL_NSA], BF16)
    g_cmp_d = dscr("g_cmp_d", [8, DL_CMP], BF16)
    t_swa_d = dscr("t_swa_d", [4, 128, DL_SWA], BF16)
    t_nsa_d = dscr("t_nsa_d", [8, 128, DL_NSA], BF16)
    t_cmp_d = dscr("t_cmp_d", [8, 128, DL_CMP], BF16)
    wgu_d = dscr("wgu_d", [DEPTH, 22, 128, 2, 8, 128], BF16)
    wd_d = dscr("wd_d", [DEPTH, 128, 22, D], BF16)
    wo_d = dscr("wo_d", [DEPTH, 128, 8, D], BF16)
    dbg = {}
    if debug:
        dbg["qT"] = nc.dram_tensor("dbg_qT", [NFT, S], BF16, kind="ExternalOutput").ap()
        dbg["v"] = nc.dram_tensor("dbg_v", [S, NV], BF16, kind="ExternalOutput").ap()
        dbg["o"] = nc.dram_tensor("dbg_o", [S, D], F32, kind="ExternalOutput").ap()
        dbg["mod"] = nc.dram_tensor("dbg_mod", [128, 6 * D], F32, kind="ExternalOutput").ap()
        dbg["fg"] = nc.dram_tensor("dbg_fg", [128, NT * 28], F32, kind="ExternalOutput").ap()
        dbg["x1"] = nc.dram_tensor("dbg_x1", [S, D], F32, kind="ExternalOutput").ap()

    stack = ExitStack()
    with stack:
        ARENA_BYTES = 206 * 1024
        arena_t = stack.enter_context(nc.sbuf_tensor("arena", [128, ARENA_BYTES], U8))
        banks = [stack.enter_context(nc.psum_tensor("pb%d" % k, [128, 512], F32)) for k in range(8)]
        A = Arena(arena_t[:, :], ARENA_BYTES)
        PS = PsumArena([b[:, :] for b in banks])
        P = Prog(nc)
        RQT = Res("qT_d")
        RV = Res("v_d")
        RO = Res("o_d")
        RX = [Res("xblk%d" % k) for k in range(NQ)]
        RW = Res("wconv")
        rr = [0]

        def evac(out_t, out_ap, in_t, in_ap, scale=None, eng=None):
            rr[0] += 1
            if eng == "act" or (eng is None and rr[0] % 2 == 0):
                if scale is None:
                    P.add("act", lambda e: e.copy(out=out_ap, in_=in_ap), r=[in_t], w=[out_t])
                else:
                    P.add("act", lambda e: e.mul(out=out_ap, in_=in_ap, mul=scale), r=[in_t], w=[out_t])
            else:
                if scale is None:
                    P.add("dve", lambda e: e.tensor_copy(out=out_ap, in_=in_ap), r=[in_t], w=[out_t])
                else:
                    P.add("dve", lambda e: e.tensor_scalar(out=out_ap, in0=in_ap, scalar1=scale, scalar2=None,
                                                           op0=ALU.mult), r=[in_t], w=[out_t])

        def mm(out_t, out_ap, lt, lap, rt, rap, start, stop):
            P.add("pe", lambda e: e.matmul(out_ap, lap, rap, start=start, stop=stop), r=[lt, rt], w=[out_t])

        def tr(out_t, out_ap, in_t, in_ap, ident_t, ident_ap):
            P.add("pe", lambda e: e.transpose(out_ap, in_ap, ident_ap), r=[in_t, ident_t], w=[out_t])

        identb = A.alloc("identb", [128], BF16)
        identf = A.alloc("identf", [128], F32)
        tri = A.alloc("tri", [128], F32)
        onesf = A.alloc("onesf", [128], F32)
        onesb = A.alloc("onesb", [512], BF16)
        cmt = A.alloc("cm", [512], BF16)
        wmt = A.alloc("wm", [1024], BF16)
        kmt = A.alloc("km", [127], F32)
        ovt = A.alloc("ov", [127], F32)
        P.dma("sp", identb[:, :], K["k_identb"], w=[identb])
        P.dma("sp", identf[:, :], K["k_identf"], w=[identf])
        P.dma("sp", tri[:, :], K["k_tri"], w=[tri])
        P.dma("sp", cmt[:, :], K["k_cm"], w=[cmt])
        P.dma("sp", wmt[:, :], K["k_wm"], w=[wmt])
        P.dma("sp", kmt[:, :], K["k_km"], w=[kmt])
        P.dma("sp", ovt[:, :], K["k_ov"], w=[ovt])
        P.add("dve", lambda e: e.memset(onesf[:, :], 1.0), w=[onesf])
        P.add("dve", lambda e: e.memset(onesb[:, :], 1.0), w=[onesb])

        cvf = [A.alloc("cvf%d" % k, [1024], F32) for k in range(2)]
        cvb = [A.alloc("cvb%d" % k, [1024], BF16) for k in range(2)]
        cvk = [0]
        pend = []

        def conv_chunks(l):
            jobs = []

            def job(load_src_ap, a, dst_ap):
                def run(q="sp"):
                    f, b = cvf[cvk[0] % 2], cvb[cvk[0] % 2]
                    cvk[0] += 1
                    fv = f[:, :].rearrange("p (a b) -> p a b", a=a) if a > 1 else f[:, :]
                    bv = b[:, :].rearrange("p (a b) -> p a b", a=a) if a > 1 else b[:, :]
                    P.dma(q, fv, load_src_ap, w=[f])
                    if pend:
                        pend.pop(0)()
                    P.add("pool", lambda e: e.tensor_copy(out=b[:, :], in_=f[:, :]), r=[f], w=[b])
                    pend.append(lambda: P.dma(q, dst_ap, bv, r=[b], w=[RW]))
                jobs.append(run)

            for hc in range(22):
                for gi, src in enumerate((w_gate, w_up)):
                    job(src[l, :, hc * 128:(hc + 1) * 128].rearrange("(c p) n -> p c n", p=128), 8, wgu_d[l, hc, :, gi])
            for hc in range(22):
                job(w_down[l, hc * 128:(hc + 1) * 128, :], 1, wd_d[l, :, hc, :])
            for c in range(8):
                job(w_out[l, c * 128:(c + 1) * 128, :], 1, wo_d[l, :, c, :])
            return jobs

        m0 = A.mark()
        pm0 = PS.mark()
        tabl = A.alloc("tabl", [12], F32, parts=33)
        P.add("dve", lambda e: e.memset(tabl[32:33, :], NEG), w=[tabl])
        P.dma("sp", tabl[0:32, :], rel_bias, w=[tabl])
        oht = [A.alloc("oht%d" % k, [512], F32, parts=33) for k in range(2)]
        gst = [A.alloc("gst%d" % k, [512], BF16, parts=8) for k in range(2)]
        pst = [PS.alloc("pst%d" % k, 512, F32, parts=8) for k in range(2)]
        RG = Res("gtab")
        kk = 0
        for (ohn, h0, nh, DL, gd, td) in (("k_ohswa", 0, 4, DL_SWA, g_swa_d, t_swa_d),
                                           ("k_ohnsa", 4, 8, DL_NSA, g_nsa_d, t_nsa_d),
                                           ("k_ohcmp", 4, 8, DL_CMP, g_cmp_d, t_cmp_d)):
            for c0 in range(0, DL, 512):
                n = min(512, DL - c0)
                ot, gs, ps = oht[kk % 2], gst[kk % 2], pst[kk % 2]
                kk += 1
                P.dma("sp", ot[:, 0:n], K[ohn][:, c0:c0 + n], w=[ot])
                mm(ps, ps[0:nh, 0:n], tabl, tabl[:, h0:h0 + nh], ot, ot[:, 0:n], True, True)
                evac(gs, gs[0:nh, 0:n], ps, ps[0:nh, 0:n])
                P.dma("sp", gd[:, c0:c0 + n], gs[0:nh, 0:n], r=[gs], w=[RG])
            import os
            if not os.environ.get("K_SKIP_REP"):
                for h in range(nh):
                    P.dma("sp", td[h], dap(gd, h * DL, [[0, 128], [1, DL]]), r=[RG], w=[RG])
        A.release(m0)
        PS.release(pm0)
        cb = A.alloc("cb", [8, 128], F32)
        m0 = A.mark()
        cl = A.alloc("cl", [8], F32)
        P.dma("sp", cl[:, :], c_in.rearrange("(c p) -> p c", p=128), w=[cl], slow=True)
        P.add("act", lambda e: e.activation(out=cl[:, :], in_=cl[:, :], func=AF.Silu), r=[cl], w=[cl])
        P.add("dve", lambda e: e.tensor_copy(out=cb[:, :, :], in_=cl[:, :].unsqueeze(2).to_broadcast([128, 8, 128])),
              r=[cl], w=[cb])
        A.release(m0)

        import os
        conv_jobs = {}
        if not os.environ.get("K_SKIP_CONV"):
            for l in range(DEPTH):
                conv_jobs[l] = conv_chunks(l)

        persist_mark = A.mark()
        pspersist = PS.mark()

        for l in range(DEPTH):
            P.epoch = l
            xsrc = x_in if l == 0 else out
            mod = A.alloc("mod", [6, D], F32)
            gn = A.alloc("gn", [D], F32)
            mA = A.mark()
            pA = PS.mark()
            ones1 = A.alloc("ones1", [128], F32, parts=1)
            P.add("dve", lambda e: e.memset(ones1[:, :], 1.0), w=[ones1])
            wts = [A.alloc("adaw%d" % k, [8, 512], F32) for k in range(2)]
            bts = [A.alloc("adab%d" % k, [512], F32, parts=1) for k in range(2)]
            pss = [PS.alloc("psA%d" % k, 512) for k in range(2)]
            for ch in range(12):
                wt, bt, ps = wts[ch % 2], bts[ch % 2], pss[ch % 2]
                n0 = ch * 512
                P.dma("sp", wt[:, :, :], ada_w[l, :, n0:n0 + 512].rearrange("(c p) n -> p c n", p=128), w=[wt])
                P.dma("sp", bt[:, :], ada_b[l:l + 1, n0:n0 + 512], w=[bt])
                for c in range(8):
                    mm(ps, ps[:, :], cb, cb[:, c, :], wt, wt[:, c, :], c == 0, False)
                mm(ps, ps[:, :], ones1, ones1[:, :], bt, bt[:, :], False, True)
                evac(mod, mod[:, ch // 2, (ch % 2) * 512:(ch % 2) * 512 + 512], ps, ps[:, :])
            g4 = A.alloc("g4", [4, D], F32)
            for k, gsrc in enumerate((g_apre, g_apost, g_fpre, g_fpost)):
                P.dma("sp", g4[:, k, :], dap(gsrc, l * D, [[0, 128], [1, D]]), w=[g4])
            P.dma("sp", gn[:, :], dap(group_norm, l * D, [[0, 128], [1, D]]), w=[gn])
            for (mi, gi) in ((1, 0), (4, 2)):
                P.add("dve", lambda e, mi=mi, gi=gi: e.scalar_tensor_tensor(
                    out=mod[:, mi, :], in0=mod[:, mi, :], scalar=1.0, in1=g4[:, gi, :], op0=ALU.add, op1=ALU.mult),
                    r=[mod, g4], w=[mod])
            for (mi, gi) in ((2, 1), (5, 3)):
                P.add("dve", lambda e, mi=mi, gi=gi: e.tensor_tensor(
                    out=mod[:, mi, :], in0=mod[:, mi, :], in1=g4[:, gi, :], op=ALU.mult), r=[mod, g4], w=[mod])
            if debug and l == 0:
                P.dma("sp", dbg["mod"], mod[:, :, :].rearrange("p a b -> p (a b)"), r=[mod])
            A.release(mA)
            PS.release(pA)
            if stop_after == "A":
                break

            fg_mark = A.mark()
            fg = A.alloc("fg", [NT, 28], F32)
            mB = A.mark()
            pB = PS.mark()
            wsb = A.alloc("wsb", [8, NIN], BF16)
            dcol = {}
            c0 = 0
            for nm in TORDER + VORDER:
                dcol[nm] = c0
                c0 += [w for (n_, s_, w) in WSPLITS if n_ == nm][0]
            for (nm, src, wd) in WSPLITS:
                for cc in range(0, wd, 128):
                    n = min(128, wd - cc)
                    sft = cvf[cvk[0] % 2]
                    cvk[0] += 1
                    sf = sft[:, :].rearrange("p (a b) -> p a b", a=8)
                    P.dma("sp", sf[:, :, 0:n], w_in[l, :, src + cc:src + cc + n].rearrange("(c p) n -> p c n", p=128), w=[sft])
                    P.add(os.environ.get("K_CAST", "pool"), lambda e, sf=sf, n=n, d0=dcol[nm] + cc: e.tensor_copy(out=wsb[:, :, d0:d0 + n], in_=sf[:, :, 0:n]),
                          r=[sft], w=[wsb])
            xts = [A.alloc("xt%d" % k, [4, D], F32) for k in range(1)]
            hf = A.alloc("hf", [D], F32)
            hb = A.alloc("hb", [4, D], BF16)
            hT = A.alloc("hT", [8, 512], BF16)
            junk = A.alloc("junk", [D], BF16)
            ss = A.alloc("ss", [4], F32)
            rs = A.alloc("rs", [4], F32)
            stg = [A.alloc("stg%d" % k, [512], BF16) for k in range(3)]
            vst = [A.alloc("vst%d" % k, [4, NV], BF16) for k in range(2)]
            ptr = [PS.alloc("ptr%d" % k, 512, BF16) for k in range(2)]
            pmm = [PS.alloc("pmm%d" % k, 512) for k in range(4)]
            pv2 = [PS.alloc("pv2%d" % k, 156) for k in range(2)]
            kq = 0
            for tb in range(NQ):
                t0 = tb * 512
                xt = xts[0]
                P.dma("sp", xt[:, :, :], xsrc[t0:t0 + 512, :].rearrange("(s p) d -> p s d", p=128), r=[RX[tb]], w=[xt])
                if int(os.environ.get("K_B", "9")) < 1:
                    continue
                P.add("dve", lambda e: e.memset(ss[:, :], 0.0), w=[ss])
                for s in range(4):
                    P.add("act", lambda e, s=s, xt=xt: e.activation(out=junk[:, :], in_=xt[:, s, :], func=AF.Square,
                                                                  accum_out=ss[:, s:s + 1]), r=[xt, ss], w=[junk, ss])
                P.add("dve", lambda e: e.tensor_scalar(out=rs[:, :], in0=ss[:, :], scalar1=1.0 / D, scalar2=1e-6,
                                                       op0=ALU.mult, op1=ALU.add), r=[ss], w=[rs])
                P.add("act", lambda e: e.activation(out=rs[:, :], in_=rs[:, :], func=AF.Sqrt), r=[rs], w=[rs])
                P.add("dve", lambda e: e.reciprocal(out=rs[:, :], in_=rs[:, :]), r=[rs], w=[rs])
                for s in range(4):
                    P.add("dve", lambda e, s=s, xt=xt: e.scalar_tensor_tensor(
                        out=hf[:, :], in0=xt[:, s, :], scalar=rs[:, s:s + 1], in1=mod[:, 1, :],
                        op0=ALU.mult, op1=ALU.mult), r=[xt, rs, mod], w=[hf])
                    P.add("dve", lambda e, s=s: e.tensor_tensor(out=hb[:, s, :], in0=hf[:, :], in1=mod[:, 0, :],
                                                               op=ALU.add), r=[hf, mod], w=[hb])
                KB = int(os.environ.get("K_B", "9"))
                if KB < 2:
                    continue
                for c in range(8):
                    pt = ptr[c % 2]
                    for s in range(4):
                        tr(pt, pt[:, s * 128:(s + 1) * 128], hb, hb[:, s, c * 128:(c + 1) * 128], identb, identb[:, :])
                    evac(hT, hT[:, c, :], pt, pt[:, :])
                if KB < 3:
                    continue
                for m in range(NFT // 128):
                    ps = pmm[kq % 4]
                    sg = stg[kq % 3]
                    kq += 1
                    for c in range(8):
                        mm(ps, ps[:, :], wsb, wsb[:, c, m * 128:(m + 1) * 128], hT, hT[:, c, :], c == 0, c == 7)
                    evac(sg, sg[:, :], ps, ps[:, :], scale=(0.125 if m < 8 else None))
                    P.dma("act", qT_d[m * 128:(m + 1) * 128, t0:t0 + 512], sg[:, :], r=[sg], w=[RQT])
                if KB < 4:
                    continue
                vs_ = vst[tb % 2]
                for s in range(4):
                    ps = pmm[kq % 4]
                    kq += 1
                    p2 = pv2[s % 2]
                    for c in range(8):
                        mm(ps, ps[:, :], hT, hT[:, c, s * 128:(s + 1) * 128], wsb, wsb[:, c, NFT:NFT + 512], c == 0, c == 7)
                    for c in range(8):
                        mm(p2, p2[:, :], hT, hT[:, c, s * 128:(s + 1) * 128], wsb, wsb[:, c, NFT + 512:NFT + 668],
                           c == 0, c == 7)
                    if KB < 5:
                        continue
                    evac(vs_, vs_[:, s, 0:512], ps, ps[:, :])
                    evac(vs_, vs_[:, s, 512:640], p2, p2[:, 0:128], eng="dve")
                    if KB < 6:
                        continue
                    evac(fg, fg[:, tb * 4 + s, :], p2, p2[:, 128:156], eng="dve")
                if KB < 7:
                    continue
                P.dma("act", v_d[t0:t0 + 512, :].rearrange("(s p) f -> p s f", p=128), vs_[:, :, :], r=[vs_], w=[RV])
            if debug and l == 0 and KB >= 8:
                P.dma("sp", dbg["fg"], fg[:, :, :].rearrange("p a b -> p (a b)"), r=[fg])
                P.dma("sp", dbg["qT"], qT_d, r=[RQT])
                P.dma("sp", dbg["v"], v_d, r=[RV])
            A.release(mB)
            PS.release(pB)
            if stop_after == "B":
                break
            build_attention(locals())
            if debug and l == 0:
                if stop_after == "FS":
                    P.dma("sp", dbg["o"][:, 0:512], o_d[:, 0:512], r=[RO])
                else:
                    P.dma("sp", dbg["o"], o_d, r=[RO])
            if stop_after in ("C", "FS"):
                break
            A.release(fg_mark)
            build_ffn(locals())
            A.release(persist_mark)
            PS.release(pspersist)

        nw = P.emit(stack, max(DEPTH, 1))
        print("ops", len(P.ops), "waits", nw)
    return nc


def make_in_map(inputs, b, S, consts=None):
    m = {}
    for k, v in inputs.items():
        v = np.asarray(v)
        if k == "x" or k == "c":
            m[k] = np.ascontiguousarray(v[b], dtype=np.float32)
        else:
            m[k] = np.ascontiguousarray(v, dtype=np.float32)
    m.update(consts if consts is not None else host_consts(S))
    return m


def kernel(**inputs):
    x = np.asarray(inputs["x"])
    B, S, _ = x.shape
    DEPTH = int(np.asarray(inputs["ada_w"]).shape[0])
    nc = build(S, DEPTH)
    consts = host_consts(S)
    shared = {k: np.ascontiguousarray(np.asarray(v), dtype=np.float32) for k, v in inputs.items() if k not in ("x", "c")}
    in_maps = []
    for b in range(B):
        m = dict(shared)
        m["x"] = np.ascontiguousarray(x[b], dtype=np.float32)
        m["c"] = np.ascontiguousarray(np.asarray(inputs["c"])[b], dtype=np.float32)
        m.update(consts)
        in_maps.append(m)
    res = run_bass_kernel_spmd(nc, in_maps, core_ids=list(range(B)))
    return np.stack([np.asarray(res.results[b]["out"], dtype=np.float32) for b in range(B)], 0)


def pipeline(n, fa, fb, fc, la=2):
    for k in range(n + la):
        if k < n:
            fa(k)
        if k - la >= 0:
            fb(k - la)
            fc(k - la)


def build_attention(Ld):
    from types import SimpleNamespace
    L = SimpleNamespace(**Ld)
    P, A, PS, K = L.P, L.A, L.PS, L.K
    S, NT, NQ, NCMP, NNT, nsz, l = L.S, L.NT, L.NQ, L.NCMP, L.NNT, L.nsz, L.l
    fg, identb, identf, tri, onesf = L.fg, L.identb, L.identf, L.tri, L.onesf
    cmt, wmt, kmt, ovt = L.cmt, L.wmt, L.kmt, L.ovt
    qT_d, v_d, o_d, RQT, RV, RO, RG = L.qT_d, L.v_d, L.o_d, L.RQT, L.RV, L.RO, L.RG
    mm, tr, evac = L.mm, L.tr, L.evac
    t_swa_d, t_nsa_d = L.t_swa_d, L.t_nsa_d
    m_att = A.mark()
    mswa = A.alloc("mswa", [4, 256], BF16)
    mnsa = A.alloc("mnsa", [8, 1664], BF16)
    P.dma("sp", mswa[:, :, :], dap(t_swa_d, 127, [[DL_SWA - 1, 128], [128 * DL_SWA, 4], [1, 256]]), r=[RG], w=[mswa])
    for h in range(8):
        P.dma("sp", mnsa[:, h, :], dap(t_nsa_d, h * 128 * DL_NSA + 127, [[DL_NSA - 1, 128], [1, 1664]]),
              r=[RG], w=[mnsa])
    b31 = A.alloc("b31", [8], F32)
    P.add("dve", lambda e: e.tensor_copy(out=b31[:, :], in_=mnsa[:, :, 1663]), r=[mnsa], w=[b31])
    L.mswa, L.mnsa, L.b31 = mswa, mnsa, b31

    sg = A.alloc("sg", [NT, 24], F32)
    P.add("act", lambda e: e.activation(out=sg[:, :, :], in_=fg[:, :, 4:28], func=AF.Sigmoid), r=[fg], w=[sg])

    mF = A.mark()
    pF = PS.mark()
    m1 = A.mark()
    fbt = A.alloc("fbt", [4], F32)
    P.dma("sp", fbt[:, :], dap(L.forget_bias, l * 4, [[0, 128], [1, 4]]), w=[fbt])
    z = A.alloc("z", [NT, 4], F32)
    P.add("dve", lambda e: e.tensor_tensor(out=z[:, :, :], in0=fg[:, :, 0:4],
                                           in1=fbt[:, :].unsqueeze(1).to_broadcast([128, NT, 4]), op=ALU.add),
          r=[fg, fbt], w=[z])
    P.add("act", lambda e: e.activation(out=z[:, :, :], in_=z[:, :, :], func=AF.Exp, scale=-1.0), r=[z], w=[z])
    P.add("act", lambda e: e.activation(out=z[:, :, :], in_=z[:, :, :], func=AF.Ln, bias=1.0), r=[z], w=[z])
    psc = PS.alloc("psc", NT * 4)
    pst = PS.alloc("pst", NT * 4)
    zf = z[:, :, :].rearrange("p a b -> p (a b)")
    mm(psc, psc[:, :], tri, tri[:, :], z, zf, True, True)
    mm(pst, pst[:, :], onesf, onesf[:, :], z, zf, True, True)
    tot = A.alloc("tot", [NT, 4], F32)
    evac(tot, tot[:, :, :].rearrange("p a b -> p (a b)"), pst, pst[:, :])
    pre = A.alloc("pre", [NT, 4], F32)
    P.add("dve", lambda e: e.memset(pre[:, 0, :], 0.0), w=[pre])
    for k in range(1, NT):
        P.add("dve", lambda e, k=k: e.tensor_tensor(out=pre[:, k, :], in0=pre[:, k - 1, :], in1=tot[:, k - 1, :],
                                                    op=ALU.add), r=[pre, tot], w=[pre])
    Nn = A.alloc("Nn", [NT, 4], F32)
    P.add("dve", lambda e: e.tensor_tensor(out=Nn[:, :, :], in0=psc[:, :].rearrange("p (a b) -> p a b", b=4),
                                           in1=pre[:, :, :], op=ALU.add), r=[psc, pre], w=[Nn])
    R = A.alloc("R", [NT, 4, 6], BF16)
    r1 = A.alloc("r1", [NT, 4], F32)
    P.add("dve", lambda e: e.tensor_copy(out=R[:, :, :, 3], in_=Nn[:, :, :]), r=[Nn], w=[R])
    P.add("dve", lambda e: e.tensor_tensor(out=r1[:, :, :], in0=Nn[:, :, :], in1=R[:, :, :, 3], op=ALU.subtract),
          r=[Nn, R], w=[r1])
    P.add("dve", lambda e: e.tensor_copy(out=R[:, :, :, 4], in_=r1[:, :, :]), r=[r1], w=[R])
    P.add("dve", lambda e: e.tensor_tensor(out=r1[:, :, :], in0=r1[:, :, :], in1=R[:, :, :, 4], op=ALU.subtract),
          r=[r1, R], w=[r1])
    P.add("dve", lambda e: e.tensor_copy(out=R[:, :, :, 5], in_=r1[:, :, :]), r=[r1], w=[R])
    for r_ in range(3):
        P.add("dve", lambda e, r_=r_: e.tensor_scalar(out=R[:, :, :, r_], in0=R[:, :, :, 3 + r_], scalar1=-1.0,
                                                      scalar2=None, op0=ALU.mult), r=[R], w=[R])
    rowsT = A.alloc("rowsT", [S], BF16, parts=24)
    psR = [PS.alloc("psR%d" % k, 512, BF16, parts=24) for k in range(2)]
    for k0 in range(0, NT, 4):
        pr = psR[(k0 // 4) % 2]
        for k in range(k0, k0 + 4):
            tr(pr, pr[:, (k - k0) * 128:(k - k0 + 1) * 128], R, R[:, k, :, :].rearrange("p a b -> p (a b)"),
               identb, identb[:, :])
        evac(rowsT, rowsT[:, k0 * 128:(k0 + 4) * 128], pr, pr[:, :])
    PS.release(pF)
    qa = [A.alloc("qa%d" % k, [S], BF16) for k in range(2)]
    ka = [A.alloc("ka%d" % k, [S], BF16) for k in range(2)]
    vf = A.alloc("vf", [NT, 4, 65], BF16)
    for h in range(4):
        P.dma("sp", vf[:, :, h, 0:64], v_d[:, V_FOX + 64 * h:V_FOX + 64 * h + 64].rearrange("(k p) d -> p k d", p=128),
              r=[RV], w=[vf])
    P.add("dve", lambda e: e.memset(vf[:, :, :, 64:65], 1.0), w=[vf])
    rz = A.alloc("rz", [4], F32)
    ofs = [A.alloc("of%d" % k, [4, 64], F32) for k in range(2)]
    pts = [A.alloc("pt%d" % k, [512], BF16) for k in range(4)]
    pss = [PS.alloc("ps%d" % k, 512) for k in range(4)]
    accs = [PS.alloc("acc%d" % k, 512, parts=65) for k in range(2)]
    accN = PS.alloc("accN", 512)
    acnv = accN[:, :].rearrange("p (a b) -> p a b", a=4)
    oTs = A.alloc("oTs", [512], F32, parts=65)
    kcnt = [0]
    bg = list(L.conv_jobs.get(l, []))
    bgper = (len(bg) + 4 * NQ - 1) // (4 * NQ)

    def load_head(h):
        qaug, kaug = qa[h % 2], ka[h % 2]
        P.dma("sp", qaug[0:64, :], qT_d[QT_FOXQ + 64 * h:QT_FOXQ + 64 * h + 64, :], r=[RQT], w=[qaug])
        P.dma("sp", qaug[64:67, :], rowsT[6 * h:6 * h + 3, :], r=[rowsT], w=[qaug])
        P.dma("sp", qaug[67:70, :], K["k_ones"][0:3, :], w=[qaug])
        P.dma("sp", kaug[0:64, :], qT_d[QT_FOXK + 64 * h:QT_FOXK + 64 * h + 64, :], r=[RQT], w=[kaug])
        P.dma("sp", kaug[64:67, :], K["k_ones"][0:3, :], w=[kaug])
        P.dma("sp", kaug[67:70, :], rowsT[6 * h + 3:6 * h + 6, :], r=[rowsT], w=[kaug])

    load_head(0)
    for h in range(4):
        qaug, kaug = qa[h % 2], ka[h % 2]
        if h + 1 < 4:
            load_head(h + 1)
        units = [(i, j) for i in range(NQ) for j in range(4 * i + 4)]
        base = kcnt[0]
        kcnt[0] += len(units)

        def fa(k, units=units, base=base, qaug=qaug, kaug=kaug):
            i, j = units[k]
            bb0 = max(0, j - 4 * i)
            ps = pss[(base + k) % 4]
            diag = j >= 4 * i
            mm(ps, ps[:, bb0 * 128:512], kaug, kaug[0:70, j * 128:(j + 1) * 128],
               qaug, qaug[0:70, i * 512 + bb0 * 128:(i + 1) * 512], True, not diag)
            if diag:
                mm(ps, ps[:, bb0 * 128:512], identb, identb[:, :], cmt, cmt[:, 0:(4 - bb0) * 128], False, True)

        def fb(k, units=units, base=base):
            i, j = units[k]
            bb0 = max(0, j - 4 * i)
            ps = pss[(base + k) % 4]
            pt = pts[(base + k) % 4]
            P.add("act", lambda e: e.activation(out=pt[:, bb0 * 128:512], in_=ps[:, bb0 * 128:512], func=AF.Exp),
                  r=[ps], w=[pt])

        def fc(k, units=units, base=base, h=h):
            i, j = units[k]
            bb0 = max(0, j - 4 * i)
            pt = pts[(base + k) % 4]
            acc = accs[i % 2]
            mm(acc, acc[0:65, bb0 * 128:512], vf, vf[:, j, h, :], pt, pt[:, bb0 * 128:512], j == 0, j == 4 * i + 3)
            if j == 4 * i + 3:
                of = ofs[i % 2]
                for _ in range(bgper):
                    if bg:
                        bg.pop(0)()
                evac(oTs, oTs[0:65, :], acc, acc[0:65, :], eng="dve")
                for bb in range(4):
                    tr(accN, acnv[:, bb, 0:65], oTs, oTs[0:65, bb * 128:(bb + 1) * 128], identf, identf[0:65, 0:65])
                P.add("dve", lambda e: e.reciprocal(out=rz[:, :], in_=acnv[:, :, 64]), r=[accN], w=[rz])
                for bb in range(4):
                    P.add("dve", lambda e, bb=bb: e.tensor_scalar(out=of[:, bb, :], in0=acnv[:, bb, 0:64],
                                                                  scalar1=rz[:, bb:bb + 1], scalar2=None, op0=ALU.mult),
                          r=[accN, rz], w=[of])
                P.dma("sp", o_d[i * 512:(i + 1) * 512, 256 + 64 * h:256 + 64 * h + 64].rearrange("(s p) d -> p s d", p=128),
                      of[:, :, :], r=[of], w=[RO])

        pipeline(len(units), fa, fb, fc, la=3)
    while bg:
        bg.pop(0)()
    while L.pend:
        L.pend.pop(0)()
    A.release(mF)
    PS.release(pF)

    mS = A.mark()
    pS = PS.mark()
    qs = A.alloc("qs", [2, S], BF16)
    P.dma("sp", qs[0:64, :, :], qT_d[QT_SWAQ:QT_SWAQ + 128, :].rearrange("(g d) t -> d g t", g=2), r=[RQT], w=[qs])
    P.dma("sp", qs[64:128, :, :], qT_d[QT_SWAQ + 128:QT_SWAQ + 256, :].rearrange("(g d) t -> d g t", g=2), r=[RQT], w=[qs])
    ksw = A.alloc("ksw", [S], BF16)
    P.dma("sp", ksw[:, :], qT_d[QT_SWAK:QT_SWAK + 128, :], r=[RQT], w=[ksw])
    vsw = A.alloc("vsw", [NT, 2, 65], BF16)
    for kv in range(2):
        P.dma("sp", vsw[:, :, kv, 0:64], v_d[:, V_SWA + 64 * kv:V_SWA + 64 * kv + 64].rearrange("(k p) d -> p k d", p=128),
              r=[RV], w=[vsw])
    P.add("dve", lambda e: e.memset(vsw[:, :, :, 64:65], 1.0), w=[vsw])
    es = A.alloc("es", [4], F32)
    P.dma("sp", es[:, :], dap(L.swa_sinks, l * 4, [[0, 128], [1, 4]]), w=[es])
    P.add("act", lambda e: e.activation(out=es[:, :], in_=es[:, :], func=AF.Exp), r=[es], w=[es])
    pts = [A.alloc("spt%d" % k, [256], BF16) for k in range(4)]
    pss = [PS.alloc("sps%d" % k, 256) for k in range(4)]
    accs = [PS.alloc("sacc%d" % k, 65) for k in range(4)]
    osw = [A.alloc("osw%d" % k, [4, 64], F32) for k in range(2)]
    zz = A.alloc("zz", [4], F32)
    units = [(ti, h) for ti in range(NT) for h in range(4)]

    def fa(k):
        ti, h = units[k]
        kv, g = h // 2, h % 2
        ps = pss[k % 4]
        rows = slice(kv * 64, kv * 64 + 64)
        if ti > 0:
            mm(ps, ps[:, 0:128], ksw, ksw[rows, (ti - 1) * 128:ti * 128], qs, qs[rows, g, ti * 128:(ti + 1) * 128], True, False)
            mm(ps, ps[:, 0:128], identb, identb[:, :], mswa, mswa[:, h, 128:256], False, True)
        mm(ps, ps[:, 128:256], ksw, ksw[rows, ti * 128:(ti + 1) * 128], qs, qs[rows, g, ti * 128:(ti + 1) * 128], True, False)
        mm(ps, ps[:, 128:256], identb, identb[:, :], mswa, mswa[:, h, 0:128], False, True)

    def fb(k):
        ti, h = units[k]
        ps, pt = pss[k % 4], pts[k % 4]
        c0 = 0 if ti > 0 else 128
        P.add("act", lambda e: e.activation(out=pt[:, c0:256], in_=ps[:, c0:256], func=AF.Exp), r=[ps], w=[pt])

    def fc(k):
        ti, h = units[k]
        kv = h // 2
        pt, acc = pts[k % 4], accs[k % 4]
        if ti > 0:
            mm(acc, acc[:, 0:65], pt, pt[:, 0:128], vsw, vsw[:, ti - 1, kv, :], True, False)
        mm(acc, acc[:, 0:65], pt, pt[:, 128:256], vsw, vsw[:, ti, kv, :], ti == 0, True)
        ow = osw[ti % 2]
        P.add("dve", lambda e: e.tensor_tensor(out=zz[:, h:h + 1], in0=acc[:, 64:65], in1=es[:, h:h + 1], op=ALU.add),
              r=[acc, es], w=[zz])
        P.add("dve", lambda e: e.reciprocal(out=zz[:, h:h + 1], in_=zz[:, h:h + 1]), r=[zz], w=[zz])
        P.add("dve", lambda e: e.tensor_scalar(out=ow[:, h, :], in0=acc[:, 0:64], scalar1=zz[:, h:h + 1], scalar2=None,
                                               op0=ALU.mult), r=[acc, zz], w=[ow])
        if h == 3:
            P.dma("sp", o_d[ti * 128:(ti + 1) * 128, 0:256], ow[:, :, :].rearrange("p a b -> p (a b)"), r=[ow], w=[RO])

    pipeline(len(units), fa, fb, fc, la=3)
    A.release(mS)
    PS.release(pS)
    build_nsa(L, sg)
    A.release(m_att)


def build_nsa(L, sg):
    P, A, PS, K = L.P, L.A, L.PS, L.K
    S, NT, NQ, NCMP, NNT, nsz, l = L.S, L.NT, L.NQ, L.NCMP, L.NNT, L.nsz, L.l
    identb, identf = L.identb, L.identf
    wmt, kmt, ovt, mnsa, b31 = L.wmt, L.kmt, L.ovt, L.mnsa, L.b31
    qT_d, v_d, o_d, RQT, RV, RO, RG = L.qT_d, L.v_d, L.o_d, L.RQT, L.RV, L.RO, L.RG
    mm, tr, evac = L.mm, L.tr, L.evac
    mN = A.mark()
    pN = PS.mark()
    kcmpT = A.alloc("kcmpT", [2, 256], BF16, parts=64)
    Rg = A.alloc("Rg", [NNT, 2, 128], F32)
    for nt in range(NNT):
        for g in range(2):
            P.dma("sp", Rg[0:nsz[nt], nt, g, 64:128], K["k_ovl"][nt * 128:nt * 128 + nsz[nt], :], w=[Rg])
    m0 = A.mark()
    p0 = PS.mark()
    xTs = [A.alloc("cxT%d" % k, [S], BF16, parts=64) for k in range(2)]
    w1s = [A.alloc("cw1%d" % k, [32, 128], BF16, parts=64) for k in range(2)]
    pes = [A.alloc("cpe%d" % k, [32], BF16, parts=64) for k in range(2)]
    w2s = [A.alloc("cw2%d" % k, [64], BF16) for k in range(2)]
    w1f = A.alloc("cw1f", [32, 128], F32, parts=64)
    pef = A.alloc("cpef", [32], F32, parts=64)
    w2f = A.alloc("cw2f", [64], F32)
    bsb = [A.alloc("cbias%d" % k, [1], F32) for k in range(2)]
    Gts = [A.alloc("cG%d" % k, [256], BF16) for k in range(2)]
    psG = [PS.alloc("psG%d" % k, 256) for k in range(2)]
    psb = [PS.alloc("psb%d" % k, 1) for k in range(2)]
    psK = PS.alloc("psK", 256, parts=64)
    psV = [PS.alloc("psV%d" % k, 64) for k in range(2)]
    kk = 0
    for which in range(2):
        w1, pe, w2 = w1s[which], pes[which], w2s[which]
        P.dma("sp", w1f[:, :, :], L.cmp_w1[l, which].rearrange("(l d) j -> d l j", d=64), w=[w1f])
        P.add("pool", lambda e, w1=w1: e.tensor_copy(out=w1[:, :, :], in_=w1f[:, :, :]), r=[w1f], w=[w1])
        P.dma("sp", pef[:, :], L.cmp_pos[l, which].rearrange("l d -> d l"), w=[pef], slow=True)
        P.add("pool", lambda e, pe=pe: e.tensor_copy(out=pe[:, :], in_=pef[:, :]), r=[pef], w=[pe])
        P.dma("sp", w2f[:, :], L.cmp_w2[l, which], w=[w2f])
        P.add("pool", lambda e, w2=w2: e.tensor_copy(out=w2[:, :], in_=w2f[:, :]), r=[w2f], w=[w2])
        pb_, bs = psb[which], bsb[which]
        for ll in range(32):
            mm(pb_, pb_[:, 0:1], w1, w1[:, ll, :], pe, pe[:, ll:ll + 1], ll == 0, ll == 31)
        evac(bs, bs[:, :], pb_, pb_[:, 0:1])
        for g in range(2):
            xT, pG, Gt = xTs[kk % 2], psG[kk % 2], Gts[kk % 2]
            kk += 1
            row = (QT_KC if which == 0 else QT_VC) + 64 * g
            P.dma("sp", xT[:, :], qT_d[row:row + 64, :], r=[RQT], w=[xT])
            for ll in range(32):
                mm(pG, pG[:, 0:NCMP], w1, w1[:, ll, :], xT, xT[:, ll:ll + 16 * (NCMP - 1) + 1:16], ll == 0, ll == 31)
            P.add("act", lambda e, Gt=Gt, pG=pG, bs=bs: e.activation(out=Gt[:, 0:NCMP], in_=pG[:, 0:NCMP],
                                                                     func=AF.Gelu_apprx_tanh, bias=bs[:, 0:1]),
                  r=[pG, bs], w=[Gt])
            if which == 0:
                mm(psK, psK[:, 0:NCMP], w2, w2[:, :], Gt, Gt[:, 0:NCMP], True, True)
                evac(kcmpT, kcmpT[:, g, 0:NCMP], psK, psK[:, 0:NCMP])
            else:
                for nt in range(NNT):
                    pV = psV[nt % 2]
                    mm(pV, pV[0:nsz[nt], 0:64], Gt, Gt[:, nt * 128:nt * 128 + nsz[nt]], w2, w2[:, :], True, True)
                    evac(Rg, Rg[0:nsz[nt], nt, g, 0:64], pV, pV[0:nsz[nt], 0:64])
    A.release(m0)
    PS.release(p0)

    kaug = [A.alloc("kaug%d" % g, [S], BF16) for g in range(2)]
    kwT = A.alloc("kwT", [2, S], BF16, parts=64)
    vsl = A.alloc("vsl", [NT, 2, 65], BF16)
    vwn = A.alloc("vwn", [NT, 2, 65], BF16)
    for g in range(2):
        P.dma("sp", kaug[g][0:64, :], qT_d[QT_KS + 64 * g:QT_KS + 64 * g + 64, :], r=[RQT], w=[kaug[g]])
        P.dma("sp", kaug[g][64:128, :], K["k_sel"], w=[kaug[g]])
        P.dma("sp", kwT[:, g, :], qT_d[QT_KW + 64 * g:QT_KW + 64 * g + 64, :], r=[RQT], w=[kwT])
        P.dma("sp", vsl[:, :, g, 0:64], v_d[:, V_S + 64 * g:V_S + 64 * g + 64].rearrange("(k p) d -> p k d", p=128),
              r=[RV], w=[vsl])
        P.dma("sp", vwn[:, :, g, 0:64], v_d[:, V_W + 64 * g:V_W + 64 * g + 64].rearrange("(k p) d -> p k d", p=128),
              r=[RV], w=[vwn])
    P.add("dve", lambda e: e.memset(vsl[:, :, :, 64:65], 1.0), w=[vsl])
    P.add("dve", lambda e: e.memset(vwn[:, :, :, 64:65], 1.0), w=[vwn])
    qaugs = [[A.alloc("qaug%d_%d" % (k, h), [512], BF16) for h in range(8)] for k in range(2)]
    Tcs = [A.alloc("Tc%d" % nt, [8, 512], BF16) for nt in range(NNT)]
    pcs = [A.alloc("pc%d" % k, [512], F32) for k in range(3)]
    pts = [A.alloc("npt%d" % k, [512], BF16) for k in range(3)]
    imp = A.alloc("imp", [4, 64], F32)
    wk = A.alloc("wk", [64], F32)
    m8 = A.alloc("m8", [16], F32)
    nm = A.alloc("nm", [4, 128], BF16)
    zc = A.alloc("zc", [4], F32)
    coef = A.alloc("coef", [4], F32)
    onsa = [A.alloc("onsa%d" % k, [4, 512], F32) for k in range(2)]
    pss = [PS.alloc("nps%d" % k, 512) for k in range(3)]
    acccs = [PS.alloc("accc%d" % k, 512) for k in range(2)]
    accs = [PS.alloc("nacc%d" % k, 512, parts=65) for k in range(2)]
    pxx = PS.alloc("pxx", 512)
    psM = Tile("psMv", pxx[:, :].bitcast(BF16)[:, 0:512])
    acnv = pxx[:, :].rearrange("p (a b) -> p a b", a=4)
    oTs = A.alloc("noTs", [512], F32, parts=65)
    kc = [0]
    ac = [0]
    for i in range(NQ):
        qg = qaugs[i % 2]
        on = onsa[i % 2]
        for h in range(8):
            P.dma("sp", qg[h][0:64, :], qT_d[QT_NSAQ + 64 * h:QT_NSAQ + 64 * h + 64, i * 512:(i + 1) * 512], r=[RQT], w=[qg[h]])
        nts = [nt for nt in range(NNT) if 512 * (i + 1) - 1 >= 16 * 128 * nt + 31]
        for nt in nts:
            u0 = 512 * i - 2048 * nt
            assert u0 >= 0
            P.dma("sp", Tcs[nt][:, :, :], dap(L.t_cmp_d, u0 + 2032, [[DL_CMP - 16, 128], [128 * DL_CMP, 8], [1, 512]]),
                  r=[RG], w=[Tcs[nt]])
        for g in range(2):
            units = [(hh, nt) for hh in range(4) for nt in nts]
            base = kc[0]
            kc[0] += len(units)

            def fa(k, units=units, base=base, g=g, qg=qg):
                hh, nt = units[k]
                h = 4 * g + hh
                ps = pss[(base + k) % 3]
                n = nsz[nt]
                mm(ps, ps[0:n, :], kcmpT, kcmpT[0:64, g, nt * 128:nt * 128 + n], qg[h], qg[h][0:64, :], True, False)
                mm(ps, ps[0:n, :], identb, identb[0:n, 0:n], Tcs[nt], Tcs[nt][0:n, h, :], False, True)

            def fb(k, units=units, base=base):
                hh, nt = units[k]
                ps, pc = pss[(base + k) % 3], pcs[(base + k) % 3]
                n = nsz[nt]
                P.add("act", lambda e: e.activation(out=pc[0:n, :], in_=ps[0:n, :], func=AF.Exp), r=[ps], w=[pc])

            def fc(k, units=units, base=base, g=g, i=i, on=on):
                hh, nt = units[k]
                h = 4 * g + hh
                pc = pcs[(base + k) % 3]
                n = nsz[nt]
                accc = acccs[hh % 2]
                avc = accc[:, :].rearrange("p (a b) -> p a b", a=4)
                for bb in range(4):
                    mm(accc, avc[:, bb, :], pc, pc[0:n, bb * 128:(bb + 1) * 128], Rg, Rg[0:n, nt, g, :],
                       nt == nts[0] and bb == 0, nt == nts[-1] and bb == 3)
                if nt == nts[-1]:
                    P.add("dve", lambda e: e.reduce_sum(out=zc[:, :], in_=avc[:, :, 64:128], axis=mybir.AxisListType.X),
                          r=[accc], w=[zc])
                    P.add("dve", lambda e: e.tensor_scalar(out=zc[:, :], in0=zc[:, :], scalar1=1e-30, scalar2=None,
                                                           op0=ALU.max), r=[zc], w=[zc])
                    P.add("dve", lambda e: e.reciprocal(out=zc[:, :], in_=zc[:, :]), r=[zc], w=[zc])
                    P.add("dve", lambda e: e.tensor_tensor(out=coef[:, :], in0=zc[:, :], in1=sg[:, 4 * i:4 * i + 4, 3 * h],
                                                           op=ALU.mult), r=[zc, sg], w=[coef])
                    for bb in range(4):
                        P.add("dve", lambda e, bb=bb: e.tensor_scalar(
                            out=on[:, bb, 64 * h:64 * h + 64], in0=avc[:, bb, 0:64], scalar1=coef[:, bb:bb + 1],
                            scalar2=None, op0=ALU.mult), r=[accc, coef], w=[on])
                        if hh == 0:
                            P.add("dve", lambda e, bb=bb: e.tensor_scalar(
                                out=imp[:, bb, :], in0=avc[:, bb, 64:128], scalar1=zc[:, bb:bb + 1], scalar2=None,
                                op0=ALU.mult), r=[accc, zc], w=[imp])
                        else:
                            P.add("dve", lambda e, bb=bb: e.scalar_tensor_tensor(
                                out=imp[:, bb, :], in0=avc[:, bb, 64:128], scalar=zc[:, bb:bb + 1], in1=imp[:, bb, :],
                                op0=ALU.mult, op1=ALU.add), r=[accc, zc, imp], w=[imp])

            pipeline(len(units), fa, fb, fc, la=2)
            for bb in range(4):
                ti = 4 * i + bb
                w0 = 63 - 2 * ti
                P.add("dve", lambda e, bb=bb, w0=w0: e.tensor_tensor(out=imp[:, bb, :], in0=imp[:, bb, :],
                                                                   in1=kmt[:, w0:w0 + 64], op=ALU.mult), r=[imp, kmt], w=[imp])
                P.add("dve", lambda e, bb=bb, w0=w0: e.tensor_tensor(out=imp[:, bb, :], in0=imp[:, bb, :],
                                                                   in1=ovt[:, w0:w0 + 64], op=ALU.add), r=[imp, ovt], w=[imp])
                P.add("dve", lambda e, bb=bb: e.memset(imp[:, bb, 0:1], 1e30), r=[imp], w=[imp])
                P.add("dve", lambda e, bb=bb: e.max(out=m8[:, 0:8], in_=imp[:, bb, :]), r=[imp], w=[m8])
                P.add("dve", lambda e, bb=bb: e.match_replace(out=wk[:, :], in_to_replace=m8[:, 0:8], in_values=imp[:, bb, :],
                                                              imm_value=-3e38), r=[imp, m8], w=[wk])
                P.add("dve", lambda e: e.max(out=m8[:, 8:16], in_=wk[:, :]), r=[wk, m8], w=[m8])
                P.add("dve", lambda e, bb=bb: e.tensor_scalar(out=nm[:, bb, 0:64], in0=imp[:, bb, :], scalar1=m8[:, 15:16],
                                                              scalar2=NEG, op0=ALU.is_lt, op1=ALU.mult), r=[imp, m8], w=[nm])
                P.add("dve", lambda e, bb=bb: e.tensor_copy(out=nm[:, bb, 64:128], in_=nm[:, bb, 0:64]), r=[nm], w=[nm])

            def mask_rows(g=g, qg=qg):
                for bb in range(4):
                    tr(pxx, psM[:, bb * 128:(bb + 1) * 128], nm, nm[:, bb, :], identb, identb[:, :])
                for hh in range(4):
                    qh = qg[4 * g + hh]
                    evac(qh, qh[64:128, :], pxx, psM[64:128, :], eng="dve")

            for br in (1, 0):
                if br == 0:
                    mask_rows()
                if br == 0:
                    js = list(range(4 * i + 4))
                else:
                    js = list(range(max(0, 4 * i - 4), 4 * i + 4))
                units = [(hh, j) for hh in range(4) for j in js]
                base = kc[0]
                kc[0] += len(units)
                abase = ac[0]
                ac[0] += 4
                jfirst = js[0]

                def rng(j, br=br, i=i):
                    if br == 0:
                        return max(0, j - 4 * i), 4
                    if j == max(0, 4 * i - 4):
                        return 0, 4
                    bbs = [bb for bb in range(4) if 0 <= 4 * i + bb - j <= 4]
                    return bbs[0], bbs[-1] + 1

                def fa(k, units=units, base=base, g=g, qg=qg, br=br, i=i, rng=rng):
                    hh, j = units[k]
                    h = 4 * g + hh
                    qh = qg[h]
                    bb0, bb1 = rng(j)
                    ps = pss[(base + k) % 3]
                    ucol = 128 * (4 * i + bb0 - j)
                    wdt = (bb1 - bb0) * 128
                    if br == 0:
                        near = (4 * i - j) <= 9
                        mm(ps, ps[:, bb0 * 128:bb1 * 128], kaug[g], kaug[g][:, j * 128:(j + 1) * 128],
                           qh, qh[:, bb0 * 128:bb1 * 128], True, not near)
                        if near:
                            mm(ps, ps[:, bb0 * 128:bb1 * 128], identb, identb[:, :], mnsa, mnsa[:, h, ucol:ucol + wdt],
                               False, True)
                    else:
                        needw = ucol + wdt > 512
                        mm(ps, ps[:, bb0 * 128:bb1 * 128], kwT, kwT[0:64, g, j * 128:(j + 1) * 128],
                           qh, qh[0:64, bb0 * 128:bb1 * 128], True, False)
                        mm(ps, ps[:, bb0 * 128:bb1 * 128], identb, identb[:, :], mnsa, mnsa[:, h, ucol:ucol + wdt],
                           False, not needw)
                        if needw:
                            if j == max(0, 4 * i - 4):
                                mm(ps, ps[:, bb0 * 128:bb1 * 128], identb, identb[:, :], wmt, wmt[:, ucol:ucol + wdt],
                                   False, True)
                            else:
                                bw = j - 4 * i + 4
                                mm(ps, ps[:, bw * 128:(bw + 1) * 128], identb, identb[:, :], wmt, wmt[:, 512:640], False, True)

                def fb(k, units=units, base=base, g=g, br=br, i=i, rng=rng):
                    hh, j = units[k]
                    h = 4 * g + hh
                    bb0, bb1 = rng(j)
                    ps, pt = pss[(base + k) % 3], pts[(base + k) % 3]
                    if br == 0 and (4 * i - j) > 9:
                        P.add("act", lambda e: e.activation(out=pt[:, bb0 * 128:bb1 * 128], in_=ps[:, bb0 * 128:bb1 * 128],
                                                            func=AF.Exp, bias=b31[:, h:h + 1]), r=[ps, b31], w=[pt])
                    else:
                        P.add("act", lambda e: e.activation(out=pt[:, bb0 * 128:bb1 * 128], in_=ps[:, bb0 * 128:bb1 * 128],
                                                            func=AF.Exp), r=[ps], w=[pt])

                def fc(k, units=units, base=base, abase=abase, g=g, br=br, i=i, rng=rng, jfirst=jfirst, on=on):
                    hh, j = units[k]
                    h = 4 * g + hh
                    bb0, bb1 = rng(j)
                    pt = pts[(base + k) % 3]
                    acc = accs[(abase + hh) % 2]
                    acv = acnv
                    vt = vsl if br == 0 else vwn
                    mm(acc, acc[0:65, bb0 * 128:bb1 * 128], vt, vt[:, j, g, :], pt, pt[:, bb0 * 128:bb1 * 128],
                       j == jfirst, j == 4 * i + 3)
                    for dk in [d for d in list(deferred) if d[0] <= k]:
                        deferred.remove(dk)
                        dk[1]()
                    if j == 4 * i + 3:
                        evac(oTs, oTs[0:65, :], acc, acc[0:65, :], eng="dve")

                        def fin(h=h, br=br, i=i, on=on):
                            for bb in range(4):
                                tr(pxx, acv[:, bb, 0:65], oTs, oTs[0:65, bb * 128:(bb + 1) * 128], identf, identf[0:65, 0:65])
                            P.add("dve", lambda e: e.reciprocal(out=zc[:, :], in_=acv[:, :, 64]), r=[pxx], w=[zc])
                            P.add("dve", lambda e: e.tensor_tensor(out=coef[:, :], in0=zc[:, :],
                                                                   in1=sg[:, 4 * i:4 * i + 4, 3 * h + 1 + br], op=ALU.mult),
                                  r=[zc, sg], w=[coef])
                            for bb in range(4):
                                P.add("dve", lambda e, bb=bb: e.scalar_tensor_tensor(
                                    out=on[:, bb, 64 * h:64 * h + 64], in0=acv[:, bb, 0:64], scalar=coef[:, bb:bb + 1],
                                    in1=on[:, bb, 64 * h:64 * h + 64], op0=ALU.mult, op1=ALU.add), r=[pxx, coef, on], w=[on])
                        deferred.append((k + 3, fin))

                deferred = []
                pipeline(len(units), fa, fb, fc, la=2)
                for dk in deferred:
                    dk[1]()
        P.dma("sp", o_d[i * 512:(i + 1) * 512, 512:1024].rearrange("(s p) d -> p s d", p=128), on[:, :, :], r=[on], w=[RO])
    A.release(mN)
    PS.release(pN)


def build_ffn(Ld):
    from types import SimpleNamespace
    L = SimpleNamespace(**Ld)
    P, A, PS = L.P, L.A, L.PS
    S, NT, NQ, l = L.S, L.NT, L.NQ, L.l
    mod, gn, identb = L.mod, L.gn, L.identb
    o_d, RO, RX, RW = L.o_d, L.RO, L.RX, L.RW
    mm, tr, evac = L.mm, L.tr, L.evac
    xsrc, out = L.xsrc, L.out
    mE = A.mark()
    pE = PS.mark()
    wo = A.alloc("wo", [8, D], BF16)
    P.dma("sp", wo[:, :, :], L.wo_d[l], r=[RW], w=[wo])
    ots = [A.alloc("ot%d" % k, [D], F32) for k in range(2)]
    xts = [A.alloc("fx%d" % k, [D], F32) for k in range(2)]
    x1s = [A.alloc("x1_%d" % k, [4, D], F32) for k in range(2)]
    mxb = A.alloc("mxb", [D], BF16)
    mxT = A.alloc("mxT", [8, 128], BF16)
    hb = A.alloc("fhb", [4, D], BF16)
    hT = A.alloc("fhT", [8, 512], BF16)
    tmp = A.alloc("ftmp", [D], F32)
    junk = A.alloc("fjunk", [D], BF16)
    st = A.alloc("fst", [8], F32)
    actT = A.alloc("actT", [22, 512], BF16)
    sil = [A.alloc("sil%d" % k, [512], F32) for k in range(2)]
    wgus = [A.alloc("wgu%d" % k, [2, 8, 128], BF16) for k in range(4)]
    wds = [A.alloc("wd%d" % k, [2, D], BF16) for k in range(3)]
    xo = [A.alloc("xo%d" % k, [D], F32) for k in range(2)]
    ptr = [PS.alloc("fptr%d" % k, 512) for k in range(2)]
    pg = [PS.alloc("pg%d" % k, 512) for k in range(2)]

    def bfv(p_):
        return p_[:, :].bitcast(BF16)[:, 0:512]

    pu = [PS.alloc("pu%d" % k, 512) for k in range(2)]
    py = [PS.alloc("py%d" % k, 512) for k in range(2)]
    kt = [0]
    kw = [0]

    def rstd_from(ss_ap, ss_t, n):
        P.add("dve", lambda e: e.tensor_scalar(out=ss_ap, in0=ss_ap, scalar1=1.0 / n, scalar2=1e-6, op0=ALU.mult, op1=ALU.add),
              r=[ss_t], w=[ss_t])
        P.add("act", lambda e: e.activation(out=ss_ap, in_=ss_ap, func=AF.Sqrt), r=[ss_t], w=[ss_t])
        P.add("dve", lambda e: e.reciprocal(out=ss_ap, in_=ss_ap), r=[ss_t], w=[ss_t])

    def dprime(tb, s_):
        t0 = tb * 512
        x1 = x1s[tb % 2]
        r0 = t0 + s_ * 128
        ot, xt = ots[s_ % 2], xts[s_ % 2]
        P.dma("sp", ot[:, :], o_d[r0:r0 + 128, :], r=[RO], w=[ot])
        P.dma("sp", xt[:, :], xsrc[r0:r0 + 128, :], r=[RX[tb]], w=[xt])
        P.add("dve", lambda e: e.memset(st[:, :], 0.0), w=[st])
        for gi, (c0, c1) in enumerate(((0, 256), (256, 512), (512, 1024))):
            P.add("act", lambda e, ot=ot, gi=gi, c0=c0, c1=c1: e.activation(
                out=junk[:, c0:c1], in_=ot[:, c0:c1], func=AF.Square, accum_out=st[:, gi:gi + 1]), r=[ot, st], w=[junk, st])
        P.add("dve", lambda e: e.tensor_scalar(out=st[:, 0:2], in0=st[:, 0:2], scalar1=1.0 / 256, scalar2=1e-6,
                                               op0=ALU.mult, op1=ALU.add), r=[st], w=[st])
        P.add("dve", lambda e: e.tensor_scalar(out=st[:, 2:3], in0=st[:, 2:3], scalar1=1.0 / 512, scalar2=1e-6,
                                               op0=ALU.mult, op1=ALU.add), r=[st], w=[st])
        P.add("act", lambda e: e.activation(out=st[:, 0:3], in_=st[:, 0:3], func=AF.Sqrt), r=[st], w=[st])
        P.add("dve", lambda e: e.reciprocal(out=st[:, 0:3], in_=st[:, 0:3]), r=[st], w=[st])
        for gi, (c0, c1) in enumerate(((0, 256), (256, 512), (512, 1024))):
            P.add("dve", lambda e, ot=ot, gi=gi, c0=c0, c1=c1: e.scalar_tensor_tensor(
                out=mxb[:, c0:c1], in0=ot[:, c0:c1], scalar=st[:, gi:gi + 1], in1=gn[:, c0:c1],
                op0=ALU.mult, op1=ALU.mult), r=[ot, st, gn], w=[mxb])
        for c in range(8):
            pt = ptr[kt[0] % 2]
            tr(pt, bfv(pt)[:, (c % 4) * 128:(c % 4 + 1) * 128], mxb, mxb[:, c * 128:(c + 1) * 128], identb, identb[:, :])
            if c % 4 == 3:
                evac(mxT, mxT[:, c - 3:c + 1, :].rearrange("p a b -> p (a b)"), pt, bfv(pt))
                kt[0] += 1
        for nh in range(2):
            p_ = py[nh]
            for c in range(8):
                mm(p_, p_[:, :], mxT, mxT[:, c, :], wo, wo[:, c, nh * 512:(nh + 1) * 512], c == 0, c == 7)
        P.add("dve", lambda e: e.memset(st[:, 4:6], 0.0), w=[st])
        for nh in range(2):
            P.add("act", lambda e, nh=nh: e.activation(out=junk[:, nh * 512:(nh + 1) * 512], in_=py[nh][:, :], func=AF.Square,
                                                       accum_out=st[:, 4 + nh:5 + nh]), r=[py[nh], st], w=[junk, st])
        P.add("dve", lambda e: e.tensor_tensor(out=st[:, 6:7], in0=st[:, 4:5], in1=st[:, 5:6], op=ALU.add), r=[st], w=[st])
        rstd_from(st[:, 6:7], st, D)
        for nh in range(2):
            P.add("dve", lambda e, nh=nh: e.scalar_tensor_tensor(
                out=tmp[:, nh * 512:(nh + 1) * 512], in0=py[nh][:, :], scalar=st[:, 6:7], in1=mod[:, 2, nh * 512:(nh + 1) * 512],
                op0=ALU.mult, op1=ALU.mult), r=[py[nh], st, mod], w=[tmp])
        P.add("dve", lambda e, xt=xt, s_=s_: e.tensor_tensor(out=x1[:, s_, :], in0=tmp[:, :], in1=xt[:, :], op=ALU.add),
              r=[tmp, xt], w=[x1])
        P.add("dve", lambda e: e.memset(st[:, 7:8], 0.0), w=[st])
        P.add("act", lambda e, s_=s_: e.activation(out=junk[:, :], in_=x1[:, s_, :], func=AF.Square, accum_out=st[:, 7:8]),
              r=[x1, st], w=[junk, st])
        rstd_from(st[:, 7:8], st, D)
        P.add("dve", lambda e, s_=s_: e.scalar_tensor_tensor(out=tmp[:, :], in0=x1[:, s_, :], scalar=st[:, 7:8],
                                                            in1=mod[:, 4, :], op0=ALU.mult, op1=ALU.mult),
              r=[x1, st, mod], w=[tmp])
        P.add("dve", lambda e, s_=s_: e.tensor_tensor(out=hb[:, s_, :], in0=tmp[:, :], in1=mod[:, 3, :], op=ALU.add),
              r=[tmp, mod], w=[hb])

    nxt = []
    nper = (len(nxt) + NQ * 11 - 1) // (NQ * 11) if nxt else 0
    for s_ in range(4):
        dprime(0, s_)
    for tb in range(NQ):
        t0 = tb * 512
        x1 = x1s[tb % 2]
        if L.debug and l == 0:
            P.dma("sp", L.dbg["x1"][t0:t0 + 512, :].rearrange("(s p) d -> p s d", p=128), x1[:, :, :], r=[x1])
        for c in range(8):
            pt = ptr[kt[0] % 2]
            kt[0] += 1
            for s_ in range(4):
                tr(pt, bfv(pt)[:, s_ * 128:(s_ + 1) * 128], hb, hb[:, s_, c * 128:(c + 1) * 128], identb, identb[:, :])
            evac(hT, hT[:, c, :], pt, bfv(pt))
        for hc in range(22):
            wgu = wgus[kw[0] % 4]
            kw[0] += 1
            P.dma("sp", wgu[:, :, :, :], L.wgu_d[l, hc], r=[RW], w=[wgu])
            pg_, pu_, sl = pg[hc % 2], pu[hc % 2], sil[hc % 2]
            for c in range(8):
                mm(pg_, pg_[:, :], wgu, wgu[:, 0, c, :], hT, hT[:, c, :], c == 0, c == 7)
            for c in range(8):
                mm(pu_, pu_[:, :], wgu, wgu[:, 1, c, :], hT, hT[:, c, :], c == 0, c == 7)
            P.add("act", lambda e, pg_=pg_, sl=sl: e.activation(out=sl[:, :], in_=pg_[:, :], func=AF.Silu), r=[pg_], w=[sl])
            P.add("dve", lambda e, pu_=pu_, sl=sl, hc=hc: e.tensor_tensor(out=actT[:, hc, :], in0=sl[:, :], in1=pu_[:, :],
                                                                        op=ALU.mult), r=[sl, pu_], w=[actT])
            if tb + 1 < NQ and hc in (3, 8, 13, 18):
                dprime(tb + 1, (hc - 3) // 5)
            if hc % 2 == 1:
                for _ in range(nper):
                    if nxt:
                        nxt.pop(0)("act")
        yps = [py[0], py[1], pg[0], pg[1], pu[0], pu[1], ptr[0], ptr[1]]

        def ybank(p_):
            return p_[:, :]

        for hc in range(22):
            wd_ = wds[(hc // 2) % 3]
            if hc % 2 == 0:
                P.dma("sp", wd_[:, :, :], L.wd_d[l, :, hc:hc + 2, :], r=[RW], w=[wd_])
            for s_ in range(4):
                for nh in range(2):
                    p_ = yps[s_ * 2 + nh]
                    mm(p_, ybank(p_), actT, actT[:, hc, s_ * 128:(s_ + 1) * 128], wd_, wd_[:, hc % 2, nh * 512:(nh + 1) * 512],
                       hc == 0, hc == 21)
        for s_ in range(4):
            r0 = t0 + s_ * 128
            xo_ = xo[s_ % 2]
            P.add("dve", lambda e: e.memset(st[:, 4:6], 0.0), w=[st])
            for nh in range(2):
                p_ = yps[s_ * 2 + nh]
                P.add("act", lambda e, nh=nh, p_=p_: e.activation(out=junk[:, nh * 512:(nh + 1) * 512], in_=ybank(p_), func=AF.Square,
                                                                 accum_out=st[:, 4 + nh:5 + nh]), r=[p_, st], w=[junk, st])
            P.add("dve", lambda e: e.tensor_tensor(out=st[:, 6:7], in0=st[:, 4:5], in1=st[:, 5:6], op=ALU.add), r=[st], w=[st])
            rstd_from(st[:, 6:7], st, D)
            for nh in range(2):
                p_ = yps[s_ * 2 + nh]
                P.add("dve", lambda e, nh=nh, p_=p_: e.scalar_tensor_tensor(
                    out=tmp[:, nh * 512:(nh + 1) * 512], in0=ybank(p_), scalar=st[:, 6:7], in1=mod[:, 5, nh * 512:(nh + 1) * 512],
                    op0=ALU.mult, op1=ALU.mult), r=[p_, st, mod], w=[tmp])
            P.add("dve", lambda e, s_=s_, xo_=xo_, x1=x1: e.tensor_tensor(out=xo_[:, :], in0=tmp[:, :], in1=x1[:, s_, :], op=ALU.add),
                  r=[tmp, x1], w=[xo_])
            P.dma("act", out[r0:r0 + 128, :], xo_[:, :], r=[xo_], w=[RX[tb]])
    while nxt:
        nxt.pop(0)("act")
    A.release(mE)
    PS.release(pE)
```

```python
import math
import numpy as np
import ml_dtypes
import concourse.bass as bass
import concourse.mybir as mybir
from concourse.bass_utils import run_bass_kernel_spmd

F32 = mybir.dt.float32
BF16 = mybir.dt.bfloat16
U8 = mybir.dt.uint8
AF = mybir.ActivationFunctionType
ALU = mybir.AluOpType

D = 1024
HD = 64
FFN = 2816
NIN = 2588
NEG = -30000.0
DSZ = {F32: 4, BF16: 2, U8: 1}

COMPUTE = ("act", "dve", "pool", "pe")
DMAQ = ("sp", "act")


class Res:
    __slots__ = ("name", "writers", "readers", "wdeps")

    def __init__(self, name=""):
        self.name = name
        self.writers = []
        self.readers = []
        self.wdeps = []


class Tile(Res):
    __slots__ = ("ap",)

    def __init__(self, name, ap):
        Res.__init__(self, name)
        self.ap = ap

    def __getitem__(self, k):
        return self.ap[k]


class Op:
    __slots__ = ("eng", "fn", "deps", "dma", "signal", "sem", "val", "epoch", "prev")

    def __init__(self, eng, fn, dma, epoch):
        self.eng = eng
        self.fn = fn
        self.dma = dma
        self.deps = {}
        self.signal = False
        self.sem = None
        self.val = 0
        self.epoch = epoch
        self.prev = None


class Prog:
    def __init__(self, nc):
        self.nc = nc
        self.ops = []
        self.epoch = 0

    @staticmethod
    def _push(lst, op):
        if not op.dma:
            for i, o in enumerate(lst):
                if (not o.dma) and o.eng == op.eng:
                    lst[i] = op
                    return
        lst.append(op)

    def add(self, eng, fn, r=(), w=(), dma=False):
        op = Op(eng, fn, dma, self.epoch)
        deps = op.deps
        for res in r:
            for wop in res.writers:
                deps[wop] = True
        for res in w:
            if res.readers:
                for rop in res.readers:
                    deps.setdefault(rop, False)
                for wop in res.writers:
                    deps.setdefault(wop, False)
            else:
                for pop in res.wdeps:
                    deps.setdefault(pop, False)
        for res in w:
            if res.readers or (res in r):
                if res.readers:
                    res.wdeps = list(res.readers) + list(res.writers)
                res.writers = [op]
                res.readers = []
            else:
                self._push(res.writers, op)
        for res in r:
            if res not in w:
                self._push(res.readers, op)
        self.ops.append(op)
        return op

    def dma(self, q, out_ap, in_ap, r=(), w=(), slow=False):
        if slow:
            return self.add(q, lambda e: e.dma_start(out=out_ap, in_=in_ap, allow_slow_non_contiguous=True),
                            r=r, w=w, dma=True)
        return self.add(q, lambda e: e.dma_start(out=out_ap, in_=in_ap), r=r, w=w, dma=True)

    def emit(self, stack, n_epochs):
        nc = self.nc
        NPOOL = 6
        engsem = {}
        for e in COMPUTE:
            engsem[e] = [stack.enter_context(nc.semaphore("c_%s_%d" % (e, k))) for k in range(n_epochs)]
        dmasem = {}
        for q in DMAQ:
            dmasem[q] = [stack.enter_context(nc.semaphore("d_%s_%d" % (q, k))) for k in range(NPOOL)]
        for op in self.ops:
            for d in op.deps:
                d.signal = True
        cnt = {}
        dcount = {}
        dlast = {}
        dk = {q: 0 for q in DMAQ}
        for op in self.ops:
            if op.dma:
                s = dmasem[op.eng][dk[op.eng] % NPOOL]
                dk[op.eng] += 1
                dcount[s] = dcount.get(s, 0) + 1
                op.sem = s
                op.val = 16 * dcount[s]
                op.prev = dlast.get(s)
                dlast[s] = op
            elif op.signal:
                key = (op.eng, op.epoch)
                cnt[key] = cnt.get(key, 0) + 1
                op.sem = engsem[op.eng][op.epoch]
                op.val = cnt[key]
        print("sem counts", {k: v for k, v in cnt.items()}, "dma max", max(dcount.values()) * 16 if dcount else 0)
        streams = {"sp": [], "act": [], "dve": [], "pool": [], "pe": []}
        for op in self.ops:
            streams[op.eng].append(op)
        nwaits = [0]

        def run(engname, e):
            known = {}
            kep = {}

            def need(d):
                if d.dma:
                    return known.get(id(d.sem), 0) < d.val
                if kep.get(d.eng, -1) > d.epoch:
                    return False
                return known.get(id(d.sem), 0) < d.val

            def wait(d):
                e.wait_ge(d.sem, d.val)
                nwaits[0] += 1
                known[id(d.sem)] = d.val
                if not d.dma:
                    if kep.get(d.eng, -1) < d.epoch:
                        kep[d.eng] = d.epoch

            for op in streams[engname]:
                for d, raw in op.deps.items():
                    if (not d.dma) and (not op.dma) and d.eng == op.eng:
                        if engname == "pe":
                            continue
                    if need(d):
                        wait(d)
                if op.dma and op.prev is not None and need(op.prev):
                    wait(op.prev)
                ins = op.fn(e)
                if op.dma:
                    ins.then_inc(op.sem, 16)
                elif op.signal:
                    ins.then_inc(op.sem, 1)
            if engname in DMAQ:
                for s in dmasem[engname]:
                    if s in dlast and known.get(id(s), 0) < dlast[s].val:
                        e.wait_ge(s, dlast[s].val)

        with nc.Block() as block:
            @block.sync
            def _(e):
                run("sp", e)

            @block.scalar
            def _(e):
                run("act", e)

            @block.vector
            def _(e):
                run("dve", e)

            @block.gpsimd
            def _(e):
                run("pool", e)

            @block.tensor
            def _(e):
                run("pe", e)
        return nwaits[0]


class Arena:
    def __init__(self, base_ap, size):
        self.base = base_ap
        self.size = size
        self.top = 0
        self.live = []
        self.grave = []

    def alloc(self, name, free_shape, dt, parts=128):
        n = 1
        for s in free_shape:
            n *= s
        nb = n * DSZ[dt]
        nb_al = (nb + 63) // 64 * 64
        off = self.top
        assert off + nb_al <= self.size, "SBUF arena overflow at %s: %d + %d > %d" % (name, off, nb_al, self.size)
        self.top += nb_al
        ap = self.base[0:parts, off:off + nb]
        if dt != U8:
            ap = ap.bitcast(dt)
        if len(free_shape) == 2:
            ap = ap.rearrange("p (a b) -> p a b", a=free_shape[0])
        elif len(free_shape) == 3:
            ap = ap.rearrange("p (a b c) -> p a b c", a=free_shape[0], b=free_shape[1])
        elif len(free_shape) == 4:
            ap = ap.rearrange("p (a b c d) -> p a b c d", a=free_shape[0], b=free_shape[1], c=free_shape[2])
        t = Tile(name, ap)
        keep = []
        for (g0, g1, gt) in self.grave:
            if g0 < off + nb_al and off < g1:
                t.readers.extend(gt.readers)
                t.readers.extend(gt.writers)
                if g0 >= off and g1 <= off + nb_al:
                    continue
            keep.append((g0, g1, gt))
        self.grave = keep
        self.live.append((off, off + nb_al, t))
        return t

    def mark(self):
        return (self.top, len(self.live))

    def release(self, m):
        top, nl = m
        for ent in self.live[nl:]:
            self.grave.append(ent)
        del self.live[nl:]
        self.top = top


class PsumArena:
    def __init__(self, banks):
        self.banks = banks
        self.top = 0
        self.live = []
        self.grave = []

    def alloc(self, name, ncols, dt=F32, parts=128):
        nb = ncols * DSZ[dt]
        nb = (nb + 3) // 4 * 4
        if self.top % 2048:
            self.top = (self.top // 2048 + 1) * 2048
        off = self.top
        assert off + nb <= 8 * 2048, "PSUM overflow at %s" % name
        self.top += nb
        bank = off // 2048
        c0 = (off % 2048) // 4
        ap = self.banks[bank][0:parts, c0:c0 + nb // 4]
        if dt != F32:
            ap = ap.bitcast(dt)
        t = Tile(name, ap)
        keep = []
        for (g0, g1, gt) in self.grave:
            if g0 < off + nb and off < g1:
                t.readers.extend(gt.readers)
                t.readers.extend(gt.writers)
                if g0 >= off and g1 <= off + nb:
                    continue
            keep.append((g0, g1, gt))
        self.grave = keep
        self.live.append((off, off + nb, t))
        return t

    def mark(self):
        return (self.top, len(self.live))

    def release(self, m):
        top, nl = m
        for ent in self.live[nl:]:
            self.grave.append(ent)
        del self.live[nl:]
        self.top = top


def _t5_bucket(dist):
    n = np.maximum(dist, 0)
    nf = np.maximum(n, 1).astype(np.float32)
    large = 16 + (np.log(nf / np.float32(16)) / np.float32(math.log(1024 / 16)) * np.float32(16)).astype(np.int32)
    large = np.minimum(large, 31)
    return np.where(n < 16, n, large)


def _onehot(dvals, valid):
    oh = np.zeros((33, len(dvals)), np.float32)
    b = _t5_bucket(dvals)
    for x in range(len(dvals)):
        if valid[x]:
            oh[b[x], x] = 1.0
        else:
            oh[32, x] = 1.0
    return oh


DL_SWA = 383
DL_NSA = 1791
DL_CMP = 6128


def host_consts(S):
    bf = ml_dtypes.bfloat16
    c = {}
    c["k_identb"] = np.eye(128, dtype=np.float32).astype(bf)
    c["k_identf"] = np.eye(128, dtype=np.float32)
    tp = np.arange(128)
    c["k_tri"] = (tp[:, None] <= tp[None, :]).astype(np.float32)
    d = np.arange(DL_SWA) - 127
    c["k_ohswa"] = _onehot(d, (d >= 0) & (d < 128))
    d = np.arange(DL_NSA) - 127
    c["k_ohnsa"] = _onehot(d, d >= 0)
    d = np.arange(DL_CMP) - 2063
    c["k_ohcmp"] = _onehot(d, d >= 0)
    s = np.arange(S)
    c["k_sel"] = (s[None, :] // 64 == np.arange(64)[:, None]).astype(np.float32).astype(bf)
    a = np.arange(128)[:, None]
    u = np.arange(1024)[None, :]
    c["k_wm"] = np.where(u - a < 512, 0.0, NEG).astype(np.float32).astype(bf)
    u = np.arange(512)[None, :]
    c["k_cm"] = np.where(u - a >= 0, 0.0, NEG).astype(np.float32).astype(bf)
    z = np.arange(127)[None, :] - 63
    rel = z - (np.arange(128)[:, None] // 64)
    forced = (rel == 0) | (rel == -1)
    future = rel > 0
    c["k_km"] = np.where(forced | future, 0.0, 1.0).astype(np.float32)
    c["k_ov"] = np.where(forced, 1e30, np.where(future, -1e30, 0.0)).astype(np.float32)
    n_c = S // 16 - 1
    cs = np.arange(n_c) * 16
    ss = np.arange(64) * 64
    ov = np.clip(np.minimum(cs[:, None] + 32, ss[None, :] + 64) - np.maximum(cs[:, None], ss[None, :]), 0, None)
    ovl = np.zeros((256, 64), np.float32)
    ovl[:n_c] = ov.astype(np.float32) / 32.0
    c["k_ovl"] = ovl
    c["k_ones"] = np.ones((8, S), np.float32).astype(bf)
    return c


CONST_SPECS = [
    ("k_identb", [128, 128], BF16), ("k_identf", [128, 128], F32), ("k_tri", [128, 128], F32),
    ("k_ohswa", [33, DL_SWA], F32), ("k_ohnsa", [33, DL_NSA], F32), ("k_ohcmp", [33, DL_CMP], F32),
    ("k_sel", None, BF16), ("k_wm", [128, 1024], BF16), ("k_cm", [128, 512], BF16),
    ("k_km", [128, 127], F32), ("k_ov", [128, 127], F32), ("k_ovl", [256, 64], F32),
    ("k_ones", 8, BF16),
]


WSPLITS = [
    ("swa_q", 0, 256), ("swa_k", 256, 128), ("swa_v", 384, 128), ("fox_q", 512, 256), ("fox_k", 768, 256),
    ("fox_v", 1024, 256), ("fb", 1280, 4), ("nsa_q", 1284, 512), ("kc", 1796, 128), ("vc", 1924, 128),
    ("ks", 2052, 128), ("vs", 2180, 128), ("kw", 2308, 128), ("vw", 2436, 128), ("gc", 2564, 24),
]
TORDER = ["swa_q", "fox_q", "nsa_q", "swa_k", "fox_k", "kc", "vc", "ks", "kw"]
VORDER = ["swa_v", "fox_v", "vs", "vw", "fb", "gc"]
QT_SWAQ, QT_FOXQ, QT_NSAQ, QT_SWAK, QT_FOXK, QT_KC, QT_VC, QT_KS, QT_KW = 0, 256, 512, 1024, 1152, 1408, 1536, 1664, 1792
NFT = 1920
V_SWA, V_FOX, V_S, V_W = 0, 128, 384, 512
NV = 640


def dap(t, offset, dims):
    return bass.AP(tensor=t.tensor, offset=offset, ap=[list(x) for x in dims])


def build(S, DEPTH, debug=False, stop_after=None):
    from contextlib import ExitStack
    nc = bass.Bass("TRN2", target_bir_lowering=False)
    NT = S // 128
    NQ = S // 512
    NCMP = S // 16 - 1
    NNT = (NCMP + 127) // 128
    nsz = [min(128, NCMP - 128 * k) for k in range(NNT)]

    def din(name, shape, dt=F32):
        return nc.dram_tensor(name, list(shape), dt, kind="ExternalInput").ap()

    def dscr(name, shape, dt):
        return nc.dram_tensor(name, list(shape), dt, kind="Internal").ap()

    x_in = din("x", [S, D])
    c_in = din("c", [D])
    rel_bias = din("rel_bias", [32, 12])
    ada_w = din("ada_w", [DEPTH, D, 6 * D])
    ada_b = din("ada_b", [DEPTH, 6 * D])
    g_apre = din("attn_pre_norm", [DEPTH, D])
    g_apost = din("attn_post_norm", [DEPTH, D])
    g_fpre = din("ffn_pre_norm", [DEPTH, D])
    g_fpost = din("ffn_post_norm", [DEPTH, D])
    w_in = din("w_in", [DEPTH, D, NIN])
    forget_bias = din("forget_bias", [DEPTH, 4])
    swa_sinks = din("swa_sinks", [DEPTH, 4])
    cmp_pos = din("cmp_pos", [DEPTH, 2, 32, 64])
    cmp_w1 = din("cmp_w1", [DEPTH, 2, 2048, 128])
    cmp_w2 = din("cmp_w2", [DEPTH, 2, 128, 64])
    group_norm = din("group_norm", [DEPTH, D])
    w_out = din("w_out", [DEPTH, D, D])
    w_gate = din("ffn_w_gate", [DEPTH, D, FFN])
    w_up = din("ffn_w_up", [DEPTH, D, FFN])
    w_down = din("ffn_w_down", [DEPTH, FFN, D])
    K = {}
    for name, shape, dt in CONST_SPECS:
        if shape is None:
            shape = [64, S]
        elif shape == 8:
            shape = [8, S]
        K[name] = din(name, shape, dt)
    out = nc.dram_tensor("out", [S, D], F32, kind="ExternalOutput").ap()

    qT_d = dscr("qT_d", [NFT, S], BF16)
    v_d = dscr("v_d", [S, NV], BF16)
    o_d = dscr("o_d", [S, D], F32)
    g_swa_d = dscr("g_swa_d", [4, DL_SWA], BF16)
    g_nsa_d = dscr("g_nsa_d", [8, DL_NSA], BF16)
    g_cmp_d = dscr("g_cmp_d", [8, DL_CMP], BF16)
    t_swa_d = dscr("t_swa_d", [4, 128, DL_SWA], BF16)
    t_nsa_d = dscr("t_nsa_d", [8, 128, DL_NSA], BF16)
    t_cmp_d = dscr("t_cmp_d", [8, 128, DL_CMP], BF16)
    wgu_d = dscr("wgu_d", [DEPTH, 22, 128, 2, 8, 128], BF16)
    wd_d = dscr("wd_d", [DEPTH, 128, 22, D], BF16)
    wo_d = dscr("wo_d", [DEPTH, 128, 8, D], BF16)
    dbg = {}
    if debug:
        dbg["qT"] = nc.dram_tensor("dbg_qT", [NFT, S], BF16, kind="ExternalOutput").ap()
        dbg["v"] = nc.dram_tensor("dbg_v", [S, NV], BF16, kind="ExternalOutput").ap()
        dbg["o"] = nc.dram_tensor("dbg_o", [S, D], F32, kind="ExternalOutput").ap()
        dbg["mod"] = nc.dram_tensor("dbg_mod", [128, 6 * D], F32, kind="ExternalOutput").ap()
        dbg["fg"] = nc.dram_tensor("dbg_fg", [128, NT * 28], F32, kind="ExternalOutput").ap()
        dbg["x1"] = nc.dram_tensor("dbg_x1", [S, D], F32, kind="ExternalOutput").ap()

    stack = ExitStack()
    with stack:
        ARENA_BYTES = 206 * 1024
        arena_t = stack.enter_context(nc.sbuf_tensor("arena", [128, ARENA_BYTES], U8))
        banks = [stack.enter_context(nc.psum_tensor("pb%d" % k, [128, 512], F32)) for k in range(8)]
        A = Arena(arena_t[:, :], ARENA_BYTES)
        PS = PsumArena([b[:, :] for b in banks])
        P = Prog(nc)
        RQT = Res("qT_d")
        RV = Res("v_d")
        RO = Res("o_d")
        RX = [Res("xblk%d" % k) for k in range(NQ)]
        RW = Res("wconv")
        rr = [0]

        def evac(out_t, out_ap, in_t, in_ap, scale=None, eng=None):
            rr[0] += 1
            if eng == "act" or (eng is None and rr[0] % 2 == 0):
                if scale is None:
                    P.add("act", lambda e: e.copy(out=out_ap, in_=in_ap), r=[in_t], w=[out_t])
                else:
                    P.add("act", lambda e: e.mul(out=out_ap, in_=in_ap, mul=scale), r=[in_t], w=[out_t])
            else:
                if scale is None:
                    P.add("dve", lambda e: e.tensor_copy(out=out_ap, in_=in_ap), r=[in_t], w=[out_t])
                else:
                    P.add("dve", lambda e: e.tensor_scalar(out=out_ap, in0=in_ap, scalar1=scale, scalar2=None,
                                                           op0=ALU.mult), r=[in_t], w=[out_t])

        def mm(out_t, out_ap, lt, lap, rt, rap, start, stop):
            P.add("pe", lambda e: e.matmul(out_ap, lap, rap, start=start, stop=stop), r=[lt, rt], w=[out_t])

        def tr(out_t, out_ap, in_t, in_ap, ident_t, ident_ap):
            P.add("pe", lambda e: e.transpose(out_ap, in_ap, ident_ap), r=[in_t, ident_t], w=[out_t])

        identb = A.alloc("identb", [128], BF16)
        identf = A.alloc("identf", [128], F32)
        tri = A.alloc("tri", [128], F32)
        onesf = A.alloc("onesf", [128], F32)
        onesb = A.alloc("onesb", [512], BF16)
        cmt = A.alloc("cm", [512], BF16)
        wmt = A.alloc("wm", [1024], BF16)
        kmt = A.alloc("km", [127], F32)
        ovt = A.alloc("ov", [127], F32)
        P.dma("sp", identb[:, :], K["k_identb"], w=[identb])
        P.dma("sp", identf[:, :], K["k_identf"], w=[identf])
        P.dma("sp", tri[:, :], K["k_tri"], w=[tri])
        P.dma("sp", cmt[:, :], K["k_cm"], w=[cmt])
        P.dma("sp", wmt[:, :], K["k_wm"], w=[wmt])
        P.dma("sp", kmt[:, :], K["k_km"], w=[kmt])
        P.dma("sp", ovt[:, :], K["k_ov"], w=[ovt])
        P.add("dve", lambda e: e.memset(onesf[:, :], 1.0), w=[onesf])
        P.add("dve", lambda e: e.memset(onesb[:, :], 1.0), w=[onesb])

        cvf = [A.alloc("cvf%d" % k, [1024], F32) for k in range(2)]
        cvb = [A.alloc("cvb%d" % k, [1024], BF16) for k in range(2)]
        cvk = [0]
        pend = []

        def conv_chunks(l):
            jobs = []

            def job(load_src_ap, a, dst_ap):
                def run(q="sp"):
                    f, b = cvf[cvk[0] % 2], cvb[cvk[0] % 2]
                    cvk[0] += 1
                    fv = f[:, :].rearrange("p (a b) -> p a b", a=a) if a > 1 else f[:, :]
                    bv = b[:, :].rearrange("p (a b) -> p a b", a=a) if a > 1 else b[:, :]
                    P.dma(q, fv, load_src_ap, w=[f])
                    if pend:
                        pend.pop(0)()
                    P.add("pool", lambda e: e.tensor_copy(out=b[:, :], in_=f[:, :]), r=[f], w=[b])
                    pend.append(lambda: P.dma(q, dst_ap, bv, r=[b], w=[RW]))
                jobs.append(run)

            for hc in range(22):
                for gi, src in enumerate((w_gate, w_up)):
                    job(src[l, :, hc * 128:(hc + 1) * 128].rearrange("(c p) n -> p c n", p=128), 8, wgu_d[l, hc, :, gi])
            for hc in range(22):
                job(w_down[l, hc * 128:(hc + 1) * 128, :], 1, wd_d[l, :, hc, :])
            for c in range(8):
                job(w_out[l, c * 128:(c + 1) * 128, :], 1, wo_d[l, :, c, :])
            return jobs

        m0 = A.mark()
        pm0 = PS.mark()
        tabl = A.alloc("tabl", [12], F32, parts=33)
        P.add("dve", lambda e: e.memset(tabl[32:33, :], NEG), w=[tabl])
        P.dma("sp", tabl[0:32, :], rel_bias, w=[tabl])
        oht = [A.alloc("oht%d" % k, [512], F32, parts=33) for k in range(2)]
        gst = [A.alloc("gst%d" % k, [512], BF16, parts=8) for k in range(2)]
        pst = [PS.alloc("pst%d" % k, 512, F32, parts=8) for k in range(2)]
        RG = Res("gtab")
        kk = 0
        for (ohn, h0, nh, DL, gd, td) in (("k_ohswa", 0, 4, DL_SWA, g_swa_d, t_swa_d),
                                           ("k_ohnsa", 4, 8, DL_NSA, g_nsa_d, t_nsa_d),
                                           ("k_ohcmp", 4, 8, DL_CMP, g_cmp_d, t_cmp_d)):
            for c0 in range(0, DL, 512):
                n = min(512, DL - c0)
                ot, gs, ps = oht[kk % 2], gst[kk % 2], pst[kk % 2]
                kk += 1
                P.dma("sp", ot[:, 0:n], K[ohn][:, c0:c0 + n], w=[ot])
                mm(ps, ps[0:nh, 0:n], tabl, tabl[:, h0:h0 + nh], ot, ot[:, 0:n], True, True)
                evac(gs, gs[0:nh, 0:n], ps, ps[0:nh, 0:n])
                P.dma("sp", gd[:, c0:c0 + n], gs[0:nh, 0:n], r=[gs], w=[RG])
            import os
            if not os.environ.get("K_SKIP_REP"):
                for h in range(nh):
                    P.dma("sp", td[h], dap(gd, h * DL, [[0, 128], [1, DL]]), r=[RG], w=[RG])
        A.release(m0)
        PS.release(pm0)
        cb = A.alloc("cb", [8, 128], F32)
        m0 = A.mark()
        cl = A.alloc("cl", [8], F32)
        P.dma("sp", cl[:, :], c_in.rearrange("(c p) -> p c", p=128), w=[cl], slow=True)
        P.add("act", lambda e: e.activation(out=cl[:, :], in_=cl[:, :], func=AF.Silu), r=[cl], w=[cl])
        P.add("dve", lambda e: e.tensor_copy(out=cb[:, :, :], in_=cl[:, :].unsqueeze(2).to_broadcast([128, 8, 128])),
              r=[cl], w=[cb])
        A.release(m0)

        import os
        conv_jobs = {}
        if not os.environ.get("K_SKIP_CONV"):
            for l in range(DEPTH):
                conv_jobs[l] = conv_chunks(l)

        persist_mark = A.mark()
        pspersist = PS.mark()

        for l in range(DEPTH):
            P.epoch = l
            xsrc = x_in if l == 0 else out
            mod = A.alloc("mod", [6, D], F32)
            gn = A.alloc("gn", [D], F32)
            mA = A.mark()
            pA = PS.mark()
            ones1 = A.alloc("ones1", [128], F32, parts=1)
            P.add("dve", lambda e: e.memset(ones1[:, :], 1.0), w=[ones1])
            wts = [A.alloc("adaw%d" % k, [8, 512], F32) for k in range(2)]
            bts = [A.alloc("adab%d" % k, [512], F32, parts=1) for k in range(2)]
            pss = [PS.alloc("psA%d" % k, 512) for k in range(2)]
            for ch in range(12):
                wt, bt, ps = wts[ch % 2], bts[ch % 2], pss[ch % 2]
                n0 = ch * 512
                P.dma("sp", wt[:, :, :], ada_w[l, :, n0:n0 + 512].rearrange("(c p) n -> p c n", p=128), w=[wt])
                P.dma("sp", bt[:, :], ada_b[l:l + 1, n0:n0 + 512], w=[bt])
                for c in range(8):
                    mm(ps, ps[:, :], cb, cb[:, c, :], wt, wt[:, c, :], c == 0, False)
                mm(ps, ps[:, :], ones1, ones1[:, :], bt, bt[:, :], False, True)
                evac(mod, mod[:, ch // 2, (ch % 2) * 512:(ch % 2) * 512 + 512], ps, ps[:, :])
            g4 = A.alloc("g4", [4, D], F32)
            for k, gsrc in enumerate((g_apre, g_apost, g_fpre, g_fpost)):
                P.dma("sp", g4[:, k, :], dap(gsrc, l * D, [[0, 128], [1, D]]), w=[g4])
            P.dma("sp", gn[:, :], dap(group_norm, l * D, [[0, 128], [1, D]]), w=[gn])
            for (mi, gi) in ((1, 0), (4, 2)):
                P.add("dve", lambda e, mi=mi, gi=gi: e.scalar_tensor_tensor(
                    out=mod[:, mi, :], in0=mod[:, mi, :], scalar=1.0, in1=g4[:, gi, :], op0=ALU.add, op1=ALU.mult),
                    r=[mod, g4], w=[mod])
            for (mi, gi) in ((2, 1), (5, 3)):
                P.add("dve", lambda e, mi=mi, gi=gi: e.tensor_tensor(
                    out=mod[:, mi, :], in0=mod[:, mi, :], in1=g4[:, gi, :], op=ALU.mult), r=[mod, g4], w=[mod])
            if debug and l == 0:
                P.dma("sp", dbg["mod"], mod[:, :, :].rearrange("p a b -> p (a b)"), r=[mod])
            A.release(mA)
            PS.release(pA)
            if stop_after == "A":
                break

            fg_mark = A.mark()
            fg = A.alloc("fg", [NT, 28], F32)
            mB = A.mark()
            pB = PS.mark()
            wsb = A.alloc("wsb", [8, NIN], BF16)
            dcol = {}
            c0 = 0
            for nm in TORDER + VORDER:
                dcol[nm] = c0
                c0 += [w for (n_, s_, w) in WSPLITS if n_ == nm][0]
            for (nm, src, wd) in WSPLITS:
                for cc in range(0, wd, 128):
                    n = min(128, wd - cc)
                    sft = cvf[cvk[0] % 2]
                    cvk[0] += 1
                    sf = sft[:, :].rearrange("p (a b) -> p a b", a=8)
                    P.dma("sp", sf[:, :, 0:n], w_in[l, :, src + cc:src + cc + n].rearrange("(c p) n -> p c n", p=128), w=[sft])
                    P.add(os.environ.get("K_CAST", "pool"), lambda e, sf=sf, n=n, d0=dcol[nm] + cc: e.tensor_copy(out=wsb[:, :, d0:d0 + n], in_=sf[:, :, 0:n]),
                          r=[sft], w=[wsb])
            xts = [A.alloc("xt%d" % k, [4, D], F32) for k in range(1)]
            hf = A.alloc("hf", [D], F32)
            hbs = [A.alloc("hb%d" % k, [4, D], BF16) for k in range(2)]
            hTs = [A.alloc("hT%d" % k, [8, 512], BF16) for k in range(2)]
            junk = A.alloc("junk", [D], BF16)
            ss = A.alloc("ss", [4], F32)
            rs = A.alloc("rs", [4], F32)
            stg = [A.alloc("stg%d" % k, [512], BF16) for k in range(3)]
            vst = [A.alloc("vst%d" % k, [4, NV], BF16) for k in range(2)]
            ptr = [PS.alloc("ptr%d" % k, 512, BF16) for k in range(2)]
            pmm = [PS.alloc("pmm%d" % k, 512) for k in range(4)]
            pv2 = [PS.alloc("pv2%d" % k, 156) for k in range(2)]
            kq = 0
            KB = 9

            def prep_a(tb):
                t0 = tb * 512
                xt = xts[0]
                hb = hbs[tb % 2]
                P.dma("sp", xt[:, :, :], xsrc[t0:t0 + 512, :].rearrange("(s p) d -> p s d", p=128), r=[RX[tb]], w=[xt])
                P.add("dve", lambda e: e.memset(ss[:, :], 0.0), w=[ss])
                for s in range(4):
                    P.add("act", lambda e, s=s: e.activation(out=junk[:, :], in_=xt[:, s, :], func=AF.Square,
                                                           accum_out=ss[:, s:s + 1]), r=[xt, ss], w=[junk, ss])
                P.add("dve", lambda e: e.tensor_scalar(out=rs[:, :], in0=ss[:, :], scalar1=1.0 / D, scalar2=1e-6,
                                                       op0=ALU.mult, op1=ALU.add), r=[ss], w=[rs])
                P.add("act", lambda e: e.activation(out=rs[:, :], in_=rs[:, :], func=AF.Sqrt), r=[rs], w=[rs])
                P.add("dve", lambda e: e.reciprocal(out=rs[:, :], in_=rs[:, :]), r=[rs], w=[rs])
                for s in range(4):
                    P.add("dve", lambda e, s=s: e.scalar_tensor_tensor(
                        out=hf[:, :], in0=xt[:, s, :], scalar=rs[:, s:s + 1], in1=mod[:, 1, :],
                        op0=ALU.mult, op1=ALU.mult), r=[xt, rs, mod], w=[hf])
                    P.add("dve", lambda e, s=s: e.tensor_tensor(out=hb[:, s, :], in0=hf[:, :], in1=mod[:, 0, :],
                                                               op=ALU.add), r=[hf, mod], w=[hb])

            def prep_b(tb):
                hb, hT = hbs[tb % 2], hTs[tb % 2]
                for c in range(8):
                    pt = ptr[c % 2]
                    for s in range(4):
                        tr(pt, pt[:, s * 128:(s + 1) * 128], hb, hb[:, s, c * 128:(c + 1) * 128], identb, identb[:, :])
                    evac(hT, hT[:, c, :], pt, pt[:, :])

            prep_a(0)
            prep_b(0)
            for tb in range(NQ):
                t0 = tb * 512
                hT = hTs[tb % 2]
                if tb + 1 < NQ:
                    prep_a(tb + 1)
                for m in range(NFT // 128):
                    ps = pmm[kq % 4]
                    sg = stg[kq % 3]
                    kq += 1
                    for c in range(8):
                        mm(ps, ps[:, :], wsb, wsb[:, c, m * 128:(m + 1) * 128], hT, hT[:, c, :], c == 0, c == 7)
                    evac(sg, sg[:, :], ps, ps[:, :], scale=(0.125 if m < 8 else None))
                    P.dma("act", qT_d[m * 128:(m + 1) * 128, t0:t0 + 512], sg[:, :], r=[sg], w=[RQT])
                    if m == 11 and tb + 1 < NQ:
                        prep_b(tb + 1)
                vs_ = vst[tb % 2]
                for s in range(4):
                    ps = pmm[kq % 4]
                    kq += 1
                    p2 = pv2[s % 2]
                    for c in range(8):
                        mm(ps, ps[:, :], hT, hT[:, c, s * 128:(s + 1) * 128], wsb, wsb[:, c, NFT:NFT + 512], c == 0, c == 7)
                    for c in range(8):
                        mm(p2, p2[:, :], hT, hT[:, c, s * 128:(s + 1) * 128], wsb, wsb[:, c, NFT + 512:NFT + 668],
                           c == 0, c == 7)
                    evac(vs_, vs_[:, s, 0:512], ps, ps[:, :])
                    evac(vs_, vs_[:, s, 512:640], p2, p2[:, 0:128], eng="dve")
                    evac(fg, fg[:, tb * 4 + s, :], p2, p2[:, 128:156], eng="dve")
                P.dma("act", v_d[t0:t0 + 512, :].rearrange("(s p) f -> p s f", p=128), vs_[:, :, :], r=[vs_], w=[RV])
            if debug and l == 0 and KB >= 8:
                P.dma("sp", dbg["fg"], fg[:, :, :].rearrange("p a b -> p (a b)"), r=[fg])
                P.dma("sp", dbg["qT"], qT_d, r=[RQT])
                P.dma("sp", dbg["v"], v_d, r=[RV])
            A.release(mB)
            PS.release(pB)
            if stop_after == "B":
                break
            build_attention(locals())
            if debug and l == 0:
                if stop_after == "FS":
                    P.dma("sp", dbg["o"][:, 0:512], o_d[:, 0:512], r=[RO])
                else:
                    P.dma("sp", dbg["o"], o_d, r=[RO])
            if stop_after in ("C", "FS"):
                break
            A.release(fg_mark)
            build_ffn(locals())
            A.release(persist_mark)
            PS.release(pspersist)

        nw = P.emit(stack, max(DEPTH, 1))
        print("ops", len(P.ops), "waits", nw)
    return nc


def make_in_map(inputs, b, S, consts=None):
    m = {}
    for k, v in inputs.items():
        v = np.asarray(v)
        if k == "x" or k == "c":
            m[k] = np.ascontiguousarray(v[b], dtype=np.float32)
        else:
            m[k] = np.ascontiguousarray(v, dtype=np.float32)
    m.update(consts if consts is not None else host_consts(S))
    return m


def kernel(**inputs):
    x = np.asarray(inputs["x"])
    B, S, _ = x.shape
    DEPTH = int(np.asarray(inputs["ada_w"]).shape[0])
    nc = build(S, DEPTH)
    consts = host_consts(S)
    shared = {k: np.ascontiguousarray(np.asarray(v), dtype=np.float32) for k, v in inputs.items() if k not in ("x", "c")}
    in_maps = []
    for b in range(B):
        m = dict(shared)
        m["x"] = np.ascontiguousarray(x[b], dtype=np.float32)
        m["c"] = np.ascontiguousarray(np.asarray(inputs["c"])[b], dtype=np.float32)
        m.update(consts)
        in_maps.append(m)
    res = run_bass_kernel_spmd(nc, in_maps, core_ids=list(range(B)))
    return np.stack([np.asarray(res.results[b]["out"], dtype=np.float32) for b in range(B)], 0)


def pipeline(n, fa, fb, fc, la=2):
    for k in range(n + la):
        if k < n:
            fa(k)
        if k - la >= 0:
            fb(k - la)
            fc(k - la)


def build_attention(Ld):
    from types import SimpleNamespace
    L = SimpleNamespace(**Ld)
    P, A, PS, K = L.P, L.A, L.PS, L.K
    S, NT, NQ, NCMP, NNT, nsz, l = L.S, L.NT, L.NQ, L.NCMP, L.NNT, L.nsz, L.l
    fg, identb, identf, tri, onesf = L.fg, L.identb, L.identf, L.tri, L.onesf
    cmt, wmt, kmt, ovt = L.cmt, L.wmt, L.kmt, L.ovt
    qT_d, v_d, o_d, RQT, RV, RO, RG = L.qT_d, L.v_d, L.o_d, L.RQT, L.RV, L.RO, L.RG
    mm, tr, evac = L.mm, L.tr, L.evac
    t_swa_d, t_nsa_d = L.t_swa_d, L.t_nsa_d
    m_att = A.mark()
    mswa = A.alloc("mswa", [4, 256], BF16)
    mnsa = A.alloc("mnsa", [8, 1664], BF16)
    P.dma("sp", mswa[:, :, :], dap(t_swa_d, 127, [[DL_SWA - 1, 128], [128 * DL_SWA, 4], [1, 256]]), r=[RG], w=[mswa])
    for h in range(8):
        P.dma("sp", mnsa[:, h, :], dap(t_nsa_d, h * 128 * DL_NSA + 127, [[DL_NSA - 1, 128], [1, 1664]]),
              r=[RG], w=[mnsa])
    b31 = A.alloc("b31", [8], F32)
    P.add("dve", lambda e: e.tensor_copy(out=b31[:, :], in_=mnsa[:, :, 1663]), r=[mnsa], w=[b31])
    L.mswa, L.mnsa, L.b31 = mswa, mnsa, b31

    sg = A.alloc("sg", [NT, 24], F32)
    P.add("act", lambda e: e.activation(out=sg[:, :, :], in_=fg[:, :, 4:28], func=AF.Sigmoid), r=[fg], w=[sg])

    mF = A.mark()
    pF = PS.mark()
    m1 = A.mark()
    fbt = A.alloc("fbt", [4], F32)
    P.dma("sp", fbt[:, :], dap(L.forget_bias, l * 4, [[0, 128], [1, 4]]), w=[fbt])
    z = A.alloc("z", [NT, 4], F32)
    P.add("dve", lambda e: e.tensor_tensor(out=z[:, :, :], in0=fg[:, :, 0:4],
                                           in1=fbt[:, :].unsqueeze(1).to_broadcast([128, NT, 4]), op=ALU.add),
          r=[fg, fbt], w=[z])
    P.add("act", lambda e: e.activation(out=z[:, :, :], in_=z[:, :, :], func=AF.Exp, scale=-1.0), r=[z], w=[z])
    P.add("act", lambda e: e.activation(out=z[:, :, :], in_=z[:, :, :], func=AF.Ln, bias=1.0), r=[z], w=[z])
    psc = PS.alloc("psc", NT * 4)
    pst = PS.alloc("pst", NT * 4)
    zf = z[:, :, :].rearrange("p a b -> p (a b)")
    mm(psc, psc[:, :], tri, tri[:, :], z, zf, True, True)
    mm(pst, pst[:, :], onesf, onesf[:, :], z, zf, True, True)
    tot = A.alloc("tot", [NT, 4], F32)
    evac(tot, tot[:, :, :].rearrange("p a b -> p (a b)"), pst, pst[:, :])
    pre = A.alloc("pre", [NT, 4], F32)
    P.add("dve", lambda e: e.memset(pre[:, 0, :], 0.0), w=[pre])
    for k in range(1, NT):
        P.add("dve", lambda e, k=k: e.tensor_tensor(out=pre[:, k, :], in0=pre[:, k - 1, :], in1=tot[:, k - 1, :],
                                                    op=ALU.add), r=[pre, tot], w=[pre])
    Nn = A.alloc("Nn", [NT, 4], F32)
    P.add("dve", lambda e: e.tensor_tensor(out=Nn[:, :, :], in0=psc[:, :].rearrange("p (a b) -> p a b", b=4),
                                           in1=pre[:, :, :], op=ALU.add), r=[psc, pre], w=[Nn])
    R = A.alloc("R", [NT, 4, 6], BF16)
    r1 = A.alloc("r1", [NT, 4], F32)
    P.add("dve", lambda e: e.tensor_copy(out=R[:, :, :, 3], in_=Nn[:, :, :]), r=[Nn], w=[R])
    P.add("dve", lambda e: e.tensor_tensor(out=r1[:, :, :], in0=Nn[:, :, :], in1=R[:, :, :, 3], op=ALU.subtract),
          r=[Nn, R], w=[r1])
    P.add("dve", lambda e: e.tensor_copy(out=R[:, :, :, 4], in_=r1[:, :, :]), r=[r1], w=[R])
    P.add("dve", lambda e: e.tensor_tensor(out=r1[:, :, :], in0=r1[:, :, :], in1=R[:, :, :, 4], op=ALU.subtract),
          r=[r1, R], w=[r1])
    P.add("dve", lambda e: e.tensor_copy(out=R[:, :, :, 5], in_=r1[:, :, :]), r=[r1], w=[R])
    for r_ in range(3):
        P.add("dve", lambda e, r_=r_: e.tensor_scalar(out=R[:, :, :, r_], in0=R[:, :, :, 3 + r_], scalar1=-1.0,
                                                      scalar2=None, op0=ALU.mult), r=[R], w=[R])
    rowsT = A.alloc("rowsT", [S], BF16, parts=24)
    psR = [PS.alloc("psR%d" % k, 512, BF16, parts=24) for k in range(2)]
    for k0 in range(0, NT, 4):
        pr = psR[(k0 // 4) % 2]
        for k in range(k0, k0 + 4):
            tr(pr, pr[:, (k - k0) * 128:(k - k0 + 1) * 128], R, R[:, k, :, :].rearrange("p a b -> p (a b)"),
               identb, identb[:, :])
        evac(rowsT, rowsT[:, k0 * 128:(k0 + 4) * 128], pr, pr[:, :])
    PS.release(pF)
    qa = [A.alloc("qa%d" % k, [S], BF16) for k in range(2)]
    ka = [A.alloc("ka%d" % k, [S], BF16) for k in range(2)]
    vf = A.alloc("vf", [NT, 4, 65], BF16)
    for h in range(4):
        P.dma("sp", vf[:, :, h, 0:64], v_d[:, V_FOX + 64 * h:V_FOX + 64 * h + 64].rearrange("(k p) d -> p k d", p=128),
              r=[RV], w=[vf])
    P.add("dve", lambda e: e.memset(vf[:, :, :, 64:65], 1.0), w=[vf])
    rz = A.alloc("rz", [4], F32)
    ofs = [A.alloc("of%d" % k, [4, 64], F32) for k in range(2)]
    pts = [A.alloc("pt%d" % k, [512], BF16) for k in range(4)]
    pss = [PS.alloc("ps%d" % k, 512) for k in range(4)]
    accs = [PS.alloc("acc%d" % k, 512, parts=65) for k in range(2)]
    accN = PS.alloc("accN", 512)
    acnv = accN[:, :].rearrange("p (a b) -> p a b", a=4)
    oTs = A.alloc("oTs", [512], F32, parts=65)
    kcnt = [0]
    bg = list(L.conv_jobs.get(l, []))
    bgper = (len(bg) + 4 * NQ - 1) // (4 * NQ)

    def load_head(h):
        qaug, kaug = qa[h % 2], ka[h % 2]
        P.dma("sp", qaug[0:64, :], qT_d[QT_FOXQ + 64 * h:QT_FOXQ + 64 * h + 64, :], r=[RQT], w=[qaug])
        P.dma("sp", qaug[64:67, :], rowsT[6 * h:6 * h + 3, :], r=[rowsT], w=[qaug])
        P.dma("sp", qaug[67:70, :], K["k_ones"][0:3, :], w=[qaug])
        P.dma("sp", kaug[0:64, :], qT_d[QT_FOXK + 64 * h:QT_FOXK + 64 * h + 64, :], r=[RQT], w=[kaug])
        P.dma("sp", kaug[64:67, :], K["k_ones"][0:3, :], w=[kaug])
        P.dma("sp", kaug[67:70, :], rowsT[6 * h + 3:6 * h + 6, :], r=[rowsT], w=[kaug])

    load_head(0)
    for h in range(4):
        qaug, kaug = qa[h % 2], ka[h % 2]
        if h + 1 < 4:
            load_head(h + 1)
        units = [(i, j) for i in range(NQ) for j in range(4 * i + 4)]
        base = kcnt[0]
        kcnt[0] += len(units)

        def fa(k, units=units, base=base, qaug=qaug, kaug=kaug):
            i, j = units[k]
            bb0 = max(0, j - 4 * i)
            ps = pss[(base + k) % 4]
            diag = j >= 4 * i
            mm(ps, ps[:, bb0 * 128:512], kaug, kaug[0:70, j * 128:(j + 1) * 128],
               qaug, qaug[0:70, i * 512 + bb0 * 128:(i + 1) * 512], True, not diag)
            if diag:
                mm(ps, ps[:, bb0 * 128:512], identb, identb[:, :], cmt, cmt[:, 0:(4 - bb0) * 128], False, True)

        def fb(k, units=units, base=base):
            i, j = units[k]
            bb0 = max(0, j - 4 * i)
            ps = pss[(base + k) % 4]
            pt = pts[(base + k) % 4]
            P.add("act", lambda e: e.activation(out=pt[:, bb0 * 128:512], in_=ps[:, bb0 * 128:512], func=AF.Exp),
                  r=[ps], w=[pt])

        def fc(k, units=units, base=base, h=h):
            i, j = units[k]
            bb0 = max(0, j - 4 * i)
            pt = pts[(base + k) % 4]
            acc = accs[i % 2]
            mm(acc, acc[0:65, bb0 * 128:512], vf, vf[:, j, h, :], pt, pt[:, bb0 * 128:512], j == 0, j == 4 * i + 3)
            if j == 4 * i + 3:
                of = ofs[i % 2]
                for _ in range(bgper):
                    if bg:
                        bg.pop(0)()
                evac(oTs, oTs[0:65, :], acc, acc[0:65, :], eng="dve")
                for bb in range(4):
                    tr(accN, acnv[:, bb, 0:65], oTs, oTs[0:65, bb * 128:(bb + 1) * 128], identf, identf[0:65, 0:65])
                P.add("dve", lambda e: e.reciprocal(out=rz[:, :], in_=acnv[:, :, 64]), r=[accN], w=[rz])
                for bb in range(4):
                    P.add("dve", lambda e, bb=bb: e.tensor_scalar(out=of[:, bb, :], in0=acnv[:, bb, 0:64],
                                                                  scalar1=rz[:, bb:bb + 1], scalar2=None, op0=ALU.mult),
                          r=[accN, rz], w=[of])
                P.dma("sp", o_d[i * 512:(i + 1) * 512, 256 + 64 * h:256 + 64 * h + 64].rearrange("(s p) d -> p s d", p=128),
                      of[:, :, :], r=[of], w=[RO])

        pipeline(len(units), fa, fb, fc, la=3)
    while bg:
        bg.pop(0)()
    while L.pend:
        L.pend.pop(0)()
    A.release(mF)
    PS.release(pF)

    mS = A.mark()
    pS = PS.mark()
    qs = A.alloc("qs", [2, S], BF16)
    P.dma("sp", qs[0:64, :, :], qT_d[QT_SWAQ:QT_SWAQ + 128, :].rearrange("(g d) t -> d g t", g=2), r=[RQT], w=[qs])
    P.dma("sp", qs[64:128, :, :], qT_d[QT_SWAQ + 128:QT_SWAQ + 256, :].rearrange("(g d) t -> d g t", g=2), r=[RQT], w=[qs])
    ksw = A.alloc("ksw", [S], BF16)
    P.dma("sp", ksw[:, :], qT_d[QT_SWAK:QT_SWAK + 128, :], r=[RQT], w=[ksw])
    vsw = A.alloc("vsw", [NT, 2, 65], BF16)
    for kv in range(2):
        P.dma("sp", vsw[:, :, kv, 0:64], v_d[:, V_SWA + 64 * kv:V_SWA + 64 * kv + 64].rearrange("(k p) d -> p k d", p=128),
              r=[RV], w=[vsw])
    P.add("dve", lambda e: e.memset(vsw[:, :, :, 64:65], 1.0), w=[vsw])
    es = A.alloc("es", [4], F32)
    P.dma("sp", es[:, :], dap(L.swa_sinks, l * 4, [[0, 128], [1, 4]]), w=[es])
    P.add("act", lambda e: e.activation(out=es[:, :], in_=es[:, :], func=AF.Exp), r=[es], w=[es])
    pts = [A.alloc("spt%d" % k, [256], BF16) for k in range(4)]
    pss = [PS.alloc("sps%d" % k, 256) for k in range(4)]
    accs = [PS.alloc("sacc%d" % k, 65) for k in range(4)]
    osw = [A.alloc("osw%d" % k, [4, 64], F32) for k in range(2)]
    zz = A.alloc("zz", [4], F32)
    units = [(ti, h) for ti in range(NT) for h in range(4)]

    def fa(k):
        ti, h = units[k]
        kv, g = h // 2, h % 2
        ps = pss[k % 4]
        rows = slice(kv * 64, kv * 64 + 64)
        if ti > 0:
            mm(ps, ps[:, 0:128], ksw, ksw[rows, (ti - 1) * 128:ti * 128], qs, qs[rows, g, ti * 128:(ti + 1) * 128], True, False)
            mm(ps, ps[:, 0:128], identb, identb[:, :], mswa, mswa[:, h, 128:256], False, True)
        mm(ps, ps[:, 128:256], ksw, ksw[rows, ti * 128:(ti + 1) * 128], qs, qs[rows, g, ti * 128:(ti + 1) * 128], True, False)
        mm(ps, ps[:, 128:256], identb, identb[:, :], mswa, mswa[:, h, 0:128], False, True)

    def fb(k):
        ti, h = units[k]
        ps, pt = pss[k % 4], pts[k % 4]
        c0 = 0 if ti > 0 else 128
        P.add("act", lambda e: e.activation(out=pt[:, c0:256], in_=ps[:, c0:256], func=AF.Exp), r=[ps], w=[pt])

    def fc(k):
        ti, h = units[k]
        kv = h // 2
        pt, acc = pts[k % 4], accs[k % 4]
        if ti > 0:
            mm(acc, acc[:, 0:65], pt, pt[:, 0:128], vsw, vsw[:, ti - 1, kv, :], True, False)
        mm(acc, acc[:, 0:65], pt, pt[:, 128:256], vsw, vsw[:, ti, kv, :], ti == 0, True)
        ow = osw[ti % 2]
        P.add("dve", lambda e: e.tensor_tensor(out=zz[:, h:h + 1], in0=acc[:, 64:65], in1=es[:, h:h + 1], op=ALU.add),
              r=[acc, es], w=[zz])
        P.add("dve", lambda e: e.reciprocal(out=zz[:, h:h + 1], in_=zz[:, h:h + 1]), r=[zz], w=[zz])
        P.add("dve", lambda e: e.tensor_scalar(out=ow[:, h, :], in0=acc[:, 0:64], scalar1=zz[:, h:h + 1], scalar2=None,
                                               op0=ALU.mult), r=[acc, zz], w=[ow])
        if h == 3:
            P.dma("sp", o_d[ti * 128:(ti + 1) * 128, 0:256], ow[:, :, :].rearrange("p a b -> p (a b)"), r=[ow], w=[RO])

    pipeline(len(units), fa, fb, fc, la=3)
    A.release(mS)
    PS.release(pS)
    build_nsa(L, sg)
    A.release(m_att)


def build_nsa(L, sg):
    P, A, PS, K = L.P, L.A, L.PS, L.K
    S, NT, NQ, NCMP, NNT, nsz, l = L.S, L.NT, L.NQ, L.NCMP, L.NNT, L.nsz, L.l
    identb, identf = L.identb, L.identf
    wmt, kmt, ovt, mnsa, b31 = L.wmt, L.kmt, L.ovt, L.mnsa, L.b31
    qT_d, v_d, o_d, RQT, RV, RO, RG = L.qT_d, L.v_d, L.o_d, L.RQT, L.RV, L.RO, L.RG
    mm, tr, evac = L.mm, L.tr, L.evac
    mN = A.mark()
    pN = PS.mark()
    kcmpT = A.alloc("kcmpT", [2, 256], BF16, parts=64)
    Rg = A.alloc("Rg", [NNT, 2, 128], F32)
    for nt in range(NNT):
        for g in range(2):
            P.dma("sp", Rg[0:nsz[nt], nt, g, 64:128], K["k_ovl"][nt * 128:nt * 128 + nsz[nt], :], w=[Rg])
    m0 = A.mark()
    p0 = PS.mark()
    xTs = [A.alloc("cxT%d" % k, [S], BF16, parts=64) for k in range(2)]
    w1s = [A.alloc("cw1%d" % k, [32, 128], BF16, parts=64) for k in range(2)]
    pes = [A.alloc("cpe%d" % k, [32], BF16, parts=64) for k in range(2)]
    w2s = [A.alloc("cw2%d" % k, [64], BF16) for k in range(2)]
    w1f = A.alloc("cw1f", [32, 128], F32, parts=64)
    pef = A.alloc("cpef", [32], F32, parts=64)
    w2f = A.alloc("cw2f", [64], F32)
    bsb = [A.alloc("cbias%d" % k, [1], F32) for k in range(2)]
    Gts = [A.alloc("cG%d" % k, [256], BF16) for k in range(2)]
    psG = [PS.alloc("psG%d" % k, 256) for k in range(2)]
    psb = [PS.alloc("psb%d" % k, 1) for k in range(2)]
    psK = PS.alloc("psK", 256, parts=64)
    psV = [PS.alloc("psV%d" % k, 64) for k in range(2)]
    kk = 0
    for which in range(2):
        w1, pe, w2 = w1s[which], pes[which], w2s[which]
        P.dma("sp", w1f[:, :, :], L.cmp_w1[l, which].rearrange("(l d) j -> d l j", d=64), w=[w1f])
        P.add("pool", lambda e, w1=w1: e.tensor_copy(out=w1[:, :, :], in_=w1f[:, :, :]), r=[w1f], w=[w1])
        P.dma("sp", pef[:, :], L.cmp_pos[l, which].rearrange("l d -> d l"), w=[pef], slow=True)
        P.add("pool", lambda e, pe=pe: e.tensor_copy(out=pe[:, :], in_=pef[:, :]), r=[pef], w=[pe])
        P.dma("sp", w2f[:, :], L.cmp_w2[l, which], w=[w2f])
        P.add("pool", lambda e, w2=w2: e.tensor_copy(out=w2[:, :], in_=w2f[:, :]), r=[w2f], w=[w2])
        pb_, bs = psb[which], bsb[which]
        for ll in range(32):
            mm(pb_, pb_[:, 0:1], w1, w1[:, ll, :], pe, pe[:, ll:ll + 1], ll == 0, ll == 31)
        evac(bs, bs[:, :], pb_, pb_[:, 0:1])
        for g in range(2):
            xT, pG, Gt = xTs[kk % 2], psG[kk % 2], Gts[kk % 2]
            kk += 1
            row = (QT_KC if which == 0 else QT_VC) + 64 * g
            P.dma("sp", xT[:, :], qT_d[row:row + 64, :], r=[RQT], w=[xT])
            for ll in range(32):
                mm(pG, pG[:, 0:NCMP], w1, w1[:, ll, :], xT, xT[:, ll:ll + 16 * (NCMP - 1) + 1:16], ll == 0, ll == 31)
            P.add("act", lambda e, Gt=Gt, pG=pG, bs=bs: e.activation(out=Gt[:, 0:NCMP], in_=pG[:, 0:NCMP],
                                                                     func=AF.Gelu_apprx_tanh, bias=bs[:, 0:1]),
                  r=[pG, bs], w=[Gt])
            if which == 0:
                mm(psK, psK[:, 0:NCMP], w2, w2[:, :], Gt, Gt[:, 0:NCMP], True, True)
                evac(kcmpT, kcmpT[:, g, 0:NCMP], psK, psK[:, 0:NCMP])
            else:
                for nt in range(NNT):
                    pV = psV[nt % 2]
                    mm(pV, pV[0:nsz[nt], 0:64], Gt, Gt[:, nt * 128:nt * 128 + nsz[nt]], w2, w2[:, :], True, True)
                    evac(Rg, Rg[0:nsz[nt], nt, g, 0:64], pV, pV[0:nsz[nt], 0:64])
    A.release(m0)
    PS.release(p0)

    kaug = [A.alloc("kaug%d" % g, [S], BF16) for g in range(2)]
    kwT = A.alloc("kwT", [2, S], BF16, parts=64)
    vsl = A.alloc("vsl", [NT, 2, 65], BF16)
    vwn = A.alloc("vwn", [NT, 2, 65], BF16)
    for g in range(2):
        P.dma("sp", kaug[g][0:64, :], qT_d[QT_KS + 64 * g:QT_KS + 64 * g + 64, :], r=[RQT], w=[kaug[g]])
        P.dma("sp", kaug[g][64:128, :], K["k_sel"], w=[kaug[g]])
        P.dma("sp", kwT[:, g, :], qT_d[QT_KW + 64 * g:QT_KW + 64 * g + 64, :], r=[RQT], w=[kwT])
        P.dma("sp", vsl[:, :, g, 0:64], v_d[:, V_S + 64 * g:V_S + 64 * g + 64].rearrange("(k p) d -> p k d", p=128),
              r=[RV], w=[vsl])
        P.dma("sp", vwn[:, :, g, 0:64], v_d[:, V_W + 64 * g:V_W + 64 * g + 64].rearrange("(k p) d -> p k d", p=128),
              r=[RV], w=[vwn])
    P.add("dve", lambda e: e.memset(vsl[:, :, :, 64:65], 1.0), w=[vsl])
    P.add("dve", lambda e: e.memset(vwn[:, :, :, 64:65], 1.0), w=[vwn])
    qaugs = [[A.alloc("qaug%d_%d" % (k, h), [512], BF16) for h in range(8)] for k in range(2)]
    Tcs = [A.alloc("Tc%d" % nt, [8, 512], BF16) for nt in range(NNT)]
    pcs = [A.alloc("pc%d" % k, [512], F32) for k in range(3)]
    pts = [A.alloc("npt%d" % k, [512], BF16) for k in range(3)]
    imp = A.alloc("imp", [4, 64], F32)
    wk = A.alloc("wk", [64], F32)
    m8 = A.alloc("m8", [16], F32)
    nm = A.alloc("nm", [4, 128], BF16)
    zc = A.alloc("zc", [4], F32)
    coef = A.alloc("coef", [4], F32)
    onsa = [A.alloc("onsa%d" % k, [4, 512], F32) for k in range(2)]
    pss = [PS.alloc("nps%d" % k, 512) for k in range(3)]
    acccs = [PS.alloc("accc%d" % k, 512) for k in range(2)]
    accs = [PS.alloc("nacc%d" % k, 512, parts=65) for k in range(2)]
    pxx = PS.alloc("pxx", 512)
    psM = Tile("psMv", pxx[:, :].bitcast(BF16)[:, 0:512])
    acnv = pxx[:, :].rearrange("p (a b) -> p a b", a=4)
    oTs = A.alloc("noTs", [512], F32, parts=65)
    kc = [0]
    ac = [0]
    for i in range(NQ):
        qg = qaugs[i % 2]
        on = onsa[i % 2]
        for h in range(8):
            P.dma("sp", qg[h][0:64, :], qT_d[QT_NSAQ + 64 * h:QT_NSAQ + 64 * h + 64, i * 512:(i + 1) * 512], r=[RQT], w=[qg[h]])
        nts = [nt for nt in range(NNT) if 512 * (i + 1) - 1 >= 16 * 128 * nt + 31]
        for nt in nts:
            u0 = 512 * i - 2048 * nt
            assert u0 >= 0
            P.dma("sp", Tcs[nt][:, :, :], dap(L.t_cmp_d, u0 + 2032, [[DL_CMP - 16, 128], [128 * DL_CMP, 8], [1, 512]]),
                  r=[RG], w=[Tcs[nt]])
        for g in range(2):
            units = [(hh, nt) for hh in range(4) for nt in nts]
            base = kc[0]
            kc[0] += len(units)

            def fa(k, units=units, base=base, g=g, qg=qg):
                hh, nt = units[k]
                h = 4 * g + hh
                ps = pss[(base + k) % 3]
                n = nsz[nt]
                mm(ps, ps[0:n, :], kcmpT, kcmpT[0:64, g, nt * 128:nt * 128 + n], qg[h], qg[h][0:64, :], True, False)
                mm(ps, ps[0:n, :], identb, identb[0:n, 0:n], Tcs[nt], Tcs[nt][0:n, h, :], False, True)

            def fb(k, units=units, base=base):
                hh, nt = units[k]
                ps, pc = pss[(base + k) % 3], pcs[(base + k) % 3]
                n = nsz[nt]
                P.add("act", lambda e: e.activation(out=pc[0:n, :], in_=ps[0:n, :], func=AF.Exp), r=[ps], w=[pc])

            def fc(k, units=units, base=base, g=g, i=i, on=on):
                hh, nt = units[k]
                h = 4 * g + hh
                pc = pcs[(base + k) % 3]
                n = nsz[nt]
                accc = acccs[hh % 2]
                avc = accc[:, :].rearrange("p (a b) -> p a b", a=4)
                for bb in range(4):
                    mm(accc, avc[:, bb, :], pc, pc[0:n, bb * 128:(bb + 1) * 128], Rg, Rg[0:n, nt, g, :],
                       nt == nts[0] and bb == 0, nt == nts[-1] and bb == 3)
                if nt == nts[-1]:
                    P.add("dve", lambda e: e.reduce_sum(out=zc[:, :], in_=avc[:, :, 64:128], axis=mybir.AxisListType.X),
                          r=[accc], w=[zc])
                    P.add("dve", lambda e: e.tensor_scalar(out=zc[:, :], in0=zc[:, :], scalar1=1e-30, scalar2=None,
                                                           op0=ALU.max), r=[zc], w=[zc])
                    P.add("dve", lambda e: e.reciprocal(out=zc[:, :], in_=zc[:, :]), r=[zc], w=[zc])
                    P.add("dve", lambda e: e.tensor_tensor(out=coef[:, :], in0=zc[:, :], in1=sg[:, 4 * i:4 * i + 4, 3 * h],
                                                           op=ALU.mult), r=[zc, sg], w=[coef])
                    for bb in range(4):
                        P.add("dve", lambda e, bb=bb: e.tensor_scalar(
                            out=on[:, bb, 64 * h:64 * h + 64], in0=avc[:, bb, 0:64], scalar1=coef[:, bb:bb + 1],
                            scalar2=None, op0=ALU.mult), r=[accc, coef], w=[on])
                        if hh == 0:
                            P.add("dve", lambda e, bb=bb: e.tensor_scalar(
                                out=imp[:, bb, :], in0=avc[:, bb, 64:128], scalar1=zc[:, bb:bb + 1], scalar2=None,
                                op0=ALU.mult), r=[accc, zc], w=[imp])
                        else:
                            P.add("dve", lambda e, bb=bb: e.scalar_tensor_tensor(
                                out=imp[:, bb, :], in0=avc[:, bb, 64:128], scalar=zc[:, bb:bb + 1], in1=imp[:, bb, :],
                                op0=ALU.mult, op1=ALU.add), r=[accc, zc, imp], w=[imp])

            pipeline(len(units), fa, fb, fc, la=2)
            for bb in range(4):
                ti = 4 * i + bb
                w0 = 63 - 2 * ti
                P.add("dve", lambda e, bb=bb, w0=w0: e.tensor_tensor(out=imp[:, bb, :], in0=imp[:, bb, :],
                                                                   in1=kmt[:, w0:w0 + 64], op=ALU.mult), r=[imp, kmt], w=[imp])
                P.add("dve", lambda e, bb=bb, w0=w0: e.tensor_tensor(out=imp[:, bb, :], in0=imp[:, bb, :],
                                                                   in1=ovt[:, w0:w0 + 64], op=ALU.add), r=[imp, ovt], w=[imp])
                P.add("dve", lambda e, bb=bb: e.memset(imp[:, bb, 0:1], 1e30), r=[imp], w=[imp])
                P.add("dve", lambda e, bb=bb: e.max(out=m8[:, 0:8], in_=imp[:, bb, :]), r=[imp], w=[m8])
                P.add("dve", lambda e, bb=bb: e.match_replace(out=wk[:, :], in_to_replace=m8[:, 0:8], in_values=imp[:, bb, :],
                                                              imm_value=-3e38), r=[imp, m8], w=[wk])
                P.add("dve", lambda e: e.max(out=m8[:, 8:16], in_=wk[:, :]), r=[wk, m8], w=[m8])
                P.add("dve", lambda e, bb=bb: e.tensor_scalar(out=nm[:, bb, 0:64], in0=imp[:, bb, :], scalar1=m8[:, 15:16],
                                                              scalar2=NEG, op0=ALU.is_lt, op1=ALU.mult), r=[imp, m8], w=[nm])
                P.add("dve", lambda e, bb=bb: e.tensor_copy(out=nm[:, bb, 64:128], in_=nm[:, bb, 0:64]), r=[nm], w=[nm])

            def mask_rows(g=g, qg=qg):
                for bb in range(4):
                    tr(pxx, psM[:, bb * 128:(bb + 1) * 128], nm, nm[:, bb, :], identb, identb[:, :])
                for hh in range(4):
                    qh = qg[4 * g + hh]
                    evac(qh, qh[64:128, :], pxx, psM[64:128, :], eng="dve")

            for br in (1, 0):
                if br == 0:
                    mask_rows()
                if br == 0:
                    js = list(range(4 * i + 4))
                else:
                    js = list(range(max(0, 4 * i - 4), 4 * i + 4))
                units = [(hh, j) for hh in range(4) for j in js]
                base = kc[0]
                kc[0] += len(units)
                abase = ac[0]
                ac[0] += 4
                jfirst = js[0]

                def rng(j, br=br, i=i):
                    if br == 0:
                        return max(0, j - 4 * i), 4
                    if j == max(0, 4 * i - 4):
                        return 0, 4
                    bbs = [bb for bb in range(4) if 0 <= 4 * i + bb - j <= 4]
                    return bbs[0], bbs[-1] + 1

                def fa(k, units=units, base=base, g=g, qg=qg, br=br, i=i, rng=rng):
                    hh, j = units[k]
                    h = 4 * g + hh
                    qh = qg[h]
                    bb0, bb1 = rng(j)
                    ps = pss[(base + k) % 3]
                    ucol = 128 * (4 * i + bb0 - j)
                    wdt = (bb1 - bb0) * 128
                    if br == 0:
                        near = (4 * i - j) <= 9
                        mm(ps, ps[:, bb0 * 128:bb1 * 128], kaug[g], kaug[g][:, j * 128:(j + 1) * 128],
                           qh, qh[:, bb0 * 128:bb1 * 128], True, not near)
                        if near:
                            mm(ps, ps[:, bb0 * 128:bb1 * 128], identb, identb[:, :], mnsa, mnsa[:, h, ucol:ucol + wdt],
                               False, True)
                    else:
                        needw = ucol + wdt > 512
                        mm(ps, ps[:, bb0 * 128:bb1 * 128], kwT, kwT[0:64, g, j * 128:(j + 1) * 128],
                           qh, qh[0:64, bb0 * 128:bb1 * 128], True, False)
                        mm(ps, ps[:, bb0 * 128:bb1 * 128], identb, identb[:, :], mnsa, mnsa[:, h, ucol:ucol + wdt],
                           False, not needw)
                        if needw:
                            if j == max(0, 4 * i - 4):
                                mm(ps, ps[:, bb0 * 128:bb1 * 128], identb, identb[:, :], wmt, wmt[:, ucol:ucol + wdt],
                                   False, True)
                            else:
                                bw = j - 4 * i + 4
                                mm(ps, ps[:, bw * 128:(bw + 1) * 128], identb, identb[:, :], wmt, wmt[:, 512:640], False, True)

                def fb(k, units=units, base=base, g=g, br=br, i=i, rng=rng):
                    hh, j = units[k]
                    h = 4 * g + hh
                    bb0, bb1 = rng(j)
                    ps, pt = pss[(base + k) % 3], pts[(base + k) % 3]
                    if br == 0 and (4 * i - j) > 9:
                        P.add("act", lambda e: e.activation(out=pt[:, bb0 * 128:bb1 * 128], in_=ps[:, bb0 * 128:bb1 * 128],
                                                            func=AF.Exp, bias=b31[:, h:h + 1]), r=[ps, b31], w=[pt])
                    else:
                        P.add("act", lambda e: e.activation(out=pt[:, bb0 * 128:bb1 * 128], in_=ps[:, bb0 * 128:bb1 * 128],
                                                            func=AF.Exp), r=[ps], w=[pt])

                def fc(k, units=units, base=base, abase=abase, g=g, br=br, i=i, rng=rng, jfirst=jfirst, on=on):
                    hh, j = units[k]
                    h = 4 * g + hh
                    bb0, bb1 = rng(j)
                    pt = pts[(base + k) % 3]
                    acc = accs[(abase + hh) % 2]
                    acv = acnv
                    vt = vsl if br == 0 else vwn
                    mm(acc, acc[0:65, bb0 * 128:bb1 * 128], vt, vt[:, j, g, :], pt, pt[:, bb0 * 128:bb1 * 128],
                       j == jfirst, j == 4 * i + 3)
                    for dk in [d for d in list(deferred) if d[0] <= k]:
                        deferred.remove(dk)
                        dk[1]()
                    if j == 4 * i + 3:
                        evac(oTs, oTs[0:65, :], acc, acc[0:65, :], eng="dve")

                        def fin(h=h, br=br, i=i, on=on):
                            for bb in range(4):
                                tr(pxx, acv[:, bb, 0:65], oTs, oTs[0:65, bb * 128:(bb + 1) * 128], identf, identf[0:65, 0:65])
                            P.add("dve", lambda e: e.reciprocal(out=zc[:, :], in_=acv[:, :, 64]), r=[pxx], w=[zc])
                            P.add("dve", lambda e: e.tensor_tensor(out=coef[:, :], in0=zc[:, :],
                                                                   in1=sg[:, 4 * i:4 * i + 4, 3 * h + 1 + br], op=ALU.mult),
                                  r=[zc, sg], w=[coef])
                            for bb in range(4):
                                P.add("dve", lambda e, bb=bb: e.scalar_tensor_tensor(
                                    out=on[:, bb, 64 * h:64 * h + 64], in0=acv[:, bb, 0:64], scalar=coef[:, bb:bb + 1],
                                    in1=on[:, bb, 64 * h:64 * h + 64], op0=ALU.mult, op1=ALU.add), r=[pxx, coef, on], w=[on])
                        deferred.append((k + 3, fin))

                deferred = []
                pipeline(len(units), fa, fb, fc, la=2)
                for dk in deferred:
                    dk[1]()
        P.dma("sp", o_d[i * 512:(i + 1) * 512, 512:1024].rearrange("(s p) d -> p s d", p=128), on[:, :, :], r=[on], w=[RO])
    A.release(mN)
    PS.release(pN)


def build_ffn(Ld):
    from types import SimpleNamespace
    L = SimpleNamespace(**Ld)
    P, A, PS = L.P, L.A, L.PS
    S, NT, NQ, l = L.S, L.NT, L.NQ, L.l
    mod, gn, identb = L.mod, L.gn, L.identb
    o_d, RO, RX, RW = L.o_d, L.RO, L.RX, L.RW
    mm, tr, evac = L.mm, L.tr, L.evac
    xsrc, out = L.xsrc, L.out
    mE = A.mark()
    pE = PS.mark()
    wo = A.alloc("wo", [8, D], BF16)
    P.dma("sp", wo[:, :, :], L.wo_d[l], r=[RW], w=[wo])
    ots = [A.alloc("ot%d" % k, [D], F32) for k in range(2)]
    xts = [A.alloc("fx%d" % k, [D], F32) for k in range(2)]
    x1s = [A.alloc("x1_%d" % k, [4, D], F32) for k in range(2)]
    mxb = A.alloc("mxb", [D], BF16)
    mxT = A.alloc("mxT", [8, 128], BF16)
    hb = A.alloc("fhb", [4, D], BF16)
    hT = A.alloc("fhT", [8, 512], BF16)
    tmp = A.alloc("ftmp", [D], F32)
    junk = A.alloc("fjunk", [D], BF16)
    st = A.alloc("fst", [8], F32)
    actT = A.alloc("actT", [22, 512], BF16)
    sil = [A.alloc("sil%d" % k, [512], F32) for k in range(2)]
    wgus = [A.alloc("wgu%d" % k, [2, 8, 128], BF16) for k in range(4)]
    wds = [A.alloc("wd%d" % k, [2, D], BF16) for k in range(3)]
    xo = [A.alloc("xo%d" % k, [D], F32) for k in range(2)]
    ptr = [PS.alloc("fptr%d" % k, 512) for k in range(2)]
    pg = [PS.alloc("pg%d" % k, 512) for k in range(2)]

    def bfv(p_):
        return p_[:, :].bitcast(BF16)[:, 0:512]

    pu = [PS.alloc("pu%d" % k, 512) for k in range(2)]
    py = [PS.alloc("py%d" % k, 512) for k in range(2)]
    kt = [0]
    kw = [0]

    def rstd_from(ss_ap, ss_t, n):
        P.add("dve", lambda e: e.tensor_scalar(out=ss_ap, in0=ss_ap, scalar1=1.0 / n, scalar2=1e-6, op0=ALU.mult, op1=ALU.add),
              r=[ss_t], w=[ss_t])
        P.add("act", lambda e: e.activation(out=ss_ap, in_=ss_ap, func=AF.Sqrt), r=[ss_t], w=[ss_t])
        P.add("dve", lambda e: e.reciprocal(out=ss_ap, in_=ss_ap), r=[ss_t], w=[ss_t])

    def dprime(tb, s_):
        t0 = tb * 512
        x1 = x1s[tb % 2]
        r0 = t0 + s_ * 128
        ot, xt = ots[s_ % 2], xts[s_ % 2]
        P.dma("sp", ot[:, :], o_d[r0:r0 + 128, :], r=[RO], w=[ot])
        P.dma("sp", xt[:, :], xsrc[r0:r0 + 128, :], r=[RX[tb]], w=[xt])
        P.add("dve", lambda e: e.memset(st[:, :], 0.0), w=[st])
        for gi, (c0, c1) in enumerate(((0, 256), (256, 512), (512, 1024))):
            P.add("act", lambda e, ot=ot, gi=gi, c0=c0, c1=c1: e.activation(
                out=junk[:, c0:c1], in_=ot[:, c0:c1], func=AF.Square, accum_out=st[:, gi:gi + 1]), r=[ot, st], w=[junk, st])
        P.add("dve", lambda e: e.tensor_scalar(out=st[:, 0:2], in0=st[:, 0:2], scalar1=1.0 / 256, scalar2=1e-6,
                                               op0=ALU.mult, op1=ALU.add), r=[st], w=[st])
        P.add("dve", lambda e: e.tensor_scalar(out=st[:, 2:3], in0=st[:, 2:3], scalar1=1.0 / 512, scalar2=1e-6,
                                               op0=ALU.mult, op1=ALU.add), r=[st], w=[st])
        P.add("act", lambda e: e.activation(out=st[:, 0:3], in_=st[:, 0:3], func=AF.Sqrt), r=[st], w=[st])
        P.add("dve", lambda e: e.reciprocal(out=st[:, 0:3], in_=st[:, 0:3]), r=[st], w=[st])
        for gi, (c0, c1) in enumerate(((0, 256), (256, 512), (512, 1024))):
            P.add("dve", lambda e, ot=ot, gi=gi, c0=c0, c1=c1: e.scalar_tensor_tensor(
                out=mxb[:, c0:c1], in0=ot[:, c0:c1], scalar=st[:, gi:gi + 1], in1=gn[:, c0:c1],
                op0=ALU.mult, op1=ALU.mult), r=[ot, st, gn], w=[mxb])
        for c in range(8):
            pt = ptr[kt[0] % 2]
            tr(pt, bfv(pt)[:, (c % 4) * 128:(c % 4 + 1) * 128], mxb, mxb[:, c * 128:(c + 1) * 128], identb, identb[:, :])
            if c % 4 == 3:
                evac(mxT, mxT[:, c - 3:c + 1, :].rearrange("p a b -> p (a b)"), pt, bfv(pt))
                kt[0] += 1
        for nh in range(2):
            p_ = py[nh]
            for c in range(8):
                mm(p_, p_[:, :], mxT, mxT[:, c, :], wo, wo[:, c, nh * 512:(nh + 1) * 512], c == 0, c == 7)
        P.add("dve", lambda e: e.memset(st[:, 4:6], 0.0), w=[st])
        for nh in range(2):
            P.add("act", lambda e, nh=nh: e.activation(out=junk[:, nh * 512:(nh + 1) * 512], in_=py[nh][:, :], func=AF.Square,
                                                       accum_out=st[:, 4 + nh:5 + nh]), r=[py[nh], st], w=[junk, st])
        P.add("dve", lambda e: e.tensor_tensor(out=st[:, 6:7], in0=st[:, 4:5], in1=st[:, 5:6], op=ALU.add), r=[st], w=[st])
        rstd_from(st[:, 6:7], st, D)
        for nh in range(2):
            P.add("dve", lambda e, nh=nh: e.scalar_tensor_tensor(
                out=tmp[:, nh * 512:(nh + 1) * 512], in0=py[nh][:, :], scalar=st[:, 6:7], in1=mod[:, 2, nh * 512:(nh + 1) * 512],
                op0=ALU.mult, op1=ALU.mult), r=[py[nh], st, mod], w=[tmp])
        P.add("dve", lambda e, xt=xt, s_=s_: e.tensor_tensor(out=x1[:, s_, :], in0=tmp[:, :], in1=xt[:, :], op=ALU.add),
              r=[tmp, xt], w=[x1])
        P.add("dve", lambda e: e.memset(st[:, 7:8], 0.0), w=[st])
        P.add("act", lambda e, s_=s_: e.activation(out=junk[:, :], in_=x1[:, s_, :], func=AF.Square, accum_out=st[:, 7:8]),
              r=[x1, st], w=[junk, st])
        rstd_from(st[:, 7:8], st, D)
        P.add("dve", lambda e, s_=s_: e.scalar_tensor_tensor(out=tmp[:, :], in0=x1[:, s_, :], scalar=st[:, 7:8],
                                                            in1=mod[:, 4, :], op0=ALU.mult, op1=ALU.mult),
              r=[x1, st, mod], w=[tmp])
        P.add("dve", lambda e, s_=s_: e.tensor_tensor(out=hb[:, s_, :], in0=tmp[:, :], in1=mod[:, 3, :], op=ALU.add),
              r=[tmp, mod], w=[hb])

    nxt = []
    nper = (len(nxt) + NQ * 11 - 1) // (NQ * 11) if nxt else 0
    def tpose():
        for c in range(8):
            pt = ptr[kt[0] % 2]
            kt[0] += 1
            for s_ in range(4):
                tr(pt, bfv(pt)[:, s_ * 128:(s_ + 1) * 128], hb, hb[:, s_, c * 128:(c + 1) * 128], identb, identb[:, :])
            evac(hT, hT[:, c, :], pt, bfv(pt))

    for s_ in range(4):
        dprime(0, s_)
    for tb in range(NQ):
        t0 = tb * 512
        x1 = x1s[tb % 2]
        if L.debug and l == 0:
            P.dma("sp", L.dbg["x1"][t0:t0 + 512, :].rearrange("(s p) d -> p s d", p=128), x1[:, :, :], r=[x1])
        if tb == 0:
            tpose()
        for hc in range(22):
            wgu = wgus[kw[0] % 4]
            kw[0] += 1
            P.dma("sp", wgu[:, :, :, :], L.wgu_d[l, hc], r=[RW], w=[wgu])
            pg_, pu_, sl = pg[hc % 2], pu[hc % 2], sil[hc % 2]
            for c in range(8):
                mm(pg_, pg_[:, :], wgu, wgu[:, 0, c, :], hT, hT[:, c, :], c == 0, c == 7)
            for c in range(8):
                mm(pu_, pu_[:, :], wgu, wgu[:, 1, c, :], hT, hT[:, c, :], c == 0, c == 7)
            P.add("act", lambda e, pg_=pg_, sl=sl: e.activation(out=sl[:, :], in_=pg_[:, :], func=AF.Silu), r=[pg_], w=[sl])
            P.add("dve", lambda e, pu_=pu_, sl=sl, hc=hc: e.tensor_tensor(out=actT[:, hc, :], in0=sl[:, :], in1=pu_[:, :],
                                                                        op=ALU.mult), r=[sl, pu_], w=[actT])
            if tb + 1 < NQ and hc in (2, 6, 10, 14):
                dprime(tb + 1, (hc - 2) // 4)
            if hc % 2 == 1:
                for _ in range(nper):
                    if nxt:
                        nxt.pop(0)("act")
        if tb + 1 < NQ:
            tpose()
        yps = [py[0], py[1], pg[0], pg[1], pu[0], pu[1], ptr[0], ptr[1]]

        def ybank(p_):
            return p_[:, :]

        for hc in range(22):
            wd_ = wds[(hc // 2) % 3]
            if hc % 2 == 0:
                P.dma("sp", wd_[:, :, :], L.wd_d[l, :, hc:hc + 2, :], r=[RW], w=[wd_])
            for s_ in range(4):
                for nh in range(2):
                    p_ = yps[s_ * 2 + nh]
                    mm(p_, ybank(p_), actT, actT[:, hc, s_ * 128:(s_ + 1) * 128], wd_, wd_[:, hc % 2, nh * 512:(nh + 1) * 512],
                       hc == 0, hc == 21)
        for es_, s_ in enumerate((1, 2, 0, 3)):
            r0 = t0 + s_ * 128
            xo_ = xo[es_ % 2]
            P.add("dve", lambda e: e.memset(st[:, 4:6], 0.0), w=[st])
            for nh in range(2):
                p_ = yps[s_ * 2 + nh]
                P.add("act", lambda e, nh=nh, p_=p_: e.activation(out=junk[:, nh * 512:(nh + 1) * 512], in_=ybank(p_), func=AF.Square,
                                                                 accum_out=st[:, 4 + nh:5 + nh]), r=[p_, st], w=[junk, st])
            P.add("dve", lambda e: e.tensor_tensor(out=st[:, 6:7], in0=st[:, 4:5], in1=st[:, 5:6], op=ALU.add), r=[st], w=[st])
            rstd_from(st[:, 6:7], st, D)
            for nh in range(2):
                p_ = yps[s_ * 2 + nh]
                P.add("dve", lambda e, nh=nh, p_=p_: e.scalar_tensor_tensor(
                    out=tmp[:, nh * 512:(nh + 1) * 512], in0=ybank(p_), scalar=st[:, 6:7], in1=mod[:, 5, nh * 512:(nh + 1) * 512],
                    op0=ALU.mult, op1=ALU.mult), r=[p_, st, mod], w=[tmp])
            P.add("dve", lambda e, s_=s_, xo_=xo_, x1=x1: e.tensor_tensor(out=xo_[:, :], in0=tmp[:, :], in1=x1[:, s_, :], op=ALU.add),
                  r=[tmp, x1], w=[xo_])
            P.dma("act", out[r0:r0 + 128, :], xo_[:, :], r=[xo_], w=[RX[tb]])
    while nxt:
        nxt.pop(0)("act")
    A.release(mE)
    PS.release(pE)
```
